# Optimizing a Trainium2 kernel written in Bass

```python
import math
import jax, jax.numpy as jnp
from jax import lax
import numpy as np

D_MODEL = 1024
BATCH = 2
SEQ = 8192
DEPTH = 2

GRID_W = 64
CTX_LEN = 256
EPS = 1e-6
NEG_INF = -1e30

CHUNK = 128
SGU_GROUPS = 4
SGU_CH = 128
SGU_WIDTH = SGU_GROUPS * SGU_CH
HEAD_DIM = 64
N_Q_HEADS = 8
N_KV_HEADS = 2
Q_PER_KV = N_Q_HEADS // N_KV_HEADS
ATTN_WIDTH = N_Q_HEADS * HEAD_DIM
KV_WIDTH = N_KV_HEADS * HEAD_DIM
WINDOW = 128
ATTN_BLOCK = 128
ATTN_SCALE = HEAD_DIM ** -0.5
ROPE_BASE = 10000.0
ROPE_FREQS = HEAD_DIM // 4
EVEN_SPLITS = (SGU_WIDTH, 2 * SGU_WIDTH, 2 * SGU_WIDTH + ATTN_WIDTH, 2 * SGU_WIDTH + ATTN_WIDTH + KV_WIDTH)
IN_EVEN = 2 * SGU_WIDTH + ATTN_WIDTH + 2 * KV_WIDTH
MIX_EVEN = SGU_WIDTH + ATTN_WIDTH
D_RNN = 1280
LRU_BLOCKS = 10
LRU_BLOCK = D_RNN // LRU_BLOCKS
CONV_W = 4
CONV_LEFT = 2
LRU_C = 8.0
D_FF = 2816
N_EXPERTS = 8
TOP_K = 2

N_EVEN = (DEPTH + 1) // 2
N_ODD = DEPTH // 2

kernel_name = "hybrid_sgu_swa_rglru_moe_dit_block"


def rmsnorm(x, g):
    xf = x.astype(jnp.float32)
    y = xf * lax.rsqrt(jnp.mean(xf * xf, axis=-1, keepdims=True) + EPS)
    return (y * g.astype(jnp.float32)).astype(x.dtype)


def adaln_params(act, w, b):
    return jnp.split(act @ w + b, 6, axis=-1)


def axial_rope_tables(rows):
    row = jnp.repeat(jnp.arange(rows, dtype=jnp.float32), GRID_W)
    col = jnp.tile(jnp.arange(GRID_W, dtype=jnp.float32), rows)
    freqs = ROPE_BASE ** (-jnp.arange(ROPE_FREQS, dtype=jnp.float32) / ROPE_FREQS)
    ang = jnp.stack([row[:, None] * freqs, col[:, None] * freqs], axis=1)
    return jnp.cos(ang), jnp.sin(ang)


def apply_axial_rope(t, cos, sin):
    B, S, H, _ = t.shape
    tf = t.astype(jnp.float32).reshape(B, S, H, 2, 2, ROPE_FREQS)
    t1, t2 = tf[..., 0, :], tf[..., 1, :]
    cs, sn = cos[None, :, None], sin[None, :, None]
    out = jnp.stack([t1 * cs - t2 * sn, t2 * cs + t1 * sn], axis=-2)
    return out.reshape(B, S, H, HEAD_DIM).astype(t.dtype)


def chunk_spatial_gating(u, v, w_s, b_s):
    B, N, _ = v.shape
    vg = v.astype(jnp.float32).reshape(B, N // CHUNK, CHUNK, SGU_GROUPS, SGU_CH)
    mu = jnp.mean(vg, axis=-1, keepdims=True)
    var = jnp.mean(jnp.square(vg - mu), axis=-1, keepdims=True)
    vg = (vg - mu) * lax.rsqrt(var + EPS)
    mixed = jnp.einsum('gpq,bnqgc->bnpgc', w_s.astype(jnp.float32), vg) \
        + b_s.T.astype(jnp.float32)[None, None, :, :, None]
    return (u.astype(jnp.float32) * mixed.reshape(B, N, SGU_WIDTH)).astype(u.dtype)


def window_attention(q, k, v, k_ctx, v_ctx, sink):
    B, S = q.shape[:2]
    nb = S // ATTN_BLOCK
    qb = q.reshape(B, nb, ATTN_BLOCK, N_KV_HEADS, Q_PER_KV, HEAD_DIM)
    pad = ((0, 0), (ATTN_BLOCK, ATTN_BLOCK), (0, 0), (0, 0))

    def band(t):
        tb = jnp.pad(t, pad).reshape(B, nb + 2, ATTN_BLOCK, N_KV_HEADS, HEAD_DIM)
        return jnp.concatenate([tb[:, :-2], tb[:, 1:-1], tb[:, 2:]], axis=2)

    kw, vw = band(k), band(v)
    s_loc = jnp.einsum('bnqkgd,bnskd->bnkgqs', qb, kw,
                       preferred_element_type=jnp.float32) * ATTN_SCALE
    q_off = jnp.arange(ATTN_BLOCK)[:, None]
    k_off = jnp.arange(3 * ATTN_BLOCK)[None, :] - ATTN_BLOCK
    k_pos = jnp.arange(nb)[:, None, None] * ATTN_BLOCK + k_off[None]
    valid = (jnp.abs(k_off - q_off) <= WINDOW)[None] & (k_pos >= 0) & (k_pos < S)
    s_loc = jnp.where(valid[None, :, None, None], s_loc, NEG_INF)
    s_ctx = jnp.einsum('bnqkgd,blkd->bnkgql', qb, k_ctx,
                       preferred_element_type=jnp.float32) * ATTN_SCALE
    s_sink = sink.astype(jnp.float32).reshape(1, 1, N_KV_HEADS, Q_PER_KV, 1, 1)
    m = jnp.maximum(jnp.maximum(jnp.max(s_loc, axis=-1, keepdims=True),
                                jnp.max(s_ctx, axis=-1, keepdims=True)), s_sink)
    p_loc = jnp.exp(s_loc - m)
    p_ctx = jnp.exp(s_ctx - m)
    inv = 1.0 / (jnp.sum(p_loc, axis=-1, keepdims=True) + jnp.sum(p_ctx, axis=-1, keepdims=True)
                 + jnp.exp(s_sink - m))
    o = jnp.einsum('bnkgqs,bnskd->bnqkgd', (p_loc * inv).astype(v.dtype), vw) \
        + jnp.einsum('bnkgql,blkd->bnqkgd', (p_ctx * inv).astype(v.dtype), v_ctx)
    return o.reshape(B, S, ATTN_WIDTH)


def context_attention(q, k, v, sink):
    B, L = q.shape[:2]
    qg = q.reshape(B, L, N_KV_HEADS, Q_PER_KV, HEAD_DIM)
    s = jnp.einsum('bqkgd,blkd->bkgql', qg, k, preferred_element_type=jnp.float32) * ATTN_SCALE
    sk = jnp.broadcast_to(sink.astype(jnp.float32).reshape(1, N_KV_HEADS, Q_PER_KV, 1, 1), s.shape[:-1] + (1,))
    p = jax.nn.softmax(jnp.concatenate([s, sk], axis=-1), axis=-1)[..., :-1]
    o = jnp.einsum('bkgql,blkd->bqkgd', p.astype(v.dtype), v)
    return o.reshape(B, L, ATTN_WIDTH)


def centred_depthwise_conv(t, w, b):
    out = lax.conv_general_dilated(t, w[:, None, :], window_strides=(1,),
                                   padding=[(CONV_LEFT, CONV_W - 1 - CONV_LEFT)],
                                   dimension_numbers=('NWC', 'WIO', 'NWC'),
                                   feature_group_count=t.shape[-1])
    return out + b


def block_diag_linear(t, w, b):
    lead = t.shape[:-1]
    tb = t.reshape(*lead, LRU_BLOCKS, LRU_BLOCK)
    return jnp.einsum('...hi,hij->...hj', tb, w).reshape(*lead, D_RNN) + b


def rglru_coefficients(t, w_a, b_a, w_x, b_x, lam):
    tf = t.astype(jnp.float32)
    r = jax.nn.sigmoid(block_diag_linear(tf, w_a.astype(jnp.float32), b_a.astype(jnp.float32)))
    i = jax.nn.sigmoid(block_diag_linear(tf, w_x.astype(jnp.float32), b_x.astype(jnp.float32)))
    log_a = -LRU_C * r * jax.nn.softplus(-lam.astype(jnp.float32))
    return jnp.exp(log_a), jnp.sqrt(-jnp.expm1(2.0 * log_a)) * (i * tf)


def _lin_combine(e1, e2):
    a1, b1 = e1
    a2, b2 = e2
    return a1 * a2, a2 * b1 + b2


def linear_recurrence(a, b, h0, reverse):
    edge = -1 if reverse else 0
    b = b.at[:, edge].add(a[:, edge] * h0)
    _, h = lax.associative_scan(_lin_combine, (a, b), reverse=reverse, axis=1)
    return h


def swiglu(t, w1, w3, w2):
    return (jax.nn.silu(t @ w1) * (t @ w3)) @ w2


def moe_swiglu(h, router_w, w1, w3, w2):
    B, N, D = h.shape
    t = h.reshape(B * N, D)
    logits = (t @ router_w).astype(jnp.float32)
    top_v, top_i = lax.top_k(logits, TOP_K)
    gk = jax.nn.softmax(top_v, axis=-1)
    gates = jnp.sum(jax.nn.one_hot(top_i, N_EXPERTS, dtype=jnp.float32) * gk[..., None], axis=1)
    out = jnp.zeros_like(t)
    for e in range(N_EXPERTS):
        out = out + gates[:, e:e + 1].astype(t.dtype) * swiglu(t, w1[e], w3[e], w2[e])
    return out.reshape(B, N, D)


def even_layer(x, xc, c_act, cc_act, cos, sin, ada_w, ada_b, n1, n2, w_in, sgu_w, sgu_b, sink,
               w_out, w1, w3, w2, need_ctx_out):
    B, S, _ = x.shape
    L = xc.shape[1]
    sh1, sc1, g1, sh2, sc2, g2 = [m[:, None, :] for m in adaln_params(c_act, ada_w, ada_b)]
    csh1, csc1, cg1, csh2, csc2, cg2 = adaln_params(cc_act, ada_w, ada_b)
    h = rmsnorm(x, n1) * (1.0 + sc1) + sh1
    hc = rmsnorm(xc, n1) * (1.0 + csc1) + csh1
    u, v, q, k, val = jnp.split(h @ w_in, EVEN_SPLITS, axis=-1)
    uc, vc, qc, kc, valc = jnp.split(hc @ w_in, EVEN_SPLITS, axis=-1)
    kc = kc.reshape(B, L, N_KV_HEADS, HEAD_DIM)
    valc = valc.reshape(B, L, N_KV_HEADS, HEAD_DIM)
    a_out = chunk_spatial_gating(jax.nn.gelu(u), jax.nn.gelu(v), sgu_w, sgu_b)
    q = apply_axial_rope(q.reshape(B, S, N_Q_HEADS, HEAD_DIM), cos, sin)
    k = apply_axial_rope(k.reshape(B, S, N_KV_HEADS, HEAD_DIM), cos, sin)
    b_out = window_attention(q, k, val.reshape(B, S, N_KV_HEADS, HEAD_DIM), kc, valc, sink)
    x = x + g1 * (jnp.concatenate([a_out, b_out], axis=-1) @ w_out)
    x = x + g2 * swiglu(rmsnorm(x, n2) * (1.0 + sc2) + sh2, w1, w3, w2)
    if need_ctx_out:
        ac = chunk_spatial_gating(jax.nn.gelu(uc), jax.nn.gelu(vc), sgu_w, sgu_b)
        bc = context_attention(qc.reshape(B, L, N_Q_HEADS, HEAD_DIM), kc, valc, sink)
        xc = xc + cg1 * (jnp.concatenate([ac, bc], axis=-1) @ w_out)
        xc = xc + cg2 * swiglu(rmsnorm(xc, n2) * (1.0 + csc2) + csh2, w1, w3, w2)
    return x, xc


def odd_layer(x, xc, c_act, cc_act, ada_w, ada_b, n1, n2, w_in, conv_w, conv_b, lru_wa, lru_ba,
              lru_wx, lru_bx, lru_lambda, w_out, router_w, w1, w3, w2, need_ctx_out):
    B = x.shape[0]
    sh1, sc1, g1, sh2, sc2, g2 = [m[:, None, :] for m in adaln_params(c_act, ada_w, ada_b)]
    csh1, csc1, cg1, csh2, csc2, cg2 = adaln_params(cc_act, ada_w, ada_b)
    h = rmsnorm(x, n1) * (1.0 + sc1) + sh1
    hc = rmsnorm(xc, n1) * (1.0 + csc1) + csh1
    gate_l, rec_l = jnp.split(h @ w_in, 2, axis=-1)
    gate_c, rec_c = jnp.split(hc @ w_in, 2, axis=-1)
    rec_l = centred_depthwise_conv(rec_l, conv_w, conv_b)
    rec_c = centred_depthwise_conv(rec_c, conv_w, conv_b)
    lat_dirs, ctx_dirs = [], []
    for d in range(2):
        rev = d == 1
        a_c, b_c = rglru_coefficients(rec_c, lru_wa[d], lru_ba[d], lru_wx[d], lru_bx[d], lru_lambda[d])
        h_ctx = linear_recurrence(a_c, b_c, jnp.zeros((B, D_RNN), jnp.float32), rev)
        h0 = h_ctx[:, 0] if rev else h_ctx[:, -1]
        a_l, b_l = rglru_coefficients(rec_l, lru_wa[d], lru_ba[d], lru_wx[d], lru_bx[d], lru_lambda[d])
        lat_dirs.append(linear_recurrence(a_l, b_l, h0, rev))
        ctx_dirs.append(h_ctx)
    y = jax.nn.gelu(gate_l) * (lat_dirs[0] + lat_dirs[1]).astype(x.dtype)
    x = x + g1 * (y @ w_out)
    x = x + g2 * moe_swiglu(rmsnorm(x, n2) * (1.0 + sc2) + sh2, router_w, w1, w3, w2)
    if need_ctx_out:
        yc = jax.nn.gelu(gate_c) * (ctx_dirs[0] + ctx_dirs[1]).astype(xc.dtype)
        xc = xc + cg1 * (yc @ w_out)
        xc = xc + cg2 * moe_swiglu(rmsnorm(xc, n2) * (1.0 + csc2) + csh2, router_w, w1, w3, w2)
    return x, xc


def setup_inputs(seed: int = 0) -> dict:
    key = jax.random.key(seed)
    keys = iter(jax.random.split(key, 48))
    D = D_MODEL

    def nrm(shape, scale):
        return scale * jax.random.normal(next(keys), shape, jnp.float32)

    u_lam = jax.random.uniform(next(keys), (N_ODD, 2, D_RNN), jnp.float32, minval=0.9, maxval=0.999)
    a0 = u_lam ** (1.0 / LRU_C)
    lru_lambda = jnp.log(a0) - jnp.log1p(-a0)
    return {
        "x": nrm((BATCH, SEQ, D), 1.0),
        "c": nrm((BATCH, D), 1.0),
        "ctx": nrm((BATCH, CTX_LEN, D), 1.0),
        "c_ctx": nrm((D,), 1.0),
        "ada_w_e": nrm((N_EVEN, D, 6 * D), 0.5 * D ** -0.5),
        "ada_b_e": nrm((N_EVEN, 6 * D), 0.02),
        "norm1_e": 1.0 + nrm((N_EVEN, D), 0.05),
        "norm2_e": 1.0 + nrm((N_EVEN, D), 0.05),
        "w_in_e": nrm((N_EVEN, D, IN_EVEN), D ** -0.5),
        "sgu_w": nrm((N_EVEN, SGU_GROUPS, CHUNK, CHUNK), 0.5 * CHUNK ** -0.5),
        "sgu_b": 1.0 + nrm((N_EVEN, SGU_GROUPS, CHUNK), 0.05),
        "attn_sink": nrm((N_EVEN, N_Q_HEADS), 0.5),
        "w_out_e": nrm((N_EVEN, MIX_EVEN, D), MIX_EVEN ** -0.5),
        "ffn_w1": nrm((N_EVEN, D, D_FF), D ** -0.5),
        "ffn_w3": nrm((N_EVEN, D, D_FF), D ** -0.5),
        "ffn_w2": nrm((N_EVEN, D_FF, D), D_FF ** -0.5),
        "ada_w_o": nrm((N_ODD, D, 6 * D), 0.5 * D ** -0.5),
        "ada_b_o": nrm((N_ODD, 6 * D), 0.02),
        "norm1_o": 1.0 + nrm((N_ODD, D), 0.05),
        "norm2_o": 1.0 + nrm((N_ODD, D), 0.05),
        "w_in_o": nrm((N_ODD, D, 2 * D_RNN), D ** -0.5),
        "conv_w": nrm((N_ODD, CONV_W, D_RNN), CONV_W ** -0.5),
        "conv_b": nrm((N_ODD, D_RNN), 0.02),
        "lru_wa": nrm((N_ODD, 2, LRU_BLOCKS, LRU_BLOCK, LRU_BLOCK), LRU_BLOCK ** -0.5),
        "lru_ba": nrm((N_ODD, 2, D_RNN), 0.02),
        "lru_wx": nrm((N_ODD, 2, LRU_BLOCKS, LRU_BLOCK, LRU_BLOCK), LRU_BLOCK ** -0.5),
        "lru_bx": nrm((N_ODD, 2, D_RNN), 0.02),
        "lru_lambda": lru_lambda,
        "w_out_o": nrm((N_ODD, D_RNN, D), D_RNN ** -0.5),
        "router_w": nrm((N_ODD, D, N_EXPERTS), D ** -0.5),
        "moe_w1": nrm((N_ODD, N_EXPERTS, D, D_FF), D ** -0.5),
        "moe_w3": nrm((N_ODD, N_EXPERTS, D, D_FF), D ** -0.5),
        "moe_w2": nrm((N_ODD, N_EXPERTS, D_FF, D), D_FF ** -0.5),
        "final_norm": 1.0 + nrm((D,), 0.05),
    }


def reference(x, c, ctx, c_ctx, ada_w_e, ada_b_e, norm1_e, norm2_e, w_in_e, sgu_w, sgu_b, attn_sink,
              w_out_e, ffn_w1, ffn_w3, ffn_w2, ada_w_o, ada_b_o, norm1_o, norm2_o, w_in_o, conv_w, conv_b,
              lru_wa, lru_ba, lru_wx, lru_bx, lru_lambda, w_out_o, router_w, moe_w1, moe_w3, moe_w2,
              final_norm):
    n_tok = x.shape[1]
    rows = n_tok // GRID_W
    cos, sin = axial_rope_tables(rows)
    c_act = jax.nn.silu(c)
    cc_act = jax.nn.silu(c_ctx)
    xc = ctx
    for layer in range(DEPTH):
        need_ctx_out = layer < DEPTH - 1
        j = layer // 2
        if layer % 2 == 0:
            x, xc = even_layer(x, xc, c_act, cc_act, cos, sin, ada_w_e[j], ada_b_e[j], norm1_e[j], norm2_e[j],
                               w_in_e[j], sgu_w[j], sgu_b[j], attn_sink[j], w_out_e[j],
                               ffn_w1[j], ffn_w3[j], ffn_w2[j], need_ctx_out)
        else:
            x, xc = odd_layer(x, xc, c_act, cc_act, ada_w_o[j], ada_b_o[j], norm1_o[j], norm2_o[j],
                              w_in_o[j], conv_w[j], conv_b[j], lru_wa[j], lru_ba[j], lru_wx[j], lru_bx[j],
                              lru_lambda[j], w_out_o[j], router_w[j], moe_w1[j], moe_w3[j], moe_w2[j],
                              need_ctx_out)
    return rmsnorm(x, final_norm)
```

```python
import numpy as np
import concourse.bass as bass
import concourse.mybir as mybir
from concourse.bass_utils import run_bass_kernel_spmd
from contextlib import ExitStack

F32 = mybir.dt.float32
BF16 = mybir.dt.bfloat16
AF = mybir.ActivationFunctionType
ALU = mybir.AluOpType
AX = mybir.AxisListType

D = 1024
KT = 8
T = 2048
CH = 512
MC = 256
NCH = 4
LC = 256
DFF = 2816
NFF = 22
DR = 1280
NB = 10
EPS = 1e-6
SCALE = 0.125
NEG = -1e30
TE = T + 256


class Buf:
    __slots__ = ("w", "r", "name")

    def __init__(self, name=""):
        self.w = None
        self.r = {}
        self.name = name


class Prog:
    ENG = ["pe", "act", "dve", "pool", "sp"]

    def __init__(self, nc, es, n_dsem=40):
        self.nc = nc
        self.es = es
        self.LIMIT = 30000
        self.epoch = {e: 0 for e in self.ENG}
        self.sems = {e + "#0": es.enter_context(nc.semaphore("s_" + e)) for e in self.ENG}
        self.dsems = [es.enter_context(nc.semaphore("d%d" % i)) for i in range(n_dsem)]
        self.dcnt = [0] * n_dsem
        self.dnext = 0
        self.cnt = {e: 0 for e in self.ENG}
        self.seen = {e: {} for e in self.ENG}
        self.q = {e: [] for e in self.ENG}

    def _semh(self, k):
        return self.sems[k[1]] if k[0] == "e" else self.dsems[k[1]]

    def _waits(self, eng, reads, writes, skip_self=False):
        need = {}

        def add(k, v):
            if skip_self and k[0] == "e" and k[1].split("#")[0] == eng:
                return
            if need.get(k, 0) < v:
                need[k] = v
        for b in reads:
            if b.w is not None:
                add(*b.w)
        for b in writes:
            if b.w is not None:
                add(*b.w)
            for k, v in b.r.items():
                add(k, v)
        out = []
        seen = self.seen[eng]
        for k, v in need.items():
            if seen.get(k, 0) < v:
                seen[k] = v
                out.append((k, v))
        return out

    def _mark(self, tok, reads, writes):
        k, v = tok
        for b in reads:
            if b.r.get(k, 0) < v:
                b.r[k] = v
        for b in writes:
            b.w = tok
            b.r = {}

    def op(self, eng, fn, reads=(), writes=(), skip_self=False):
        waits = self._waits(eng, reads, writes, skip_self)
        if self.cnt[eng] >= self.LIMIT:
            self.epoch[eng] += 1
            self.cnt[eng] = 0
            key = eng + "#%d" % self.epoch[eng]
            self.sems[key] = self.es.enter_context(self.nc.semaphore("s_%s_%d" % (eng, self.epoch[eng])))
        key = eng + "#%d" % self.epoch[eng]
        self.cnt[eng] += 1
        tok = (("e", key), self.cnt[eng])
        self.q[eng].append((waits, fn, ("e", key)))
        self._mark(tok, reads, writes)
        return tok

    def dma(self, eng, out, in_, reads=(), writes=(), **kw):
        i = self.dnext
        self.dnext = (i + 1) % len(self.dsems)
        waits = self._waits(eng, reads, writes)
        if self.dcnt[i] > 0:
            k = ("d", i)
            v = self.dcnt[i]
            if self.seen[eng].get(k, 0) < v:
                self.seen[eng][k] = v
                waits.append((k, v))
        self.dcnt[i] += 16
        tok = (("d", i), self.dcnt[i])
        self.q[eng].append((waits, (lambda e: e.dma_start(out=out, in_=in_, **kw)), ("d", i)))
        self._mark(tok, reads, writes)
        return tok

    def barrier(self):
        toks = [(("e", e + "#%d" % self.epoch[e]), self.cnt[e]) for e in self.ENG if self.cnt[e] > 0]
        toks += [(("d", i), c) for i, c in enumerate(self.dcnt) if c > 0]
        for e in self.ENG:
            waits = []
            for k, v in toks:
                if self.seen[e].get(k, 0) < v:
                    self.seen[e][k] = v
                    waits.append((k, v))
            if waits:
                self.q[e].append((waits, None, None))

    def emit(self, block):
        engobj = {"pe": "tensor", "act": "scalar", "dve": "vector", "pool": "gpsimd", "sp": "sync"}

        def make(ename):
            items = self.q[ename]

            def body(e):
                for waits, fn, inc in items:
                    for k, v in waits:
                        e.wait_ge(self._semh(k), v)
                    if fn is None:
                        continue
                    ins = fn(e)
                    if inc[0] == "e":
                        ins.then_inc(self.sems[inc[1]], 1)
                    else:
                        ins.then_inc(self.dsems[inc[1]], 16)
            return body
        for ename in self.ENG:
            getattr(block, engobj[ename])(make(ename))


class Ring:
    uid = 0

    def __init__(self, nc, es, name, shape, dtype, n):
        Ring.uid += 1
        self.tiles = [es.enter_context(nc.sbuf_tensor("%s%d_%d" % (name, i, Ring.uid), list(shape), dtype)) for i in range(n)]
        self.bufs = [Buf("%s%d" % (name, i)) for i in range(n)]
        self.i = 0

    def next(self):
        j = self.i % len(self.tiles)
        self.i += 1
        return self.tiles[j], self.bufs[j]


def build(phases, dbg=False):
    nc = bass.Bass("TRN2", target_bir_lowering=False)

    def din(name, shape):
        return nc.dram_tensor(name, list(shape), F32, kind="ExternalInput").ap()

    def dout(name, shape):
        return nc.dram_tensor(name, list(shape), F32, kind="ExternalOutput").ap()

    P1, P2, P3 = (1 in phases), (2 in phases), (3 in phases)
    first = min(phases)
    last = max(phases)
    I = {}
    I["ccol"] = din("ccol", [128, KT, 2])
    I["vecs"] = din("vecs", [128, 5, KT])
    I["ident"] = din("ident", [128, 128])
    if P1:
        for n_, s_ in [("xT", [128, KT, T]), ("xhT", [128, KT, 256]), ("ctxT", [128, KT, LC]), ("ada_w_e", [D, 6 * D]),
                       ("ada_b_e", [128, 48]), ("w_in_e", [D, 2432]), ("wsT", [128, 4, 128]), ("bsb", [128, 512]),
                       ("sinkb", [128, 8]), ("w_out_e", [D, D]), ("ffn_w1", [D, DFF]), ("ffn_w3", [D, DFF]),
                       ("ffn_w2", [DFF, D]), ("ropeCS", [128, 2, TE]), ("masks", [128, 3, 384])]:
            I[n_] = din(n_, s_)
    I["ada_w_o"] = din("ada_w_o", [D, 6 * D])
    I["ada_b_o"] = din("ada_b_o", [128, 48])
    I["w_in_o"] = din("w_in_o", [D, 2 * DR])
    if first > 1:
        I["x1T_in"] = din("x1T_in", [128, KT, T])
    if P2 and first > 1:
        I["xc1T_in"] = din("xc1T_in", [128, KT, LC])
    if P2 or P3:
        for n_, s_ in [("convw", [128, NB, 4]), ("convb", [128, NB]), ("lru_wa", [2, NB, 128, 128]), ("lru_wx", [2, NB, 128, 128]),
                       ("lru_ba", [128, 2, NB]), ("lru_bx", [128, 2, NB]), ("lru_lam", [128, 2, NB])]:
            I[n_] = din(n_, s_)
        if not P1:
            I["halo"] = din("halo", [128, NB, 3])
    if P3:
        for n_, s_ in [("w_out_o", [DR, D]), ("router_w", [128, KT, 8]), ("moe_w1", [8, D, DFF]), ("moe_w3", [8, D, DFF]),
                       ("moe_w2", [8, DFF, D]), ("sel", [128, 4])]:
            I[n_] = din(n_, s_)
        if not P2:
            I["summ_all"] = din("summ_all", [128, 4, NB, 6])
    O = {}
    if last == 1:
        O["x1T"] = dout("x1T", [128, KT, T])
        O["xc1T"] = dout("xc1T", [128, KT, LC])
        O["brec"] = dout("brec", [128, NB, 3])
    if last == 2:
        O["summ"] = dout("summ", [128, NB, 6])
    if last == 3:
        O["outT"] = dout("outT", [128, KT, T])
    if dbg:
        O["dbg"] = dout("dbg", [128, 4096])

    with ExitStack() as es:
        P = Prog(nc, es)

        def sb(name, shape, dt=F32, st=es):
            Ring.uid += 1
            return st.enter_context(nc.sbuf_tensor("%s_%d" % (name, Ring.uid), list(shape), dt))

        X = sb("X", [128, KT, T])
        XB = [[Buf("X%d_%d" % (kt, c)) for c in range(NCH)] for kt in range(KT)]
        Xc = sb("Xc", [128, KT, LC])
        XcB = [Buf("Xc%d" % kt) for kt in range(KT)]
        pst = es.enter_context(nc.psum_tensor("pst", [128, 4096], F32))
        PSB = [Buf("ps%d" % i) for i in range(8)]
        pstate = {"i": 0, "l": 0}

        def bank():
            i = pstate["i"] % 6
            pstate["i"] += 1
            return pst[:, i * 512:(i + 1) * 512], PSB[i]

        def bank2():
            if pstate["i"] % 2:
                pstate["i"] += 1
            i = pstate["i"] % 6
            pstate["i"] += 2
            return pst[:, i * 512:(i + 2) * 512], [PSB[i], PSB[i + 1]]

        def lbank():
            i = 6 + pstate["l"] % 2
            pstate["l"] += 1
            return pst[:, i * 512:(i + 1) * 512], PSB[i]

        ident32 = sb("ident32", [128, 128])
        identb = sb("identb", [128, 128], BF16)
        onesb = sb("onesb", [128, 128], BF16)
        ones32 = sb("ones32", [128, 128])
        vecs = sb("vecs_sb", [128, 5, KT])
        ccol = sb("ccol_sb", [128, KT, 2])
        cact = sb("cact", [128, KT, 2], BF16)
        modE = sb("modE", [128, 2, 48])
        modO = sb("modO", [128, 2, 48])
        gam = sb("gam", [128, 4, 2, KT])
        CB = Buf("consts")
        block = es.enter_context(nc.Block())

        def mm(out, outbufs, pairs, reads, skip_self=False):
            def f(e, out=out, pairs=pairs):
                n = len(pairs)
                ins = None
                for i, (l, r) in enumerate(pairs):
                    ins = e.matmul(out, lhsT=l, rhs=r, start=(i == 0), stop=(i == n - 1))
                return ins
            P.op("pe", f, reads=reads, writes=outbufs, skip_self=skip_self)

        def dve(f, reads, writes):
            P.op("dve", f, reads, writes)

        def act(f, reads, writes):
            P.op("act", f, reads, writes)

        def pool(f, reads, writes):
            P.op("pool", f, reads, writes)

        P.dma("sp", ident32[:], I["ident"], writes=[CB])
        P.dma("pool", identb[:], I["ident"], writes=[CB])
        P.dma("sp", vecs[:], I["vecs"], writes=[CB])
        P.dma("sp", ccol[:], I["ccol"], writes=[CB])
        dve(lambda e: e.memset(onesb[:], 1.0), [], [CB])
        dve(lambda e: e.memset(ones32[:], 1.0), [], [CB])
        act(lambda e: e.activation(out=cact[:], in_=ccol[:], func=AF.Silu), [CB], [CB])

        def setup_mods(wname, bname, dst):
            with ExitStack() as st:
                wr = Ring(nc, st, "adaw", [128, KT, 768], BF16, 2)
                adab = sb("adab", [128, 48], F32, st)
                ab = Buf()
                P.dma("sp", adab[:], I[bname], writes=[ab])
                ps, pb = lbank()
                wv = I[wname].rearrange("(kt p) n -> p kt n", p=128)
                for piece in range(8):
                    wt, wb = wr.next()
                    P.dma("pool", wt[:], wv[:, :, piece * 768:(piece + 1) * 768], writes=[wb])
                    for m in range(6):
                        col = piece * 6 + m
                        mm(ps[:, col * 2:col * 2 + 2], [pb],
                           [(wt[:, kt, m * 128:(m + 1) * 128], cact[:, kt, :]) for kt in range(KT)], [wb, CB], skip_self=True)
                psv = ps[:, 0:96].rearrange("p (m w) -> p m w", w=2)
                for w in range(2):
                    dve(lambda e, w=w: e.tensor_tensor(out=dst[:, w, :], in0=psv[:, :, w], in1=adab[:], op=ALU.add), [pb, ab], [CB])
                P.barrier()

        def setup_gam(idx, mod, nvec_idx, jsc):
            for w in range(2):
                dve(lambda e, w=w: e.scalar_tensor_tensor(out=gam[:, idx, w, :], in0=mod[:, w, jsc * 8:(jsc + 1) * 8], scalar=1.0,
                                                          in1=vecs[:, nvec_idx, :], op0=ALU.add, op1=ALU.mult), [CB], [CB])

        if P1:
            setup_mods("ada_w_e", "ada_b_e", modE)
            setup_gam(0, modE, 0, 1)
            setup_gam(1, modE, 1, 4)
        setup_mods("ada_w_o", "ada_b_o", modO)
        setup_gam(2, modO, 2, 1)
        setup_gam(3, modO, 3, 4)

        def norm_chunk(st_rings, src, n, gidx, w, beta, dst, dst32=None):
            sqr, rsr, tmr = st_rings
            ps, pb = lbank()
            for kt in range(KT):
                sa, sbuf_ = src(kt)
                sq, sqb = sqr.next()
                act(lambda e, sa=sa, sq=sq: e.activation(out=sq[:, :n], in_=sa, func=AF.Square), [sbuf_], [sqb])
                P.op("pe", (lambda e, ps=ps, sq=sq, kt=kt: e.matmul(ps[:, :n], lhsT=onesb[:], rhs=sq[:, :n], start=(kt == 0), stop=(kt == KT - 1))),
                     [sqb, CB], [pb], skip_self=(kt > 0))
            rs, rsb = rsr.next()
            act(lambda e, rs=rs, ps=ps: e.activation(out=rs[:, :n], in_=ps[:, :n], func=AF.Sqrt, scale=1.0 / D, bias=EPS), [pb], [rsb])
            dve(lambda e, rs=rs: e.reciprocal(out=rs[:, :n], in_=rs[:, :n]), [rsb], [rsb])
            for kt in range(KT):
                sa, sbuf_ = src(kt)
                tm, tmb = tmr.next()
                dve(lambda e, tm=tm, sa=sa, rs=rs: e.tensor_tensor(out=tm[:, :n], in0=sa, in1=rs[:, :n], op=ALU.mult), [sbuf_, rsb], [tmb])
                da, db = dst(kt)
                g = gam[:, gidx, w, kt:kt + 1] if gidx is not None else vecs[:, 4, kt:kt + 1]
                if dst32 is None:
                    if beta is not None:
                        act(lambda e, da=da, tm=tm, g=g, bt=beta(kt): e.activation(out=da, in_=tm[:, :n], func=AF.Identity, scale=g, bias=bt),
                            [tmb, CB], [db])
                    else:
                        act(lambda e, da=da, tm=tm, g=g: e.activation(out=da, in_=tm[:, :n], func=AF.Identity, scale=g), [tmb, CB], [db])
                else:
                    d32, d32b = dst32(kt)
                    act(lambda e, d32=d32, tm=tm, g=g, bt=beta(kt): e.activation(out=d32, in_=tm[:, :n], func=AF.Identity, scale=g, bias=bt),
                        [tmb, CB], [d32b])
                    pool(lambda e, da=da, d32=d32: e.tensor_copy(out=da, in_=d32), [d32b], [db])

        def norm_rings(st):
            return (Ring(nc, st, "nsq", [128, CH], BF16, 3), Ring(nc, st, "nrs", [128, CH], F32, 2), Ring(nc, st, "ntm", [128, CH], F32, 3))

        def xsrc(c, n=CH):
            return lambda kt: (X[:, kt, c * CH:c * CH + n], XB[kt][c])

        def xcsrc():
            return lambda kt: (Xc[:, kt, :], XcB[kt])

        def ffn_pass(st, w1v, w3v, w2v, chunks, gbc=None):
            groups = [(0, 4), (4, 4), (8, 4), (12, 4), (16, 4), (20, 2)]
            w1r, w3r, w2r, actr, sr, sgr = st
            stages = []
            for gi, (f0, G) in enumerate(groups):
                for ci in range(len(chunks)):
                    stages.append((gi, ci))
            wcur = {}

            def load_group(gi):
                f0, G = groups[gi]
                a, ab = w1r.next()
                b, bb = w3r.next()
                c, cb = w2r.next()
                P.dma("pool", a[:, :, :G * 128], w1v[:, :, f0 * 128:(f0 + G) * 128], writes=[ab])
                P.dma("pool", b[:, :, :G * 128], w3v[:, :, f0 * 128:(f0 + G) * 128], writes=[bb])
                P.dma("pool", c[:, :G, :], w2v[:, f0:f0 + G, :], writes=[cb])
                wcur[gi] = (a, ab, b, bb, c, cb)

            def gu_stage(s):
                gi, ci = stages[s]
                f0, G = groups[gi]
                if gi not in wcur:
                    load_group(gi)
                a, ab, b, bb, c, cb = wcur[gi]
                h2, hb, n, xdst, g2, col0 = chunks[ci]
                at, atb = actr.next()
                for i in range(G):
                    pg, pgb = bank()
                    pu, pub = bank()
                    mm(pg[:, :n], [pgb], [(a[:, kt, i * 128:(i + 1) * 128], h2(kt)) for kt in range(KT)], [ab, hb])
                    mm(pu[:, :n], [pub], [(b[:, kt, i * 128:(i + 1) * 128], h2(kt)) for kt in range(KT)], [bb, hb])
                    s_, s_b = sr.next()
                    act(lambda e, s_=s_, pg=pg: e.activation(out=s_[:, :n], in_=pg[:, :n], func=AF.Silu), [pgb], [s_b])
                    if gbc is not None:
                        gt, gtb = gbc
                        sg, sgb = sgr.next()
                        pool(lambda e, sg=sg, s_=s_, gt=gt: e.tensor_tensor(out=sg[:, :n], in0=s_[:, :n], in1=gt[:, col0:col0 + n], op=ALU.mult),
                             [s_b, gtb], [sgb])
                        s_, s_b = sg, sgb
                    dve(lambda e, at=at, i=i, s_=s_, pu=pu: e.tensor_tensor(out=at[:, i, :n], in0=s_[:, :n], in1=pu[:, :n], op=ALU.mult),
                        [s_b, pub], [atb])
                return (at, atb)

            def y_stage(s, atp):
                gi, ci = stages[s]
                f0, G = groups[gi]
                a, ab, b, bb, c, cb = wcur[gi]
                h2, hb, n, xdst, g2, col0 = chunks[ci]
                at, atb = atp
                for f in range(KT):
                    py, pyb = bank()
                    mm(py[:, :n], [pyb], [(c[:, i, f * 128:(f + 1) * 128], at[:, i, :n]) for i in range(G)], [cb, atb])
                    xa, xb_ = xdst(f)
                    dve(lambda e, xa=xa, py=py, g=g2(f): e.scalar_tensor_tensor(out=xa, in0=py[:, :n], scalar=g, in1=xa, op0=ALU.mult, op1=ALU.add),
                        [pyb, xb_, CB], [xb_])
            prev = None
            for s in range(len(stages)):
                cur = gu_stage(s)
                if prev is not None:
                    y_stage(s - 1, prev)
                prev = cur
            y_stage(len(stages) - 1, prev)

        def ffn_rings(st):
            return (Ring(nc, st, "w1g", [128, KT, 512], BF16, 2), Ring(nc, st, "w3g", [128, KT, 512], BF16, 2),
                    Ring(nc, st, "w2g", [128, 4, D], BF16, 2), Ring(nc, st, "actg", [128, 4, CH], BF16, 2),
                    Ring(nc, st, "fs", [128, CH], F32, 3), Ring(nc, st, "fsg", [128, CH], F32, 3))

        if P1:
            for kt in range(KT):
                for c in range(NCH):
                    P.dma("sp", X[:, kt, c * CH:(c + 1) * CH], I["xT"][:, kt, c * CH:(c + 1) * CH], writes=[XB[kt][c]])
                P.dma("sp", Xc[:, kt, :], I["ctxT"][:, kt, :], writes=[XcB[kt]])
            with ExitStack() as st:
                kTe = sb("kTe", [128, TE], BF16, st)
                kTeB = [Buf("kTe%d" % i) for i in range(TE // 128)]
                kcT = sb("kcT", [128, LC], BF16, st)
                kcB = Buf("kcT")
                Ve = sb("Ve", [128, TE // 128, 128], BF16, st)
                VeB = [Buf("Ve%d" % i) for i in range(TE // 128)]
                Vc = sb("Vc", [128, 2, 128], BF16, st)
                VcB = Buf("Vc")
                ropeC = sb("ropeC", [128, TE], F32, st)
                ropeS = sb("ropeS", [128, TE], F32, st)
                maskb = sb("maskb", [128, 3, 384], BF16, st)
                wsT = sb("wsT", [128, 4, 128], BF16, st)
                bsb = sb("bsb", [128, 512], F32, st)
                sinkb = sb("sinkb", [128, 8], F32, st)
                P.dma("sp", ropeC[:], I["ropeCS"][:, 0, :], writes=[CB])
                P.dma("sp", ropeS[:], I["ropeCS"][:, 1, :], writes=[CB])
                P.dma("pool", maskb[:], I["masks"], writes=[CB])
                P.dma("pool", wsT[:], I["wsT"], writes=[CB])
                P.dma("sp", bsb[:], I["bsb"], writes=[CB])
                P.dma("sp", sinkb[:], I["sinkb"], writes=[CB])
                wv = I["w_in_e"].rearrange("(kt p) n -> p kt n", p=128)
                with ExitStack() as st2:
                    Xh = sb("Xh", [128, KT, 256], F32, st2)
                    XhB = Buf("Xh")
                    P.dma("sp", Xh[:], I["xhT"], writes=[XhB])
                    nr = norm_rings(st2)
                    hT = sb("hT", [128, KT, CH], BF16, st2)
                    hTB = Buf("hT")
                    rt = Ring(nc, st2, "ropet", [128, CH], F32, 4)
                    wpre = sb("wpre", [128, KT, 384], BF16, st2)
                    wpB = Buf("wpre")
                    P.dma("pool", wpre[:], wv[:, :, 2048:2432], writes=[wpB])
                    pre = [("ctx", xcsrc(), LC, 1, None, None),
                           ("hl", (lambda kt: (Xh[:, kt, 0:128], XhB)), 128, 0, 0, 0),
                           ("hr", (lambda kt: (Xh[:, kt, 128:256], XhB)), 128, 0, TE - 128, TE // 128 - 1)]
                    for c in range(NCH):
                        pre.append(("lat", xsrc(c), CH, 0, 128 + c * CH, 1 + c * 4))
                    for kind, src, n, w, e0, vt0 in pre:
                        norm_chunk(nr, src, n, 0, w, (lambda kt, w=w: modE[:, w, 0 * 8 + kt:0 * 8 + kt + 1]),
                                   (lambda kt: (hT[:, kt, :n], hTB)))
                        pk, pkb = bank()
                        mm(pk[:, :n], [pkb], [(wpre[:, kt, 0:128], hT[:, kt, :n]) for kt in range(KT)], [wpB, hTB])
                        if kind == "ctx":
                            act(lambda e, pk=pk: e.activation(out=kcT[:, :], in_=pk[:, :LC], func=AF.Copy), [pkb], [kcB])
                        else:
                            pks, pksb = bank()
                            mm(pks[:, :n], [pksb], [(wpre[:, kt, 128:256], hT[:, kt, :n]) for kt in range(KT)], [wpB, hTB])
                            t1, t1b = rt.next()
                            t2, t2b = rt.next()
                            dve(lambda e, t1=t1, pk=pk, e0=e0, n=n: e.tensor_tensor(out=t1[:, :n], in0=pk[:, :n], in1=ropeC[:, e0:e0 + n], op=ALU.mult),
                                [pkb, CB], [t1b])
                            dve(lambda e, t2=t2, pks=pks, e0=e0, n=n: e.tensor_tensor(out=t2[:, :n], in0=pks[:, :n], in1=ropeS[:, e0:e0 + n], op=ALU.mult),
                                [pksb, CB], [t2b])
                            kb_ = kTeB[e0 // 128:(e0 + n) // 128]
                            pool(lambda e, t1=t1, t2=t2, e0=e0, n=n: e.tensor_tensor(out=kTe[:, e0:e0 + n], in0=t1[:, :n], in1=t2[:, :n], op=ALU.add),
                                 [t1b, t2b], kb_)
                        for tt in range(n // 128):
                            pv, pvb = bank()
                            mm(pv[:, 0:128], [pvb], [(hT[:, kt, tt * 128:(tt + 1) * 128], wpre[:, kt, 256:384]) for kt in range(KT)], [wpB, hTB])
                            if kind == "ctx":
                                act(lambda e, pv=pv, tt=tt: e.activation(out=Vc[:, tt, :], in_=pv[:, 0:128], func=AF.Copy), [pvb], [VcB])
                            else:
                                act(lambda e, pv=pv, vi=vt0 + tt: e.activation(out=Ve[:, vi, :], in_=pv[:, 0:128], func=AF.Copy), [pvb], [VeB[vt0 + tt]])
                    P.barrier()
                with ExitStack() as st2:
                    wmain = sb("wmain", [128, KT, 2048], BF16, st2)
                    wmB = Buf("wmain")
                    for q4 in range(4):
                        P.dma("pool", wmain[:, :, q4 * 512:(q4 + 1) * 512], wv[:, :, q4 * 512:(q4 + 1) * 512], writes=[wmB])
                    wout = sb("wout", [128, KT, D], BF16, st2)
                    woB = Buf("wout")
                    P.dma("pool", wout[:], I["w_out_e"].rearrange("(kt p) n -> p kt n", p=128), writes=[woB])
                    nr = (Ring(nc, st2, "nsq2", [128, MC], BF16, 2), Ring(nc, st2, "nrs2", [128, MC], F32, 2), Ring(nc, st2, "ntm2", [128, MC], F32, 2))
                    hT = sb("hT2", [128, KT, MC], BF16, st2)
                    hTB = Buf("hT2")
                    rt = Ring(nc, st2, "ropet2", [128, 512], F32, 3)
                    gu = sb("gu", [128, 4, MC], BF16, st2)
                    guB = Buf("gu")
                    qT = sb("qT", [128, 4, MC], BF16, st2)
                    qTB = Buf("qT")
                    mix = sb("mix", [128, KT, MC], BF16, st2)
                    mixA = Buf("mixA")
                    mixBq = [Buf("mixB%d" % i) for i in range(4)]
                    gvr = Ring(nc, st2, "gv", [128, 512], F32, 2)
                    vnr = Ring(nc, st2, "vn", [128, 512], BF16, 2)
                    str_ = Ring(nc, st2, "bnst", [128, 4, 6], F32, 2)
                    mvr = Ring(nc, st2, "bnmv", [128, 4, 2], F32, 2)
                    sdr = Ring(nc, st2, "sd", [128, 4], F32, 2)
                    ptr = Ring(nc, st2, "Pt", [128, 640], BF16, 2)
                    ptsr = Ring(nc, st2, "PTs", [128, 640], BF16, 2)
                    dgr = Ring(nc, st2, "Dg", [128, 128], BF16, 2)
                    smr = Ring(nc, st2, "sm", [128, 8], F32, 3)
                    main = [("ctx", xcsrc(), LC, 1, None)] + [("lat", (lambda kt, c2=c2: (X[:, kt, c2 * MC:(c2 + 1) * MC], XB[kt][c2 * MC // CH])), MC, 0, c2)
                                                          for c2 in range(T // MC)]
                    for kind, src, n, w, c in main:
                        norm_chunk(nr, src, n, 0, w, (lambda kt, w=w: modE[:, w, kt:kt + 1]), (lambda kt: (hT[:, kt, :n], hTB)))
                        for m in range(4):
                            pu, pub = bank()
                            mm(pu[:, :n], [pub], [(wmain[:, kt, m * 128:(m + 1) * 128], hT[:, kt, :n]) for kt in range(KT)], [wmB, hTB])
                            act(lambda e, pu=pu, m=m, n=n: e.activation(out=gu[:, m, :n], in_=pu[:, :n], func=AF.Gelu_apprx_tanh), [pub], [guB])
                        for j in range(4):
                            pq, pqb = bank()
                            mm(pq[:, :n], [pqb], [(wmain[:, kt, 1024 + j * 128:1024 + (j + 1) * 128], hT[:, kt, :n]) for kt in range(KT)], [wmB, hTB])
                            if kind == "ctx":
                                act(lambda e, pq=pq, j=j, n=n: e.activation(out=qT[:, j, :n], in_=pq[:, :n], func=AF.Copy), [pqb], [qTB])
                            else:
                                pqs, pqsb = bank()
                                mm(pqs[:, :n], [pqsb], [(wmain[:, kt, 1536 + j * 128:1536 + (j + 1) * 128], hT[:, kt, :n]) for kt in range(KT)], [wmB, hTB])
                                e0 = 128 + c * MC
                                t1, t1b = rt.next()
                                t2, t2b = rt.next()
                                dve(lambda e, t1=t1, pq=pq, e0=e0, n=n: e.tensor_tensor(out=t1[:, :n], in0=pq[:, :n], in1=ropeC[:, e0:e0 + n], op=ALU.mult),
                                    [pqb, CB], [t1b])
                                dve(lambda e, t2=t2, pqs=pqs, e0=e0, n=n: e.tensor_tensor(out=t2[:, :n], in0=pqs[:, :n], in1=ropeS[:, e0:e0 + n], op=ALU.mult),
                                    [pqsb, CB], [t2b])
                                pool(lambda e, t1=t1, t2=t2, j=j, n=n: e.tensor_tensor(out=qT[:, j, :n], in0=t1[:, :n], in1=t2[:, :n], op=ALU.add),
                                     [t1b, t2b], [qTB])
                        for tt in range(n // 128):
                            tsl = slice(tt * 128, (tt + 1) * 128)
                            pv, pvb = bank()
                            mm(pv[:, :], [pvb], [(hT[:, kt, tsl], wmain[:, kt, 512:1024]) for kt in range(KT)], [wmB, hTB])
                            gv, gvb = gvr.next()
                            act(lambda e, gv=gv, pv=pv: e.activation(out=gv[:], in_=pv[:], func=AF.Gelu_apprx_tanh), [pvb], [gvb])
                            stt, stb = str_.next()
                            mv, mvb = mvr.next()
                            for g in range(4):
                                dve(lambda e, stt=stt, gv=gv, g=g: e.bn_stats(out=stt[:, g, :], in_=gv[:, g * 128:(g + 1) * 128]), [gvb], [stb])
                                dve(lambda e, stt=stt, mv=mv, g=g: e.bn_aggr(out=mv[:, g, :], in_=stt[:, g, :]), [stb], [mvb])
                            sd, sdb = sdr.next()
                            act(lambda e, sd=sd, mv=mv: e.activation(out=sd[:], in_=mv[:, :, 1], func=AF.Sqrt, bias=EPS, scale=1.0), [mvb], [sdb])
                            dve(lambda e, sd=sd: e.reciprocal(out=sd[:], in_=sd[:]), [sdb], [sdb])
                            vn, vnb = vnr.next()
                            for g in range(4):
                                dve(lambda e, vn=vn, gv=gv, mv=mv, sd=sd, g=g: e.tensor_scalar(
                                    out=vn[:, g * 128:(g + 1) * 128], in0=gv[:, g * 128:(g + 1) * 128], scalar1=mv[:, g, 0:1],
                                    scalar2=sd[:, g:g + 1], op0=ALU.subtract, op1=ALU.mult), [gvb, mvb, sdb], [vnb])
                            pm, pmb = bank()

                            def mixmm(e, pm=pm, vn=vn):
                                ins = None
                                for g in range(4):
                                    ins = e.matmul(pm[:, g * 128:(g + 1) * 128], lhsT=vn[:, g * 128:(g + 1) * 128], rhs=wsT[:, g, :], start=True, stop=True)
                                return ins
                            P.op("pe", mixmm, [vnb, CB], [pmb])
                            t1, t1b = rt.next()
                            dve(lambda e, t1=t1, pm=pm: e.tensor_tensor(out=t1[:], in0=pm[:], in1=bsb[:], op=ALU.add), [pmb, CB], [t1b])
                            pool(lambda e, t1=t1, tsl=tsl: e.tensor_tensor(out=mix[:, 0:4, tsl], in0=t1[:].rearrange("p (g t) -> p g t", g=4),
                                                                            in1=gu[:, 0:4, tsl], op=ALU.mult), [t1b, guB], [mixA])
                        for qb in range(n // 128):
                            qsl = slice(qb * 128, (qb + 1) * 128)
                            for j in range(4):
                                po, pob = lbank()
                                for hh in range(2):
                                    h = j + 4 * hh
                                    psl = slice(hh * 64, (hh + 1) * 64)
                                    S, Sb = bank2()
                                    if kind == "ctx":
                                        nk = 256
                                        P.op("pe", (lambda e, S=S, j=j, psl=psl, qsl=qsl: e.matmul(S[:, 0:256], lhsT=qT[psl, j, qsl], rhs=kcT[psl, :], start=True, stop=True)),
                                             [qTB, kcB], Sb)
                                        vts = [(Vc, 0, VcB), (Vc, 1, VcB)]
                                    else:
                                        nk = 640
                                        E0 = c * MC + qb * 128
                                        gb = c * (MC // 128) + qb
                                        mi = 0 if (c == 0 and qb == 0) else (2 if (c == T // MC - 1 and qb == MC // 128 - 1) else 1)

                                        def qk(e, S=S, j=j, psl=psl, qsl=qsl, E0=E0, mi=mi):
                                            e.matmul(S[:, 0:384], lhsT=qT[psl, j, qsl], rhs=kTe[psl, E0:E0 + 384], start=True, stop=False)
                                            e.matmul(S[:, 0:384], lhsT=identb[:], rhs=maskb[:, mi, :], start=False, stop=True)
                                            e.matmul(S[:, 384:512], lhsT=qT[psl, j, qsl], rhs=kcT[psl, 0:128], start=True, stop=True)
                                            return e.matmul(S[:, 512:640], lhsT=qT[psl, j, qsl], rhs=kcT[psl, 128:256], start=True, stop=True)
                                        P.op("pe", qk, [qTB, kcB, CB] + kTeB[gb:gb + 3], Sb)
                                        vts = [(Ve, gb, VeB[gb]), (Ve, gb + 1, VeB[gb + 1]), (Ve, gb + 2, VeB[gb + 2]), (Vc, 0, VcB), (Vc, 1, VcB)]
                                    sm, smb = smr.next()
                                    dve(lambda e, sm=sm, S=S, nk=nk: e.reduce_max(out=sm[:, 0:1], in_=S[:, 0:nk], axis=AX.X), Sb, [smb])
                                    dve(lambda e, sm=sm, h=h: e.tensor_scalar(out=sm[:, 1:2], in0=sm[:, 0:1], scalar1=SCALE, scalar2=sinkb[:, h:h + 1],
                                                                              op0=ALU.mult, op1=ALU.max), [smb, CB], [smb])
                                    dve(lambda e, sm=sm: e.tensor_scalar(out=sm[:, 2:3], in0=sm[:, 1:2], scalar1=-1.0, scalar2=None, op0=ALU.mult), [smb], [smb])
                                    dve(lambda e, sm=sm: e.memset(sm[:, 3:4], 0.0), [smb], [smb])
                                    pt, ptb = ptr.next()
                                    act(lambda e, pt=pt, S=S, sm=sm, nk=nk: e.activation(out=pt[:, 0:nk], in_=S[:, 0:nk], func=AF.Exp, scale=SCALE,
                                                                                       bias=sm[:, 2:3], accum_out=sm[:, 3:4]), Sb + [smb], [ptb, smb])
                                    act(lambda e, sm=sm, h=h: e.activation(out=sm[:, 4:5], in_=sinkb[:, h:h + 1], func=AF.Exp, scale=1.0, bias=sm[:, 2:3]),
                                        [smb, CB], [smb])
                                    dve(lambda e, sm=sm: e.tensor_tensor(out=sm[:, 5:6], in0=sm[:, 3:4], in1=sm[:, 4:5], op=ALU.add), [smb], [smb])
                                    dve(lambda e, sm=sm: e.reciprocal(out=sm[:, 6:7], in_=sm[:, 5:6]), [smb], [smb])
                                    dg, dgb = dgr.next()
                                    dve(lambda e, dg=dg, sm=sm: e.tensor_scalar(out=dg[:], in0=identb[:], scalar1=sm[:, 6:7], scalar2=None, op0=ALU.mult),
                                        [smb, CB], [dgb])
                                    PT, PTb = bank2()
                                    nkt = nk // 128

                                    def tr(e, PT=PT, pt=pt, dg=dg, nkt=nkt):
                                        ins = None
                                        for k_ in range(nkt):
                                            ins = e.matmul(PT[:, k_ * 128:(k_ + 1) * 128], lhsT=pt[:, k_ * 128:(k_ + 1) * 128], rhs=dg[:], start=True, stop=True)
                                        return ins
                                    P.op("pe", tr, [ptb, dgb], PTb)
                                    pts, ptsb = ptsr.next()
                                    act(lambda e, pts=pts, PT=PT, nk=nk: e.activation(out=pts[:, 0:nk], in_=PT[:, 0:nk], func=AF.Copy), PTb, [ptsb])

                                    def pv_(e, po=po, pts=pts, vts=vts, psl=psl, hh=hh):
                                        ins = None
                                        for k_, (vt, vi, _) in enumerate(vts):
                                            ins = e.matmul(po[psl, 0:128], lhsT=vt[:, vi, hh * 64:(hh + 1) * 64], rhs=pts[:, k_ * 128:(k_ + 1) * 128],
                                                           start=(k_ == 0), stop=(k_ == len(vts) - 1))
                                        return ins
                                    P.op("pe", pv_, [ptsb] + [v[2] for v in vts], [pob], skip_self=(hh == 1))
                                dve(lambda e, po=po, j=j, qsl=qsl: e.tensor_copy(out=mix[:, 4 + j, qsl], in_=po[:, 0:128]), [pob], [mixBq[j]])
                        for f in range(KT):
                            py, pyb = bank()
                            mm(py[:, :n], [pyb], [(wout[:, mt, f * 128:(f + 1) * 128], mix[:, mt, :n]) for mt in range(KT)], [woB, mixA] + mixBq)
                            if kind == "ctx":
                                xa, xb_ = Xc[:, f, :], XcB[f]
                            else:
                                xa, xb_ = X[:, f, c * MC:(c + 1) * MC], XB[f][c * MC // CH]
                            dve(lambda e, xa=xa, py=py, n=n, g=modE[:, w, 16 + f:16 + f + 1]: e.scalar_tensor_tensor(
                                out=xa, in0=py[:, :n], scalar=g, in1=xa, op0=ALU.mult, op1=ALU.add), [pyb, xb_, CB], [xb_])
                    P.barrier()
                P.barrier()
            with ExitStack() as st:
                nr = norm_rings(st)
                h2T = sb("h2T", [128, KT, T], BF16, st)
                h2B = [Buf("h2_%d" % c) for c in range(NCH)]
                h2c = sb("h2c", [128, KT, LC], BF16, st)
                h2cB = Buf("h2c")
                for c in range(NCH):
                    norm_chunk(nr, xsrc(c), CH, 1, 0, (lambda kt: modE[:, 0, 24 + kt:24 + kt + 1]),
                               (lambda kt, c=c: (h2T[:, kt, c * CH:(c + 1) * CH], h2B[c])))
                norm_chunk(nr, xcsrc(), LC, 1, 1, (lambda kt: modE[:, 1, 24 + kt:24 + kt + 1]), (lambda kt: (h2c[:, kt, :], h2cB)))
                chunks = []
                for c in range(NCH):
                    chunks.append(((lambda kt, c=c: h2T[:, kt, c * CH:(c + 1) * CH]), h2B[c], CH,
                                   (lambda f, c=c: (X[:, f, c * CH:(c + 1) * CH], XB[f][c])),
                                   (lambda f: modE[:, 0, 40 + f:40 + f + 1]), c * CH))
                chunks.append(((lambda kt: h2c[:, kt, :]), h2cB, LC, (lambda f: (Xc[:, f, :], XcB[f])),
                               (lambda f: modE[:, 1, 40 + f:40 + f + 1]), 0))
                ffn_pass(ffn_rings(st), I["ffn_w1"].rearrange("(kt p) n -> p kt n", p=128), I["ffn_w3"].rearrange("(kt p) n -> p kt n", p=128),
                         I["ffn_w2"].rearrange("(ft p) n -> p ft n", p=128), chunks)
                P.barrier()
        else:
            for kt in range(KT):
                for c in range(NCH):
                    P.dma("sp", X[:, kt, c * CH:(c + 1) * CH], I["x1T_in"][:, kt, c * CH:(c + 1) * CH], writes=[XB[kt][c]])
                if P2:
                    P.dma("sp", Xc[:, kt, :], I["xc1T_in"][:, kt, :], writes=[XcB[kt]])

        wio = I["w_in_o"].rearrange("(kt p) n -> p kt n", p=128)
        if P1 and last == 1:
            with ExitStack() as st:
                nr = norm_rings(st)
                xb3 = sb("xb3", [128, KT, 4], F32, st)
                xb3B = Buf("xb3")
                hb3 = sb("hb3", [128, KT, 4], BF16, st)
                hb3B = Buf("hb3")
                brec = sb("brec", [128, NB, 3], F32, st)
                brB = Buf("brec")
                for kt in range(KT):
                    dve(lambda e, kt=kt: e.tensor_copy(out=xb3[:, kt, 0:1], in_=X[:, kt, 0:1]), [XB[kt][0]], [xb3B])
                    dve(lambda e, kt=kt: e.tensor_copy(out=xb3[:, kt, 1:3], in_=X[:, kt, T - 2:T]), [XB[kt][3]], [xb3B])
                norm_chunk(nr, (lambda kt: (xb3[:, kt, 0:3], xb3B)), 3, 2, 0, (lambda kt: modO[:, 0, kt:kt + 1]), (lambda kt: (hb3[:, kt, 0:3], hb3B)))
                wr = Ring(nc, st, "wrec", [128, KT, 256], BF16, 2)
                ps, pb = lbank()
                for piece in range(5):
                    wt, wb = wr.next()
                    P.dma("pool", wt[:], wio[:, :, DR + piece * 256:DR + (piece + 1) * 256], writes=[wb])
                    for m in range(2):
                        blk = piece * 2 + m
                        mm(ps[:, blk * 3:blk * 3 + 3], [pb], [(wt[:, kt, m * 128:(m + 1) * 128], hb3[:, kt, 0:3]) for kt in range(KT)], [wb, hb3B], skip_self=True)
                dve(lambda e: e.tensor_copy(out=brec[:].rearrange("p a b -> p (a b)"), in_=ps[:, 0:30]), [pb], [brB])
                P.dma("sp", O["brec"], brec[:], reads=[brB])
                for kt in range(KT):
                    P.dma("sp", O["x1T"][:, kt, :], X[:, kt, :], reads=XB[kt])
                    P.dma("sp", O["xc1T"][:, kt, :], Xc[:, kt, :], reads=[XcB[kt]])
                P.barrier()

        if P2 or P3:
            with ExitStack() as st:
                h1T = sb("h1T", [128, KT, T], BF16, st)
                h1B = [Buf("h1_%d" % c) for c in range(NCH)]
                if P2:
                    h1c = sb("h1c", [128, KT, LC], BF16, st)
                    h1cB = Buf("h1c")
                with ExitStack() as stn:
                    nr = norm_rings(stn)
                    for c in range(NCH):
                        norm_chunk(nr, xsrc(c), CH, 2, 0, (lambda kt: modO[:, 0, kt:kt + 1]), (lambda kt, c=c: (h1T[:, kt, c * CH:(c + 1) * CH], h1B[c])))
                    if P2:
                        norm_chunk(nr, xcsrc(), LC, 2, 1, (lambda kt: modO[:, 1, kt:kt + 1]), (lambda kt: (h1c[:, kt, :], h1cB)))
                    P.barrier()
                cw = sb("cw", [128, NB, 4], F32, st)
                cbv = sb("cbv", [128, NB], F32, st)
                lba = sb("lba", [128, 2, NB], F32, st)
                lbx = sb("lbx", [128, 2, NB], F32, st)
                lam = sb("lam", [128, 2, NB], F32, st)
                sc8 = sb("sc8", [128, 2, NB], F32, st)
                sc16 = sb("sc16", [128, 2, NB], F32, st)
                ltmp = sb("ltmp", [128, 2, NB], F32, st)
                halo = sb("halo", [128, NB, 3], F32, st)
                for t_, n_ in [(cw, "convw"), (cbv, "convb"), (lba, "lru_ba"), (lbx, "lru_bx"), (lam, "lru_lam"), (halo, "halo")]:
                    P.dma("sp", t_[:], I[n_], writes=[CB])
                dve(lambda e: e.tensor_scalar(out=ltmp[:], in0=lam[:], scalar1=-1.0, scalar2=None, op0=ALU.mult), [CB], [CB])
                dve(lambda e: e.tensor_tensor(out=ltmp[:], in0=ltmp[:], in1=lam[:], op=ALU.max), [CB], [CB])
                act(lambda e: e.activation(out=ltmp[:], in_=ltmp[:], func=AF.Exp, scale=-1.0), [CB], [CB])
                act(lambda e: e.activation(out=ltmp[:], in_=ltmp[:], func=AF.Ln, bias=1.0, scale=1.0), [CB], [CB])
                dve(lambda e: e.tensor_scalar(out=sc8[:], in0=lam[:], scalar1=-1.0, scalar2=0.0, op0=ALU.mult, op1=ALU.max), [CB], [CB])
                dve(lambda e: e.tensor_tensor(out=sc8[:], in0=sc8[:], in1=ltmp[:], op=ALU.add), [CB], [CB])
                dve(lambda e: e.tensor_scalar(out=sc16[:], in0=sc8[:], scalar1=-16.0, scalar2=None, op0=ALU.mult), [CB], [CB])
                dve(lambda e: e.tensor_scalar(out=sc8[:], in0=sc8[:], scalar1=-8.0, scalar2=None, op0=ALU.mult), [CB], [CB])
                summ = sb("summ", [128, NB, 6], F32, st)
                summB = Buf("summ")
                rsum = sb("rsum", [128, NB, 2, NCH], F32, st)
                rsB = Buf("rsum")
                hin = sb("hin", [128, NB, 2], F32, st)
                hinB = Buf("hin")
                dve(lambda e: e.memset(hin[:], 0.0), [], [hinB])
                dve(lambda e: e.memset(rsum[:], 0.0), [], [rsB])

                def mixer_pass(final):
                    with ExitStack() as st2:
                        wr = Ring(nc, st2, "wrc", [128, KT, 128], BF16, 2)
                        wg = Ring(nc, st2, "wgt", [128, KT, 128], BF16, 2)
                        lw = Ring(nc, st2, "lw", [128, 4, 128], BF16, 2)
                        wo = Ring(nc, st2, "wo", [128, D], BF16, 2)
                        recp = sb("recp", [128, T + 3], F32, st2)
                        recpB = Buf("recp")
                        rc = sb("rc", [128, T], F32, st2)
                        rcB = Buf("rc")
                        rcb = sb("rcb", [128, T], BF16, st2)
                        rcbB = Buf("rcb")
                        hs = [sb("hs%d" % d_, [128, T], F32, st2) for d_ in range(2)]
                        hsB = [Buf("hs%d" % d_) for d_ in range(2)]
                        tr_ = Ring(nc, st2, "lt", [128, CH], F32, 8)
                        yb = Ring(nc, st2, "yblk", [128, T], BF16, 2)
                        if not final:
                            recpc = sb("recpc", [128, LC + 3], F32, st2)
                            rcc = sb("rcc", [128, LC], F32, st2)
                            rccb = sb("rccb", [128, LC], BF16, st2)
                            hsc = sb("hsc", [128, LC], F32, st2)
                            cB_ = Buf("ctxlru")
                            dve(lambda e: e.memset(recpc[:], 0.0), [], [cB_])
                        for blk in range(NB):
                            wt, wb = wr.next()
                            P.dma("pool", wt[:], wio[:, :, DR + blk * 128:DR + (blk + 1) * 128], writes=[wb])
                            lt_, lb_ = lw.next()
                            for d_ in range(2):
                                P.dma("pool", lt_[:, d_, :], I["lru_wa"][d_, blk], writes=[lb_])
                                P.dma("pool", lt_[:, 2 + d_, :], I["lru_wx"][d_, blk], writes=[lb_])
                            if final:
                                gt_, gb_ = wg.next()
                                P.dma("pool", gt_[:], wio[:, :, blk * 128:(blk + 1) * 128], writes=[gb_])
                                wot, wob = wo.next()
                                P.dma("pool", wot[:], I["w_out_o"][blk * 128:(blk + 1) * 128, :], writes=[wob])
                            dve(lambda e, blk=blk: e.tensor_copy(out=recp[:, 0:2], in_=halo[:, blk, 0:2]), [CB], [recpB])
                            dve(lambda e, blk=blk: e.tensor_copy(out=recp[:, T + 2:T + 3], in_=halo[:, blk, 2:3]), [CB], [recpB])
                            for c in range(NCH):
                                pr, prb = bank()
                                mm(pr[:], [prb], [(wt[:, kt, :], h1T[:, kt, c * CH:(c + 1) * CH]) for kt in range(KT)], [wb, h1B[c]])
                                act(lambda e, pr=pr, c=c: e.activation(out=recp[:, 2 + c * CH:2 + (c + 1) * CH], in_=pr[:], func=AF.Copy), [prb], [recpB])
                            dve(lambda e, blk=blk: e.tensor_scalar(out=rc[:], in0=recp[:, 0:T], scalar1=cw[:, blk, 0:1], scalar2=cbv[:, blk:blk + 1],
                                                                    op0=ALU.mult, op1=ALU.add), [recpB, CB], [rcB])
                            for j in range(1, 4):
                                dve(lambda e, blk=blk, j=j: e.scalar_tensor_tensor(out=rc[:], in0=recp[:, j:j + T], scalar=cw[:, blk, j:j + 1], in1=rc[:],
                                                                                     op0=ALU.mult, op1=ALU.add), [recpB, rcB, CB], [rcB])
                            pool(lambda e: e.tensor_copy(out=rcb[:], in_=rc[:]), [rcB], [rcbB])
                            seqs = [("lat", rc, rcb, rcB, rcbB, T, hs)]
                            if not final:
                                pr, prb = bank()
                                mm(pr[:, :LC], [prb], [(wt[:, kt, :], h1c[:, kt, :]) for kt in range(KT)], [wb, h1cB])
                                act(lambda e, pr=pr: e.activation(out=recpc[:, 2:2 + LC], in_=pr[:, :LC], func=AF.Copy), [prb], [cB_])
                                dve(lambda e, blk=blk: e.tensor_scalar(out=rcc[:], in0=recpc[:, 0:LC], scalar1=cw[:, blk, 0:1], scalar2=cbv[:, blk:blk + 1],
                                                                        op0=ALU.mult, op1=ALU.add), [cB_, CB], [cB_])
                                for j in range(1, 4):
                                    dve(lambda e, blk=blk, j=j: e.scalar_tensor_tensor(out=rcc[:], in0=recpc[:, j:j + LC], scalar=cw[:, blk, j:j + 1], in1=rcc[:],
                                                                                         op0=ALU.mult, op1=ALU.add), [cB_, CB], [cB_])
                                pool(lambda e: e.tensor_copy(out=rccb[:], in_=rcc[:]), [cB_], [cB_])
                                seqs = [("ctx", rcc, rccb, cB_, cB_, LC, None)] + seqs
                            for kind, r32, rb16, r32B, rb16B, nt, hs_ in seqs:
                                for d_ in range(2):
                                    nch = (nt + CH - 1) // CH
                                    order = list(range(nch)) if d_ == 0 else list(range(nch - 1, -1, -1))
                                    for oi, c in enumerate(order):
                                        n = min(CH, nt - c * CH)
                                        csl = slice(c * CH, c * CH + n)
                                        pa, pab = bank()
                                        px, pxb = bank()
                                        mm(pa[:, :n], [pab], [(lt_[:, d_, :], rb16[:, csl])], [lb_, rb16B])
                                        mm(px[:, :n], [pxb], [(lt_[:, 2 + d_, :], rb16[:, csl])], [lb_, rb16B])
                                        r_, r_b = tr_.next()
                                        i_, i_b = tr_.next()
                                        a_, a_b = tr_.next()
                                        b_, b_b = tr_.next()
                                        if kind == "lat" and not final:
                                            act(lambda e, r_=r_, pa=pa, n=n, blk=blk, d_=d_, c=c: e.activation(
                                                out=r_[:, :n], in_=pa[:, :n], func=AF.Sigmoid, bias=lba[:, d_, blk:blk + 1], scale=1.0,
                                                accum_out=rsum[:, blk, d_, c:c + 1]), [pab, CB], [r_b, rsB])
                                        else:
                                            act(lambda e, r_=r_, pa=pa, n=n, blk=blk, d_=d_: e.activation(
                                                out=r_[:, :n], in_=pa[:, :n], func=AF.Sigmoid, bias=lba[:, d_, blk:blk + 1], scale=1.0), [pab, CB], [r_b])
                                        act(lambda e, i_=i_, px=px, n=n, blk=blk, d_=d_: e.activation(
                                            out=i_[:, :n], in_=px[:, :n], func=AF.Sigmoid, bias=lbx[:, d_, blk:blk + 1], scale=1.0), [pxb, CB], [i_b])
                                        act(lambda e, a_=a_, r_=r_, n=n, blk=blk, d_=d_: e.activation(
                                            out=a_[:, :n], in_=r_[:, :n], func=AF.Exp, scale=sc8[:, d_, blk:blk + 1]), [r_b, CB], [a_b])
                                        act(lambda e, b_=b_, r_=r_, n=n, blk=blk, d_=d_: e.activation(
                                            out=b_[:, :n], in_=r_[:, :n], func=AF.Exp, scale=sc16[:, d_, blk:blk + 1]), [r_b, CB], [b_b])
                                        act(lambda e, b_=b_, n=n: e.activation(out=b_[:, :n], in_=b_[:, :n], func=AF.Sqrt, scale=-1.0, bias=1.0), [b_b], [b_b])
                                        dve(lambda e, b_=b_, i_=i_, n=n: e.tensor_tensor(out=b_[:, :n], in0=b_[:, :n], in1=i_[:, :n], op=ALU.mult), [b_b, i_b], [b_b])
                                        pool(lambda e, b_=b_, r32=r32, csl=csl, n=n: e.tensor_tensor(out=b_[:, :n], in0=b_[:, :n], in1=r32[:, csl], op=ALU.mult),
                                             [b_b, r32B], [b_b])
                                        if kind == "ctx":
                                            ho, hoB = hsc, cB_
                                            init = 0.0
                                            initB = []
                                        else:
                                            ho, hoB = hs_[d_], hsB[d_]
                                            if oi == 0:
                                                init = hin[:, blk, d_:d_ + 1]
                                                initB = [hinB]
                                            elif d_ == 0:
                                                init = ho[:, c * CH - 1:c * CH]
                                                initB = []
                                            else:
                                                init = ho[:, (c + 1) * CH:(c + 1) * CH + 1]
                                                initB = []
                                        if d_ == 0:
                                            dve(lambda e, ho=ho, a_=a_, b_=b_, csl=csl, n=n, init=init: e.tensor_tensor_scan(
                                                out=ho[:, csl], data0=a_[:, :n], data1=b_[:, :n], initial=init, op0=ALU.mult, op1=ALU.add),
                                                [a_b, b_b, hoB] + initB, [hoB])
                                        else:
                                            lo = c * CH
                                            dve(lambda e, ho=ho, a_=a_, b_=b_, lo=lo, n=n, init=init: e.tensor_tensor_scan(
                                                out=ho[:, lo:lo + n][:, ::-1], data0=a_[:, 0:n][:, ::-1], data1=b_[:, 0:n][:, ::-1], initial=init,
                                                op0=ALU.mult, op1=ALU.add), [a_b, b_b, hoB] + initB, [hoB])
                                    if not final:
                                        if kind == "ctx":
                                            src_ = hsc[:, LC - 1:LC] if d_ == 0 else hsc[:, 0:1]
                                            dve(lambda e, blk=blk, d_=d_, src_=src_: e.tensor_copy(out=summ[:, blk, 4 + d_:5 + d_], in_=src_), [cB_], [summB])
                                        else:
                                            src_ = hs_[0][:, T - 1:T] if d_ == 0 else hs_[1][:, 0:1]
                                            dve(lambda e, blk=blk, d_=d_, src_=src_: e.tensor_copy(out=summ[:, blk, 2 * d_ + 1:2 * d_ + 2], in_=src_), [hsB[d_]], [summB])
                            if final:
                                yt, ytb = yb.next()
                                for c in range(NCH):
                                    csl = slice(c * CH, (c + 1) * CH)
                                    pg, pgb = bank()
                                    mm(pg[:], [pgb], [(gt_[:, kt, :], h1T[:, kt, csl]) for kt in range(KT)], [gb_, h1B[c]])
                                    g_, g_b = tr_.next()
                                    act(lambda e, g_=g_, pg=pg: e.activation(out=g_[:], in_=pg[:], func=AF.Gelu_apprx_tanh), [pgb], [g_b])
                                    s_, s_b = tr_.next()
                                    dve(lambda e, s_=s_, csl=csl: e.tensor_tensor(out=s_[:], in0=hs[0][:, csl], in1=hs[1][:, csl], op=ALU.add), hsB, [s_b])
                                    pool(lambda e, yt=yt, g_=g_, s_=s_, csl=csl: e.tensor_tensor(out=yt[:, csl], in0=g_[:], in1=s_[:], op=ALU.mult), [g_b, s_b], [ytb])
                                for c in range(NCH):
                                    csl = slice(c * CH, (c + 1) * CH)
                                    for f in range(KT):
                                        py, pyb = bank()
                                        mm(py[:], [pyb], [(wot[:, f * 128:(f + 1) * 128], yt[:, csl])], [wob, ytb])
                                        dve(lambda e, py=py, f=f, csl=csl: e.scalar_tensor_tensor(out=X[:, f, csl], in0=py[:], scalar=modO[:, 0, 16 + f:16 + f + 1],
                                                                                                   in1=X[:, f, csl], op0=ALU.mult, op1=ALU.add), [pyb, XB[f][c], CB], [XB[f][c]])
                        if not final:
                            for d_ in range(2):
                                dve(lambda e, d_=d_: e.tensor_reduce(out=summ[:, :, 2 * d_], in_=rsum[:, :, d_, :], axis=AX.X, op=ALU.add), [rsB], [summB])
                                dve(lambda e, d_=d_: e.tensor_tensor(out=summ[:, :, 2 * d_], in0=summ[:, :, 2 * d_], in1=sc8[:, d_, :], op=ALU.mult), [summB, CB], [summB])
                                act(lambda e, d_=d_: e.activation(out=summ[:, :, 2 * d_], in_=summ[:, :, 2 * d_], func=AF.Exp), [summB], [summB])
                        P.barrier()

                if P2:
                    mixer_pass(False)
                    if last == 2:
                        P.dma("sp", O["summ"], summ[:], reads=[summB])
                        P.barrier()
                if P3:
                    sa = sb("summ_all", [128, 4, NB, 6], F32, st)
                    selt = sb("selt", [128, 4], F32, st)
                    cfw = sb("cfw", [128, 4, NB], F32, st)
                    cbw = sb("cbw", [128, 4, NB], F32, st)
                    P.dma("sp", sa[:], I["summ_all"], writes=[CB])
                    P.dma("sp", selt[:], I["sel"], writes=[CB])
                    dve(lambda e: e.tensor_copy(out=cfw[:, 0, :], in_=sa[:, 0, :, 4]), [CB], [CB])
                    for j in range(3):
                        dve(lambda e, j=j: e.tensor_tensor(out=cfw[:, j + 1, :], in0=sa[:, j, :, 0], in1=cfw[:, j, :], op=ALU.mult), [CB], [CB])
                        dve(lambda e, j=j: e.tensor_tensor(out=cfw[:, j + 1, :], in0=cfw[:, j + 1, :], in1=sa[:, j, :, 1], op=ALU.add), [CB], [CB])
                    dve(lambda e: e.tensor_copy(out=cbw[:, 3, :], in_=sa[:, 0, :, 5]), [CB], [CB])
                    for j in range(3, 0, -1):
                        dve(lambda e, j=j: e.tensor_tensor(out=cbw[:, j - 1, :], in0=sa[:, j, :, 2], in1=cbw[:, j, :], op=ALU.mult), [CB], [CB])
                        dve(lambda e, j=j: e.tensor_tensor(out=cbw[:, j - 1, :], in0=cbw[:, j - 1, :], in1=sa[:, j, :, 3], op=ALU.add), [CB], [CB])
                    for d_, cc in enumerate([cfw, cbw]):
                        dve(lambda e, d_=d_, cc=cc: e.tensor_scalar(out=hin[:, :, d_], in0=cc[:, 0, :], scalar1=selt[:, 0:1], scalar2=None, op0=ALU.mult), [CB, hinB], [hinB])
                        for j in range(1, 4):
                            dve(lambda e, d_=d_, cc=cc, j=j: e.scalar_tensor_tensor(out=hin[:, :, d_], in0=cc[:, j, :], scalar=selt[:, j:j + 1], in1=hin[:, :, d_],
                                                                                     op0=ALU.mult, op1=ALU.add), [CB, hinB], [hinB])
                    mixer_pass(True)
                P.barrier()

        if P3:
            with ExitStack() as st:
                h2T = sb("h2Tm", [128, KT, T], BF16, st)
                h2B = [Buf("h2m_%d" % c) for c in range(NCH)]
                gates = sb("gates", [128, T // 128, 8], F32, st)
                gatesB = Buf("gates")
                with ExitStack() as st2:
                    nr = norm_rings(st2)
                    h32 = sb("h32", [128, KT, CH], F32, st2)
                    h32B = Buf("h32")
                    rw = sb("rw", [128, KT, 8], F32, st2)
                    P.dma("sp", rw[:], I["router_w"], writes=[CB])
                    gr = Ring(nc, st2, "gr", [128, 40], F32, 2)
                    for c in range(NCH):
                        norm_chunk(nr, xsrc(c), CH, 3, 0, (lambda kt: modO[:, 0, 24 + kt:24 + kt + 1]),
                                   (lambda kt, c=c: (h2T[:, kt, c * CH:(c + 1) * CH], h2B[c])), dst32=(lambda kt: (h32[:, kt, :], h32B)))
                        for tt in range(4):
                            ti = c * 4 + tt
                            pl, plb = bank()
                            mm(pl[:, 0:8], [plb], [(h32[:, kt, tt * 128:(tt + 1) * 128], rw[:, kt, :]) for kt in range(KT)], [h32B, CB])
                            g_, g_b = gr.next()
                            dve(lambda e, g_=g_, pl=pl: e.tensor_copy(out=g_[:, 0:8], in_=pl[:, 0:8]), [plb], [g_b])
                            dve(lambda e, g_=g_: e.reduce_max(out=g_[:, 8:9], in_=g_[:, 0:8], axis=AX.X), [g_b], [g_b])
                            dve(lambda e, g_=g_: e.tensor_scalar(out=g_[:, 16:24], in0=g_[:, 0:8], scalar1=g_[:, 8:9], scalar2=NEG, op0=ALU.is_equal, op1=ALU.mult), [g_b], [g_b])
                            dve(lambda e, g_=g_: e.tensor_tensor(out=g_[:, 24:32], in0=g_[:, 16:24], in1=g_[:, 0:8], op=ALU.add), [g_b], [g_b])
                            dve(lambda e, g_=g_: e.reduce_max(out=g_[:, 9:10], in_=g_[:, 24:32], axis=AX.X), [g_b], [g_b])
                            dve(lambda e, g_=g_: e.tensor_scalar(out=g_[:, 10:11], in0=g_[:, 8:9], scalar1=-1.0, scalar2=None, op0=ALU.mult), [g_b], [g_b])
                            act(lambda e, g_=g_: e.activation(out=g_[:, 16:24], in_=g_[:, 0:8], func=AF.Exp, bias=g_[:, 10:11], scale=1.0), [g_b], [g_b])
                            dve(lambda e, g_=g_: e.tensor_scalar(out=g_[:, 24:32], in0=g_[:, 0:8], scalar1=g_[:, 9:10], scalar2=None, op0=ALU.is_ge), [g_b], [g_b])
                            dve(lambda e, g_=g_: e.tensor_tensor(out=g_[:, 16:24], in0=g_[:, 16:24], in1=g_[:, 24:32], op=ALU.mult), [g_b], [g_b])
                            dve(lambda e, g_=g_: e.reduce_sum(out=g_[:, 11:12], in_=g_[:, 16:24], axis=AX.X), [g_b], [g_b])
                            dve(lambda e, g_=g_: e.reciprocal(out=g_[:, 11:12], in_=g_[:, 11:12]), [g_b], [g_b])
                            dve(lambda e, g_=g_, ti=ti: e.tensor_scalar(out=gates[:, ti, :], in0=g_[:, 16:24], scalar1=g_[:, 11:12], scalar2=None, op0=ALU.mult), [g_b], [gatesB])
                    P.barrier()
                fr = ffn_rings(st)
                gbr = Ring(nc, st, "gbc", [128, T], F32, 2)
                ger = Ring(nc, st, "Ge", [128, 128], F32, 3)
                for ex in range(8):
                    gt, gtb = gbr.next()
                    for c in range(NCH):
                        pgb_, pgbb = lbank()
                        for tt in range(4):
                            ti = c * 4 + tt
                            ge, geb = ger.next()
                            dve(lambda e, ge=ge, ti=ti, ex=ex: e.tensor_scalar(out=ge[:], in0=ones32[:], scalar1=gates[:, ti, ex:ex + 1], scalar2=None, op0=ALU.mult),
                                [gatesB, CB], [geb])
                            mm(pgb_[:, tt * 128:(tt + 1) * 128], [pgbb], [(ge[:], ident32[:])], [geb, CB], skip_self=True)
                        act(lambda e, gt=gt, pgb_=pgb_, c=c: e.activation(out=gt[:, c * CH:(c + 1) * CH], in_=pgb_[:], func=AF.Copy), [pgbb], [gtb])
                    chunks = []
                    for c in range(NCH):
                        chunks.append(((lambda kt, c=c: h2T[:, kt, c * CH:(c + 1) * CH]), h2B[c], CH,
                                       (lambda f, c=c: (X[:, f, c * CH:(c + 1) * CH], XB[f][c])),
                                       (lambda f: modO[:, 0, 40 + f:40 + f + 1]), c * CH))
                    ffn_pass(fr, I["moe_w1"][ex].rearrange("(kt p) n -> p kt n", p=128), I["moe_w3"][ex].rearrange("(kt p) n -> p kt n", p=128),
                             I["moe_w2"][ex].rearrange("(ft p) n -> p ft n", p=128), chunks, gbc=(gt, gtb))
                P.barrier()
            with ExitStack() as st:
                nr = norm_rings(st)
                orr = Ring(nc, st, "ot", [128, CH], F32, 4)
                for c in range(NCH):
                    sqr, rsr, tmr = nr
                    ps, pb = lbank()
                    for kt in range(KT):
                        sq, sqb = sqr.next()
                        act(lambda e, sq=sq, kt=kt, c=c: e.activation(out=sq[:], in_=X[:, kt, c * CH:(c + 1) * CH], func=AF.Square), [XB[kt][c]], [sqb])
                        P.op("pe", (lambda e, ps=ps, sq=sq, kt=kt: e.matmul(ps[:], lhsT=onesb[:], rhs=sq[:], start=(kt == 0), stop=(kt == KT - 1))),
                             [sqb, CB], [pb], skip_self=(kt > 0))
                    rs, rsb = rsr.next()
                    act(lambda e, rs=rs, ps=ps: e.activation(out=rs[:], in_=ps[:], func=AF.Sqrt, scale=1.0 / D, bias=EPS), [pb], [rsb])
                    dve(lambda e, rs=rs: e.reciprocal(out=rs[:], in_=rs[:]), [rsb], [rsb])
                    for kt in range(KT):
                        ot, otb = orr.next()
                        dve(lambda e, ot=ot, kt=kt, c=c, rs=rs: e.scalar_tensor_tensor(out=ot[:], in0=X[:, kt, c * CH:(c + 1) * CH], scalar=vecs[:, 4, kt:kt + 1],
                                                                                       in1=rs[:], op0=ALU.mult, op1=ALU.mult), [XB[kt][c], rsb, CB], [otb])
                        P.dma("sp", O["outT"][:, kt, c * CH:(c + 1) * CH], ot[:], reads=[otb])
                P.barrier()
        P.barrier()
        P.emit(block)
    return nc


def _fm(v):
    v = np.asarray(v, np.float32)
    return np.ascontiguousarray(v.reshape(-1, 128).T)


def _tokT(a):
    a = np.asarray(a, np.float32)
    return np.ascontiguousarray(a.T.reshape(KT, 128, a.shape[0]).transpose(1, 0, 2))


def _rope_tables(t0):
    pos = np.arange(t0 - 128, t0 - 128 + TE)
    row = (pos // 64).astype(np.float32)
    col = (pos % 64).astype(np.float32)
    freqs = (np.float32(10000.0) ** (-np.arange(16, dtype=np.float32) / np.float32(16))).astype(np.float32)
    out = np.zeros((128, 2, TE), np.float32)
    for p in range(128):
        d = p % 64
        axis, half, f = d // 32, (d % 32) // 16, d % 16
        ang = ((row if axis == 0 else col) * freqs[f]).astype(np.float32)
        out[p, 0] = np.cos(ang)
        out[p, 1] = np.sin(ang) * (-1.0 if half == 0 else 1.0)
    return out


def _masks(s):
    q = np.arange(128)[:, None]
    koff = np.arange(384)[None, :] - 128
    band = np.abs(koff - q) <= 128
    m = np.zeros((128, 3, 384), np.float32)
    for i in range(3):
        v = band.copy()
        if i == 0 and s == 0:
            v &= (koff >= 0)
        if i == 2 and s == 3:
            v &= (koff < 128)
        m[:, i, :] = np.where(v, 0.0, NEG)
    return m


def _swap_idx():
    d = np.arange(64)
    axis, half, f = d // 32, (d % 32) // 16, d % 16
    return axis * 32 + (1 - half) * 16 + f


_CACHE = {}


def _prog(phases):
    key = tuple(phases)
    if key not in _CACHE:
        _CACHE[key] = build(list(phases))
    return _CACHE[key]


def _common(inp, b):
    f32 = lambda a: np.ascontiguousarray(np.asarray(a, np.float32))
    ccol = np.stack([inp["c"][b], inp["c_ctx"]], 0)
    ccol = np.ascontiguousarray(ccol.reshape(2, KT, 128).transpose(2, 1, 0))
    vecs = np.stack([_fm(inp["norm1_e"][0]), _fm(inp["norm2_e"][0]), _fm(inp["norm1_o"][0]), _fm(inp["norm2_o"][0]), _fm(inp["final_norm"])], 1)
    return {"ccol": f32(ccol), "vecs": f32(vecs), "ident": np.eye(128, dtype=np.float32),
            "ada_w_o": f32(inp["ada_w_o"][0]), "ada_b_o": _fm(inp["ada_b_o"][0]), "w_in_o": f32(inp["w_in_o"][0])}


def _lru_common(inp):
    f32 = lambda a: np.ascontiguousarray(np.asarray(a, np.float32))
    cw = np.asarray(inp["conv_w"][0], np.float32)
    convw = np.ascontiguousarray(cw.reshape(4, NB, 128).transpose(2, 1, 0))
    def d2(a):
        return np.ascontiguousarray(np.asarray(a, np.float32).reshape(2, NB, 128).transpose(2, 0, 1))
    return {"convw": convw, "convb": _fm(inp["conv_b"][0]), "lru_wa": f32(inp["lru_wa"][0]), "lru_wx": f32(inp["lru_wx"][0]),
            "lru_ba": d2(inp["lru_ba"][0]), "lru_bx": d2(inp["lru_bx"][0]), "lru_lam": d2(inp["lru_lambda"][0])}


def _maps1(inp):
    f32 = lambda a: np.ascontiguousarray(np.asarray(a, np.float32))
    x = np.asarray(inp["x"], np.float32)
    S = x.shape[1]
    w_in = np.asarray(inp["w_in_e"][0], np.float32)
    u, v, q, k, val = w_in[:, 0:512], w_in[:, 512:1024], w_in[:, 1024:1536], w_in[:, 1536:1664], w_in[:, 1664:1792]
    sw = _swap_idx()
    qp = np.concatenate([np.concatenate([q[:, j * 64:(j + 1) * 64], q[:, (j + 4) * 64:(j + 5) * 64]], 1) for j in range(4)], 1)
    qps = np.concatenate([np.concatenate([q[:, j * 64:(j + 1) * 64][:, sw], q[:, (j + 4) * 64:(j + 5) * 64][:, sw]], 1) for j in range(4)], 1)
    ks = np.concatenate([k[:, 0:64][:, sw], k[:, 64:128][:, sw]], 1)
    w_in_ext = f32(np.concatenate([u, v, qp, qps, k, ks, val], 1))
    wo = np.asarray(inp["w_out_e"][0], np.float32)
    rows = list(range(512))
    for j in range(4):
        rows += list(range(512 + j * 64, 512 + (j + 1) * 64)) + list(range(512 + (j + 4) * 64, 512 + (j + 5) * 64))
    w_out_p = f32(wo[rows, :])
    wsT = f32(np.asarray(inp["sgu_w"][0], np.float32).transpose(2, 0, 1))
    bsb = f32(np.broadcast_to(np.asarray(inp["sgu_b"][0], np.float32).reshape(1, 512), (128, 512)))
    sinkb = f32(np.broadcast_to(np.asarray(inp["attn_sink"][0], np.float32).reshape(1, 8), (128, 8)))
    shared = {"ada_w_e": f32(inp["ada_w_e"][0]), "ada_b_e": _fm(inp["ada_b_e"][0]), "w_in_e": w_in_ext, "wsT": wsT, "bsb": bsb,
              "sinkb": sinkb, "w_out_e": w_out_p, "ffn_w1": f32(inp["ffn_w1"][0]), "ffn_w3": f32(inp["ffn_w3"][0]), "ffn_w2": f32(inp["ffn_w2"][0])}
    maps = []
    for c in range(8):
        b, s = c // 4, c % 4
        t0 = s * T
        xh = np.zeros((256, D), np.float32)
        if s > 0:
            xh[0:128] = x[b, t0 - 128:t0]
        if t0 + T < S:
            xh[128:256] = x[b, t0 + T:t0 + T + 128]
        m = dict(shared)
        m.update(_common(inp, b))
        m.update({"xT": _tokT(x[b, t0:t0 + T]), "xhT": _tokT(xh), "ctxT": _tokT(inp["ctx"][b]), "ropeCS": _rope_tables(t0), "masks": _masks(s)})
        maps.append(m)
    return maps


def kernel(**inp):
    inp = {k: np.asarray(v) for k, v in inp.items()}
    cores = list(range(8))
    r1 = run_bass_kernel_spmd(_prog([1]), _maps1(inp), core_ids=cores).results
    lru = _lru_common(inp)
    halos = []
    for c in range(8):
        b, s = c // 4, c % 4
        h = np.zeros((128, NB, 3), np.float32)
        if s > 0:
            h[:, :, 0:2] = r1[c - 1]["brec"][:, :, 1:3]
        if s < 3:
            h[:, :, 2] = r1[c + 1]["brec"][:, :, 0]
        halos.append(h)
    maps2 = []
    for c in range(8):
        m = dict(lru)
        m.update(_common(inp, c // 4))
        m.update({"x1T_in": r1[c]["x1T"], "xc1T_in": r1[c]["xc1T"], "halo": halos[c]})
        maps2.append(m)
    r2 = run_bass_kernel_spmd(_prog([2]), maps2, core_ids=cores).results
    f32 = lambda a: np.ascontiguousarray(np.asarray(a, np.float32))
    rw = np.ascontiguousarray(np.asarray(inp["router_w"][0], np.float32).reshape(KT, 128, 8).transpose(1, 0, 2))
    shared3 = {"w_out_o": f32(inp["w_out_o"][0]), "router_w": rw, "moe_w1": f32(inp["moe_w1"][0]), "moe_w3": f32(inp["moe_w3"][0]),
               "moe_w2": f32(inp["moe_w2"][0])}
    maps3 = []
    for c in range(8):
        b, s = c // 4, c % 4
        m = dict(lru)
        m.update(shared3)
        m.update(_common(inp, b))
        sel = np.zeros((128, 4), np.float32)
        sel[:, s] = 1.0
        sa = np.ascontiguousarray(np.stack([r2[4 * b + j]["summ"] for j in range(4)], 1))
        m.update({"x1T_in": r1[c]["x1T"], "halo": halos[c], "sel": sel, "summ_all": sa})
        maps3.append(m)
    r3 = run_bass_kernel_spmd(_prog([3]), maps3, core_ids=cores).results
    out = np.zeros((2, 4 * T, D), np.float32)
    for c in range(8):
        b, s = c // 4, c % 4
        out[b, s * T:(s + 1) * T, :] = r3[c]["outT"].transpose(2, 1, 0).reshape(T, D)
    return out
```

```python
import numpy as np
import concourse.bass as bass
import concourse.mybir as mybir
from concourse.bass_utils import run_bass_kernel_spmd
from contextlib import ExitStack
import types

F32 = mybir.dt.float32
BF16 = mybir.dt.bfloat16
AF = mybir.ActivationFunctionType
ALU = mybir.AluOpType
AX = mybir.AxisListType

D = 1024
KT = 8
T = 2048
CH = 512
MC = 256
NCH = 4
LC = 256
DFF = 2816
NFF = 22
DR = 1280
NB = 10
EPS = 1e-6
SCALE = 0.125
NEG = -1e30
TE = T + 256


def _freeze(fn):
    if fn is None or fn.__closure__ is None:
        return fn
    cells = []
    for c in fn.__closure__:
        try:
            cells.append(types.CellType(c.cell_contents))
        except ValueError:
            cells.append(c)
    return types.FunctionType(fn.__code__, fn.__globals__, fn.__name__, fn.__defaults__, tuple(cells))


class Buf:
    __slots__ = ("w", "r", "name")

    def __init__(self, name=""):
        self.w = None
        self.r = {}
        self.name = name


class Prog:
    ENG = ["pe", "act", "dve", "pool", "sp"]

    def __init__(self, nc, es, n_dsem=40):
        self.nc = nc
        self.es = es
        self.LIMIT = 30000
        self.epoch = {e: 0 for e in self.ENG}
        self.sems = {e + "#0": es.enter_context(nc.semaphore("s_" + e)) for e in self.ENG}
        self.dsems = [es.enter_context(nc.semaphore("d%d" % i)) for i in range(n_dsem)]
        self.dcnt = [0] * n_dsem
        self.dnext = 0
        self.cnt = {e: 0 for e in self.ENG}
        self.seen = {e: {} for e in self.ENG}
        self.q = {e: [] for e in self.ENG}
        self.ccsem = es.enter_context(nc.semaphore("ccsem"))
        self.cccnt = 0
        self.pcsem = es.enter_context(nc.semaphore("pcsem"))
        self.pccnt = 0

    def _semh(self, k):
        if k[0] == "c":
            return self.ccsem
        if k[0] == "p":
            return self.pcsem
        return self.sems[k[1]] if k[0] == "e" else self.dsems[k[1]]

    def cc(self, fn, reads=(), writes=()):
        waits = self._waits("pool", reads, writes)
        self.cccnt += 1
        tok = (("c", 0), self.cccnt)
        self.q["pool"].append((waits, _freeze(fn), ("c", 0)))
        self._mark(tok, reads, writes)
        return tok

    def _waits(self, eng, reads, writes, skip_self=False):
        need = {}

        def add(k, v):
            if skip_self and k[0] == "e" and k[1].split("#")[0] == eng:
                return
            if need.get(k, 0) < v:
                need[k] = v
        for b in reads:
            if b.w is not None:
                add(*b.w)
        for b in writes:
            if b.w is not None:
                add(*b.w)
            for k, v in b.r.items():
                add(k, v)
        out = []
        seen = self.seen[eng]
        for k, v in need.items():
            if seen.get(k, 0) < v:
                seen[k] = v
                out.append((k, v))
        return out

    def _mark(self, tok, reads, writes):
        k, v = tok
        for b in reads:
            if b.r.get(k, 0) < v:
                b.r[k] = v
        for b in writes:
            b.w = tok
            b.r = {}

    def op(self, eng, fn, reads=(), writes=(), skip_self=False):
        waits = self._waits(eng, reads, writes, skip_self)
        if self.cnt[eng] >= self.LIMIT:
            self.epoch[eng] += 1
            self.cnt[eng] = 0
            key = eng + "#%d" % self.epoch[eng]
            self.sems[key] = self.es.enter_context(self.nc.semaphore("s_%s_%d" % (eng, self.epoch[eng])))
        key = eng + "#%d" % self.epoch[eng]
        self.cnt[eng] += 1
        tok = (("e", key), self.cnt[eng])
        self.q[eng].append((waits, _freeze(fn), ("e", key)))
        self._mark(tok, reads, writes)
        return tok

    def dma(self, eng, out, in_, reads=(), writes=(), **kw):
        i = self.dnext
        self.dnext = (i + 1) % len(self.dsems)
        waits = self._waits(eng, reads, writes)
        if self.dcnt[i] > 0:
            k = ("d", i)
            v = self.dcnt[i]
            if self.seen[eng].get(k, 0) < v:
                self.seen[eng][k] = v
                waits.append((k, v))
        self.dcnt[i] += 16
        tok = (("d", i), self.dcnt[i])
        self.q[eng].append((waits, (lambda e: e.dma_start(out=out, in_=in_, **kw)), ("d", i)))
        self._mark(tok, reads, writes)
        return tok

    def dma_bulk(self, eng, out, in_):
        self.pccnt += 16
        self.q[eng].append(([], (lambda e: e.dma_start(out=out, in_=in_)), ("p", 0)))

    def bulk_done(self, bufs):
        for b in bufs:
            b.w = (("p", 0), self.pccnt)
            b.r = {}

    def barrier(self, final=False):
        toks = [(("e", e + "#%d" % self.epoch[e]), self.cnt[e]) for e in self.ENG if self.cnt[e] > 0]
        toks += [(("d", i), c) for i, c in enumerate(self.dcnt) if c > 0]
        if self.cccnt > 0:
            toks.append((("c", 0), self.cccnt))
        if final and self.pccnt > 0:
            toks.append((("p", 0), self.pccnt))
        for e in self.ENG:
            waits = []
            for k, v in toks:
                if self.seen[e].get(k, 0) < v:
                    self.seen[e][k] = v
                    waits.append((k, v))
            if waits:
                self.q[e].append((waits, None, None))

    def emit(self, block):
        engobj = {"pe": "tensor", "act": "scalar", "dve": "vector", "pool": "gpsimd", "sp": "sync"}

        def make(ename):
            items = self.q[ename]

            def body(e):
                for waits, fn, inc in items:
                    for k, v in waits:
                        e.wait_ge(self._semh(k), v)
                    if fn is None:
                        continue
                    ins = fn(e)
                    if inc[0] == "e":
                        ins.then_inc(self.sems[inc[1]], 1)
                    elif inc[0] == "c":
                        ins.then_inc(self.ccsem, 1)
                    elif inc[0] == "p":
                        ins.then_inc(self.pcsem, 16)
                    else:
                        ins.then_inc(self.dsems[inc[1]], 16)
            return body
        for ename in self.ENG:
            getattr(block, engobj[ename])(make(ename))


class Ring:
    uid = 0

    def __init__(self, nc, es, name, shape, dtype, n):
        Ring.uid += 1
        self.tiles = [es.enter_context(nc.sbuf_tensor("%s%d_%d" % (name, i, Ring.uid), list(shape), dtype)) for i in range(n)]
        self.bufs = [Buf("%s%d" % (name, i)) for i in range(n)]
        self.i = 0

    def next(self):
        j = self.i % len(self.tiles)
        self.i += 1
        return self.tiles[j], self.bufs[j]


FAST = False


def build(phases, dbg=False):
    nc = bass.Bass("TRN2", target_bir_lowering=False)

    def din(name, shape):
        return nc.dram_tensor(name, list(shape), F32, kind="ExternalInput").ap()

    def dout(name, shape):
        return nc.dram_tensor(name, list(shape), F32, kind="ExternalOutput").ap()

    P1, P2, P3 = (1 in phases), (2 in phases), (3 in phases)
    first = min(phases)
    last = max(phases)
    I = {}
    I["ccol"] = din("ccol", [128, KT, 2])
    I["vecs"] = din("vecs", [128, 5, KT])
    I["ident"] = din("ident", [128, 128])
    if P1:
        for n_, s_ in [("xT", [128, KT, T]), ("xhT", [128, KT, 256]), ("ctxT", [128, KT, LC]), ("ada_w_e", [D, 6 * D]),
                       ("ada_b_e", [128, 48]), ("w_in_e", [D, 2432]), ("wsT", [128, 4, 128]), ("bsb", [128, 512]),
                       ("sinkb", [128, 8]), ("w_out_e", [D, D]), ("ffn_w1", [D, DFF]), ("ffn_w3", [D, DFF]),
                       ("ffn_w2", [DFF, D]), ("ropeCS", [128, 2, TE]), ("masks", [128, 3, 384])]:
            I[n_] = din(n_, s_)
    I["ada_w_o"] = din("ada_w_o", [D, 6 * D])
    I["ada_b_o"] = din("ada_b_o", [128, 48])
    I["w_in_o"] = din("w_in_o", [D, 2 * DR])
    if first > 1:
        I["x1T_in"] = din("x1T_in", [128, KT, T])
    if P2 and first > 1:
        I["xc1T_in"] = din("xc1T_in", [128, KT, LC])
    if P2 or P3:
        for n_, s_ in [("convw", [128, NB, 4]), ("convb", [128, NB]), ("lru_wa", [2, NB, 128, 128]), ("lru_wx", [2, NB, 128, 128]),
                       ("lru_ba", [128, 2, NB]), ("lru_bx", [128, 2, NB]), ("lru_lam", [128, 2, NB])]:
            I[n_] = din(n_, s_)
        if not P1:
            I["halo"] = din("halo", [128, NB, 3])
        else:
            I["selm"] = din("selm", [128, 52])
    if P3:
        for n_, s_ in [("w_out_o", [DR, D]), ("router_w", [128, KT, 8]), ("moe_w1", [8, D, DFF]), ("moe_w3", [8, D, DFF]),
                       ("moe_w2", [8, DFF, D]), ("sel", [128, 4])]:
            I[n_] = din(n_, s_)
        if not P2:
            I["summ_all"] = din("summ_all", [128, 4, NB, 6])
    FUSED = P1 and P2 and P3
    if FUSED:
        cc1_in = nc.dram_tensor("cc1_in", [128, NB * 3], F32).ap()
        cc1_out = nc.dram_tensor("cc1_out", [8 * 128, NB * 3], F32).ap()
        cc2_in = nc.dram_tensor("cc2_in", [128, NB * 6], F32).ap()
        cc2_out = nc.dram_tensor("cc2_out", [8 * 128, NB * 6], F32).ap()
        SCR = {"w_in_o": nc.dram_tensor("w_in_o_bf", [D, 2 * DR], BF16).ap(),
               "lru_wa": nc.dram_tensor("lru_wa_bf", [2, NB, 128, 128], BF16).ap(),
               "lru_wx": nc.dram_tensor("lru_wx_bf", [2, NB, 128, 128], BF16).ap(),
               "w_out_o": nc.dram_tensor("w_out_o_bf", [DR, D], BF16).ap(),
               "moe_w1": nc.dram_tensor("moe_w1_bf", [8, D, DFF], BF16).ap(),
               "moe_w3": nc.dram_tensor("moe_w3_bf", [8, D, DFF], BF16).ap(),
               "moe_w2": nc.dram_tensor("moe_w2_bf", [8, DFF, D], BF16).ap()}
    O = {}
    if last == 1:
        O["x1T"] = dout("x1T", [128, KT, T])
        O["xc1T"] = dout("xc1T", [128, KT, LC])
        O["brec"] = dout("brec", [128, NB, 3])
    if last == 2:
        O["summ"] = dout("summ", [128, NB, 6])
    if last == 3:
        O["outT"] = dout("outT", [128, KT, T])
    if dbg:
        O["dbg"] = dout("dbg", [128, 4096])

    with ExitStack() as es:
        P = Prog(nc, es)

        def sb(name, shape, dt=F32, st=es):
            Ring.uid += 1
            return st.enter_context(nc.sbuf_tensor("%s_%d" % (name, Ring.uid), list(shape), dt))

        X = sb("X", [128, KT, T])
        XB = [[Buf("X%d_%d" % (kt, c)) for c in range(NCH)] for kt in range(KT)]
        Xc = sb("Xc", [128, KT, LC])
        XcB = [Buf("Xc%d" % kt) for kt in range(KT)]
        pst = es.enter_context(nc.psum_tensor("pst", [128, 4096], F32))
        PSB = [Buf("ps%d" % i) for i in range(8)]
        pstate = {"i": 0, "l": 0}

        def bank():
            i = pstate["i"] % 6
            pstate["i"] += 1
            return pst[:, i * 512:(i + 1) * 512], PSB[i]

        def bank2():
            if pstate["i"] % 2:
                pstate["i"] += 1
            i = pstate["i"] % 6
            pstate["i"] += 2
            return pst[:, i * 512:(i + 2) * 512], [PSB[i], PSB[i + 1]]

        def lbank():
            i = 6 + pstate["l"] % 2
            pstate["l"] += 1
            return pst[:, i * 512:(i + 1) * 512], PSB[i]

        ident32 = sb("ident32", [128, 128])
        identb = sb("identb", [128, 128], BF16)
        onesb = sb("onesb", [128, 128], BF16)
        ones32 = sb("ones32", [128, 128])
        vecs = sb("vecs_sb", [128, 5, KT])
        ccol = sb("ccol_sb", [128, KT, 2])
        cact = sb("cact", [128, KT, 2], BF16)
        modE = sb("modE", [128, 2, 48])
        modO = sb("modO", [128, 2, 48])
        gam = sb("gam", [128, 4, 2, KT])
        CB = Buf("consts")
        scrB = Buf("scratch")
        WQ = "sp" if FUSED else "pool"
        WS = SCR if FUSED else I
        WRD = [scrB] if FUSED else []

        def precast():
            RP = 256
            for nm in ("w_in_o", "w_out_o"):
                rows = SCR[nm].shape[0]
                for r0 in range(0, rows, RP):
                    P.dma_bulk("pool", SCR[nm][r0:r0 + RP, :], I[nm][r0:r0 + RP, :])
            for nm in ("lru_wa", "lru_wx"):
                P.dma_bulk("pool", SCR[nm].rearrange("d b i j -> (d b) (i j)"), I[nm].rearrange("d b i j -> (d b) (i j)"))
            for ex in range(8):
                for nm in ("moe_w1", "moe_w3", "moe_w2"):
                    rows = SCR[nm].shape[1]
                    for r0 in range(0, rows, RP):
                        P.dma_bulk("pool", SCR[nm][ex, r0:r0 + RP, :], I[nm][ex, r0:r0 + RP, :])
            P.bulk_done([scrB])
        halo = sb("halo", [128, NB, 3])
        haloB = Buf("halo")
        sa = sb("summ_all", [128, 4, NB, 6])
        saB = Buf("sa")
        selm = sb("selm", [128, 52])
        block = es.enter_context(nc.Block())

        def mm(out, outbufs, pairs, reads, skip_self=False):
            def f(e, out=out, pairs=pairs):
                n = len(pairs)
                ins = None
                for i, (l, r) in enumerate(pairs):
                    ins = e.matmul(out, lhsT=l, rhs=r, start=(i == 0), stop=(i == n - 1))
                return ins
            P.op("pe", f, reads=reads, writes=outbufs, skip_self=skip_self)

        def dve(f, reads, writes):
            P.op("dve", f, reads, writes)

        def act(f, reads, writes):
            P.op("act", f, reads, writes)

        def pool(f, reads, writes):
            P.op("pool", f, reads, writes)

        P.dma("sp", ident32[:], I["ident"], writes=[CB])
        P.dma("pool", identb[:], I["ident"], writes=[CB])
        P.dma("sp", vecs[:], I["vecs"], writes=[CB])
        P.dma("sp", ccol[:], I["ccol"], writes=[CB])
        dve(lambda e: e.memset(onesb[:], 1.0), [], [CB])
        dve(lambda e: e.memset(ones32[:], 1.0), [], [CB])
        act(lambda e: e.activation(out=cact[:], in_=ccol[:], func=AF.Silu), [CB], [CB])

        def setup_mods(wname, bname, dst):
            with ExitStack() as st:
                wr = Ring(nc, st, "adaw", [128, KT, 768], BF16, 2)
                adab = sb("adab", [128, 48], F32, st)
                ab = Buf()
                P.dma("sp", adab[:], I[bname], writes=[ab])
                ps, pb = lbank()
                wv = I[wname].rearrange("(kt p) n -> p kt n", p=128)
                for piece in range(8):
                    wt, wb = wr.next()
                    P.dma("pool", wt[:], wv[:, :, piece * 768:(piece + 1) * 768], writes=[wb])
                    for m in range(6):
                        col = piece * 6 + m
                        mm(ps[:, col * 2:col * 2 + 2], [pb],
                           [(wt[:, kt, m * 128:(m + 1) * 128], cact[:, kt, :]) for kt in range(KT)], [wb, CB], skip_self=True)
                psv = ps[:, 0:96].rearrange("p (m w) -> p m w", w=2)
                for w in range(2):
                    dve(lambda e, w=w: e.tensor_tensor(out=dst[:, w, :], in0=psv[:, :, w], in1=adab[:], op=ALU.add), [pb, ab], [CB])
                P.barrier()

        def setup_gam(idx, mod, nvec_idx, jsc):
            for w in range(2):
                dve(lambda e, w=w: e.scalar_tensor_tensor(out=gam[:, idx, w, :], in0=mod[:, w, jsc * 8:(jsc + 1) * 8], scalar=1.0,
                                                          in1=vecs[:, nvec_idx, :], op0=ALU.add, op1=ALU.mult), [CB], [CB])

        if P1:
            setup_mods("ada_w_e", "ada_b_e", modE)
            setup_gam(0, modE, 0, 1)
            setup_gam(1, modE, 1, 4)
        setup_mods("ada_w_o", "ada_b_o", modO)
        setup_gam(2, modO, 2, 1)
        setup_gam(3, modO, 3, 4)

        def norm_chunk(st_rings, src, n, gidx, w, beta, dst, dst32=None):
            sqr, rsr, tmr = st_rings
            ps, pb = lbank()
            for kt in range(KT):
                sa, sbuf_ = src(kt)
                sq, sqb = sqr.next()
                act(lambda e, sa=sa, sq=sq: e.activation(out=sq[:, :n], in_=sa, func=AF.Square), [sbuf_], [sqb])
                P.op("pe", (lambda e, ps=ps, sq=sq, kt=kt: e.matmul(ps[:, :n], lhsT=onesb[:], rhs=sq[:, :n], start=(kt == 0), stop=(kt == KT - 1))),
                     [sqb, CB], [pb], skip_self=(kt > 0))
            rs, rsb = rsr.next()
            act(lambda e, rs=rs, ps=ps: e.activation(out=rs[:, :n], in_=ps[:, :n], func=AF.Sqrt, scale=1.0 / D, bias=EPS), [pb], [rsb])
            dve(lambda e, rs=rs: e.reciprocal(out=rs[:, :n], in_=rs[:, :n]), [rsb], [rsb])
            for kt in range(KT):
                sa, sbuf_ = src(kt)
                tm, tmb = tmr.next()
                dve(lambda e, tm=tm, sa=sa, rs=rs: e.tensor_tensor(out=tm[:, :n], in0=sa, in1=rs[:, :n], op=ALU.mult), [sbuf_, rsb], [tmb])
                da, db = dst(kt)
                g = gam[:, gidx, w, kt:kt + 1] if gidx is not None else vecs[:, 4, kt:kt + 1]
                if dst32 is None:
                    if beta is not None:
                        act(lambda e, da=da, tm=tm, g=g, bt=beta(kt): e.activation(out=da, in_=tm[:, :n], func=AF.Identity, scale=g, bias=bt),
                            [tmb, CB], [db])
                    else:
                        act(lambda e, da=da, tm=tm, g=g: e.activation(out=da, in_=tm[:, :n], func=AF.Identity, scale=g), [tmb, CB], [db])
                else:
                    d32, d32b = dst32(kt)
                    act(lambda e, d32=d32, tm=tm, g=g, bt=beta(kt): e.activation(out=d32, in_=tm[:, :n], func=AF.Identity, scale=g, bias=bt),
                        [tmb, CB], [d32b])
                    pool(lambda e, da=da, d32=d32: e.tensor_copy(out=da, in_=d32), [d32b], [db])

        def norm_rings(st):
            return (Ring(nc, st, "nsq", [128, CH], BF16, 3), Ring(nc, st, "nrs", [128, CH], F32, 2), Ring(nc, st, "ntm", [128, CH], F32, 3))

        def xsrc(c, n=CH):
            return lambda kt: (X[:, kt, c * CH:c * CH + n], XB[kt][c])

        def xcsrc():
            return lambda kt: (Xc[:, kt, :], XcB[kt])

        def ffn_pass(st, w1v, w3v, w2v, chunks, gbc=None, wq="pool", wrd=()):
            groups = [(0, 4), (4, 4), (8, 4), (12, 4), (16, 4), (20, 2)]
            w1r, w3r, w2r, actr, sr, sgr = st
            stages = []
            for gi, (f0, G) in enumerate(groups):
                for ci in range(len(chunks)):
                    stages.append((gi, ci))
            wcur = {}

            def load_group(gi):
                f0, G = groups[gi]
                a, ab = w1r.next()
                b, bb = w3r.next()
                c, cb = w2r.next()
                P.dma(wq, a[:, :, :G * 128], w1v[:, :, f0 * 128:(f0 + G) * 128], reads=list(wrd), writes=[ab])
                P.dma(wq, b[:, :, :G * 128], w3v[:, :, f0 * 128:(f0 + G) * 128], reads=list(wrd), writes=[bb])
                P.dma(wq, c[:, :G, :], w2v[:, f0:f0 + G, :], reads=list(wrd), writes=[cb])
                wcur[gi] = (a, ab, b, bb, c, cb)

            def gu_stage(s):
                gi, ci = stages[s]
                f0, G = groups[gi]
                if gi not in wcur:
                    load_group(gi)
                a, ab, b, bb, c, cb = wcur[gi]
                h2, hb, n, xdst, g2, col0 = chunks[ci]
                at, atb = actr.next()
                for i in range(G):
                    pg, pgb = bank()
                    pu, pub = bank()
                    mm(pg[:, :n], [pgb], [(a[:, kt, i * 128:(i + 1) * 128], h2(kt)) for kt in range(KT)], [ab, hb])
                    mm(pu[:, :n], [pub], [(b[:, kt, i * 128:(i + 1) * 128], h2(kt)) for kt in range(KT)], [bb, hb])
                    s_, s_b = sr.next()
                    act(lambda e, s_=s_, pg=pg: e.activation(out=s_[:, :n], in_=pg[:, :n], func=AF.Silu), [pgb], [s_b])
                    if gbc is not None:
                        gt, gtb = gbc
                        sg, sgb = sgr.next()
                        pool(lambda e, sg=sg, s_=s_, gt=gt: e.tensor_tensor(out=sg[:, :n], in0=s_[:, :n], in1=gt[:, col0:col0 + n], op=ALU.mult),
                             [s_b, gtb], [sgb])
                        s_, s_b = sg, sgb
                    dve(lambda e, at=at, i=i, s_=s_, pu=pu: e.tensor_tensor(out=at[:, i, :n], in0=s_[:, :n], in1=pu[:, :n], op=ALU.mult),
                        [s_b, pub], [atb])
                return (at, atb)

            def y_stage(s, atp):
                gi, ci = stages[s]
                f0, G = groups[gi]
                a, ab, b, bb, c, cb = wcur[gi]
                h2, hb, n, xdst, g2, col0 = chunks[ci]
                at, atb = atp
                for f in range(KT):
                    py, pyb = bank()
                    mm(py[:, :n], [pyb], [(c[:, i, f * 128:(f + 1) * 128], at[:, i, :n]) for i in range(G)], [cb, atb])
                    xa, xb_ = xdst(f)
                    dve(lambda e, xa=xa, py=py, g=g2(f): e.scalar_tensor_tensor(out=xa, in0=py[:, :n], scalar=g, in1=xa, op0=ALU.mult, op1=ALU.add),
                        [pyb, xb_, CB], [xb_])
            prev = None
            for s in range(len(stages)):
                cur = gu_stage(s)
                if prev is not None:
                    y_stage(s - 1, prev)
                prev = cur
            y_stage(len(stages) - 1, prev)

        def ffn_rings(st):
            return (Ring(nc, st, "w1g", [128, KT, 512], BF16, 2), Ring(nc, st, "w3g", [128, KT, 512], BF16, 2),
                    Ring(nc, st, "w2g", [128, 4, D], BF16, 2), Ring(nc, st, "actg", [128, 4, CH], BF16, 2),
                    Ring(nc, st, "fs", [128, CH], F32, 3), Ring(nc, st, "fsg", [128, CH], F32, 3))

        if P1:
            for kt in range(KT):
                for c in range(NCH):
                    P.dma("sp", X[:, kt, c * CH:(c + 1) * CH], I["xT"][:, kt, c * CH:(c + 1) * CH], writes=[XB[kt][c]])
                P.dma("sp", Xc[:, kt, :], I["ctxT"][:, kt, :], writes=[XcB[kt]])
            with ExitStack() as st:
                kTe = sb("kTe", [128, TE], BF16, st)
                kTeB = [Buf("kTe%d" % i) for i in range(TE // 128)]
                kcT = sb("kcT", [128, LC], BF16, st)
                kcB = Buf("kcT")
                Ve = sb("Ve", [128, TE // 128, 128], BF16, st)
                VeB = [Buf("Ve%d" % i) for i in range(TE // 128)]
                Vc = sb("Vc", [128, 2, 128], BF16, st)
                VcB = Buf("Vc")
                ropeC = sb("ropeC", [128, TE], F32, st)
                ropeS = sb("ropeS", [128, TE], F32, st)
                maskb = sb("maskb", [128, 3, 384], BF16, st)
                wsT = sb("wsT", [128, 4, 128], BF16, st)
                bsb = sb("bsb", [128, 512], F32, st)
                sinkb = sb("sinkb", [128, 8], F32, st)
                P.dma("sp", ropeC[:], I["ropeCS"][:, 0, :], writes=[CB])
                P.dma("sp", ropeS[:], I["ropeCS"][:, 1, :], writes=[CB])
                P.dma("pool", maskb[:], I["masks"], writes=[CB])
                P.dma("pool", wsT[:], I["wsT"], writes=[CB])
                P.dma("sp", bsb[:], I["bsb"], writes=[CB])
                P.dma("sp", sinkb[:], I["sinkb"], writes=[CB])
                wv = I["w_in_e"].rearrange("(kt p) n -> p kt n", p=128)
                with ExitStack() as st2:
                    Xh = sb("Xh", [128, KT, 256], F32, st2)
                    XhB = Buf("Xh")
                    P.dma("sp", Xh[:], I["xhT"], writes=[XhB])
                    nr = norm_rings(st2)
                    hT = sb("hT", [128, KT, CH], BF16, st2)
                    hTB = Buf("hT")
                    rt = Ring(nc, st2, "ropet", [128, CH], F32, 4)
                    wpre = sb("wpre", [128, KT, 384], BF16, st2)
                    wpB = Buf("wpre")
                    P.dma("pool", wpre[:], wv[:, :, 2048:2432], writes=[wpB])
                    pre = [("ctx", xcsrc(), LC, 1, None, None),
                           ("hl", (lambda kt: (Xh[:, kt, 0:128], XhB)), 128, 0, 0, 0),
                           ("hr", (lambda kt: (Xh[:, kt, 128:256], XhB)), 128, 0, TE - 128, TE // 128 - 1)]
                    for c in range(NCH):
                        pre.append(("lat", xsrc(c), CH, 0, 128 + c * CH, 1 + c * 4))
                    for kind, src, n, w, e0, vt0 in ([] if FAST else pre):
                        norm_chunk(nr, src, n, 0, w, (lambda kt, w=w: modE[:, w, 0 * 8 + kt:0 * 8 + kt + 1]),
                                   (lambda kt: (hT[:, kt, :n], hTB)))
                        pk, pkb = bank()
                        mm(pk[:, :n], [pkb], [(wpre[:, kt, 0:128], hT[:, kt, :n]) for kt in range(KT)], [wpB, hTB])
                        if kind == "ctx":
                            act(lambda e, pk=pk: e.activation(out=kcT[:, :], in_=pk[:, :LC], func=AF.Copy), [pkb], [kcB])
                        else:
                            pks, pksb = bank()
                            mm(pks[:, :n], [pksb], [(wpre[:, kt, 128:256], hT[:, kt, :n]) for kt in range(KT)], [wpB, hTB])
                            t1, t1b = rt.next()
                            t2, t2b = rt.next()
                            dve(lambda e, t1=t1, pk=pk, e0=e0, n=n: e.tensor_tensor(out=t1[:, :n], in0=pk[:, :n], in1=ropeC[:, e0:e0 + n], op=ALU.mult),
                                [pkb, CB], [t1b])
                            dve(lambda e, t2=t2, pks=pks, e0=e0, n=n: e.tensor_tensor(out=t2[:, :n], in0=pks[:, :n], in1=ropeS[:, e0:e0 + n], op=ALU.mult),
                                [pksb, CB], [t2b])
                            kb_ = kTeB[e0 // 128:(e0 + n) // 128]
                            pool(lambda e, t1=t1, t2=t2, e0=e0, n=n: e.tensor_tensor(out=kTe[:, e0:e0 + n], in0=t1[:, :n], in1=t2[:, :n], op=ALU.add),
                                 [t1b, t2b], kb_)
                        for tt in range(n // 128):
                            pv, pvb = bank()
                            mm(pv[:, 0:128], [pvb], [(hT[:, kt, tt * 128:(tt + 1) * 128], wpre[:, kt, 256:384]) for kt in range(KT)], [wpB, hTB])
                            if kind == "ctx":
                                act(lambda e, pv=pv, tt=tt: e.activation(out=Vc[:, tt, :], in_=pv[:, 0:128], func=AF.Copy), [pvb], [VcB])
                            else:
                                act(lambda e, pv=pv, vi=vt0 + tt: e.activation(out=Ve[:, vi, :], in_=pv[:, 0:128], func=AF.Copy), [pvb], [VeB[vt0 + tt]])
                    P.barrier()
                with ExitStack() as st2:
                    wmain = sb("wmain", [128, KT, 2048], BF16, st2)
                    wmB = Buf("wmain")
                    for q4 in range(4):
                        P.dma("pool", wmain[:, :, q4 * 512:(q4 + 1) * 512], wv[:, :, q4 * 512:(q4 + 1) * 512], writes=[wmB])
                    wout = sb("wout", [128, KT, D], BF16, st2)
                    woB = Buf("wout")
                    P.dma("pool", wout[:], I["w_out_e"].rearrange("(kt p) n -> p kt n", p=128), writes=[woB])
                    if FUSED:
                        precast()
                    nr = (Ring(nc, st2, "nsq2", [128, MC], BF16, 2), Ring(nc, st2, "nrs2", [128, MC], F32, 2), Ring(nc, st2, "ntm2", [128, MC], F32, 2))
                    hT = sb("hT2", [128, KT, MC], BF16, st2)
                    hTB = Buf("hT2")
                    rt = Ring(nc, st2, "ropet2", [128, 512], F32, 3)
                    gu = sb("gu", [128, 4, MC], BF16, st2)
                    guB = Buf("gu")
                    qT = sb("qT", [128, 4, MC], BF16, st2)
                    qTB = Buf("qT")
                    mix = sb("mix", [128, KT, MC], BF16, st2)
                    mixA = Buf("mixA")
                    mixBq = [Buf("mixB%d" % i) for i in range(4)]
                    gvr = Ring(nc, st2, "gv", [128, 512], F32, 2)
                    vnr = Ring(nc, st2, "vn", [128, 512], BF16, 2)
                    str_ = Ring(nc, st2, "bnst", [128, 4, 6], F32, 2)
                    mvr = Ring(nc, st2, "bnmv", [128, 4, 2], F32, 2)
                    sdr = Ring(nc, st2, "sd", [128, 4], F32, 2)
                    ptr = Ring(nc, st2, "Pt", [128, 640], BF16, 3)
                    ptsr = Ring(nc, st2, "PTs", [128, 640], BF16, 2)
                    dgr = Ring(nc, st2, "Dg", [128, 128], BF16, 3)
                    smr = Ring(nc, st2, "sm", [128, 8], F32, 4)
                    main = [("ctx", xcsrc(), LC, 1, None)] + [("lat", (lambda kt, c2=c2: (X[:, kt, c2 * MC:(c2 + 1) * MC], XB[kt][c2 * MC // CH])), MC, 0, c2)
                                                          for c2 in range(T // MC)]
                    for kind, src, n, w, c in ([] if FAST else main):
                        norm_chunk(nr, src, n, 0, w, (lambda kt, w=w: modE[:, w, kt:kt + 1]), (lambda kt: (hT[:, kt, :n], hTB)))
                        for m in range(4):
                            pu, pub = bank()
                            mm(pu[:, :n], [pub], [(wmain[:, kt, m * 128:(m + 1) * 128], hT[:, kt, :n]) for kt in range(KT)], [wmB, hTB])
                            act(lambda e, pu=pu, m=m, n=n: e.activation(out=gu[:, m, :n], in_=pu[:, :n], func=AF.Gelu_apprx_tanh), [pub], [guB])
                        for j in range(4):
                            pq, pqb = bank()
                            mm(pq[:, :n], [pqb], [(wmain[:, kt, 1024 + j * 128:1024 + (j + 1) * 128], hT[:, kt, :n]) for kt in range(KT)], [wmB, hTB])
                            if kind == "ctx":
                                act(lambda e, pq=pq, j=j, n=n: e.activation(out=qT[:, j, :n], in_=pq[:, :n], func=AF.Copy), [pqb], [qTB])
                            else:
                                pqs, pqsb = bank()
                                mm(pqs[:, :n], [pqsb], [(wmain[:, kt, 1536 + j * 128:1536 + (j + 1) * 128], hT[:, kt, :n]) for kt in range(KT)], [wmB, hTB])
                                e0 = 128 + c * MC
                                t1, t1b = rt.next()
                                t2, t2b = rt.next()
                                dve(lambda e, t1=t1, pq=pq, e0=e0, n=n: e.tensor_tensor(out=t1[:, :n], in0=pq[:, :n], in1=ropeC[:, e0:e0 + n], op=ALU.mult),
                                    [pqb, CB], [t1b])
                                dve(lambda e, t2=t2, pqs=pqs, e0=e0, n=n: e.tensor_tensor(out=t2[:, :n], in0=pqs[:, :n], in1=ropeS[:, e0:e0 + n], op=ALU.mult),
                                    [pqsb, CB], [t2b])
                                pool(lambda e, t1=t1, t2=t2, j=j, n=n: e.tensor_tensor(out=qT[:, j, :n], in0=t1[:, :n], in1=t2[:, :n], op=ALU.add),
                                     [t1b, t2b], [qTB])
                        for tt in range(n // 128):
                            tsl = slice(tt * 128, (tt + 1) * 128)
                            pv, pvb = bank()
                            mm(pv[:, :], [pvb], [(hT[:, kt, tsl], wmain[:, kt, 512:1024]) for kt in range(KT)], [wmB, hTB])
                            gv, gvb = gvr.next()
                            act(lambda e, gv=gv, pv=pv: e.activation(out=gv[:], in_=pv[:], func=AF.Gelu_apprx_tanh), [pvb], [gvb])
                            stt, stb = str_.next()
                            mv, mvb = mvr.next()
                            for g in range(4):
                                dve(lambda e, stt=stt, gv=gv, g=g: e.bn_stats(out=stt[:, g, :], in_=gv[:, g * 128:(g + 1) * 128]), [gvb], [stb])
                                dve(lambda e, stt=stt, mv=mv, g=g: e.bn_aggr(out=mv[:, g, :], in_=stt[:, g, :]), [stb], [mvb])
                            sd, sdb = sdr.next()
                            act(lambda e, sd=sd, mv=mv: e.activation(out=sd[:], in_=mv[:, :, 1], func=AF.Sqrt, bias=EPS, scale=1.0), [mvb], [sdb])
                            dve(lambda e, sd=sd: e.reciprocal(out=sd[:], in_=sd[:]), [sdb], [sdb])
                            vn, vnb = vnr.next()
                            for g in range(4):
                                dve(lambda e, vn=vn, gv=gv, mv=mv, sd=sd, g=g: e.tensor_scalar(
                                    out=vn[:, g * 128:(g + 1) * 128], in0=gv[:, g * 128:(g + 1) * 128], scalar1=mv[:, g, 0:1],
                                    scalar2=sd[:, g:g + 1], op0=ALU.subtract, op1=ALU.mult), [gvb, mvb, sdb], [vnb])
                            pm, pmb = bank()

                            def mixmm(e, pm=pm, vn=vn):
                                ins = None
                                for g in range(4):
                                    ins = e.matmul(pm[:, g * 128:(g + 1) * 128], lhsT=vn[:, g * 128:(g + 1) * 128], rhs=wsT[:, g, :], start=True, stop=True)
                                return ins
                            P.op("pe", mixmm, [vnb, CB], [pmb])
                            t1, t1b = rt.next()
                            dve(lambda e, t1=t1, pm=pm: e.tensor_tensor(out=t1[:], in0=pm[:], in1=bsb[:], op=ALU.add), [pmb, CB], [t1b])
                            pool(lambda e, t1=t1, tsl=tsl: e.tensor_tensor(out=mix[:, 0:4, tsl], in0=t1[:].rearrange("p (g t) -> p g t", g=4),
                                                                            in1=gu[:, 0:4, tsl], op=ALU.mult), [t1b, guB], [mixA])
                        def att_A(qb, j, hh, po, pob):
                            qsl = slice(qb * 128, (qb + 1) * 128)
                            h = j + 4 * hh
                            psl = slice(hh * 64, (hh + 1) * 64)
                            S, Sb = bank2()
                            if kind == "ctx":
                                nk = 256
                                P.op("pe", (lambda e, S=S, j=j, psl=psl, qsl=qsl: e.matmul(S[:, 0:256], lhsT=qT[psl, j, qsl], rhs=kcT[psl, :], start=True, stop=True)),
                                     [qTB, kcB], Sb)
                                vts = [(Vc, 0, VcB), (Vc, 1, VcB)]
                            else:
                                nk = 640
                                E0 = c * MC + qb * 128
                                gb = c * (MC // 128) + qb
                                mi = 0 if (c == 0 and qb == 0) else (2 if (c == T // MC - 1 and qb == MC // 128 - 1) else 1)

                                def qk(e, S=S, j=j, psl=psl, qsl=qsl, E0=E0, mi=mi):
                                    e.matmul(S[:, 0:384], lhsT=qT[psl, j, qsl], rhs=kTe[psl, E0:E0 + 384], start=True, stop=False)
                                    e.matmul(S[:, 0:384], lhsT=identb[:], rhs=maskb[:, mi, :], start=False, stop=True)
                                    e.matmul(S[:, 384:512], lhsT=qT[psl, j, qsl], rhs=kcT[psl, 0:128], start=True, stop=True)
                                    return e.matmul(S[:, 512:640], lhsT=qT[psl, j, qsl], rhs=kcT[psl, 128:256], start=True, stop=True)
                                P.op("pe", qk, [qTB, kcB, CB] + kTeB[gb:gb + 3], Sb)
                                vts = [(Ve, gb, VeB[gb]), (Ve, gb + 1, VeB[gb + 1]), (Ve, gb + 2, VeB[gb + 2]), (Vc, 0, VcB), (Vc, 1, VcB)]
                            sm, smb = smr.next()
                            dve(lambda e, sm=sm, S=S, nk=nk: e.reduce_max(out=sm[:, 0:1], in_=S[:, 0:nk], axis=AX.X), Sb, [smb])
                            dve(lambda e, sm=sm, h=h: e.tensor_scalar(out=sm[:, 1:2], in0=sm[:, 0:1], scalar1=SCALE, scalar2=sinkb[:, h:h + 1],
                                                                      op0=ALU.mult, op1=ALU.max), [smb, CB], [smb])
                            dve(lambda e, sm=sm: e.tensor_scalar(out=sm[:, 2:3], in0=sm[:, 1:2], scalar1=-1.0, scalar2=None, op0=ALU.mult), [smb], [smb])
                            dve(lambda e, sm=sm: e.memset(sm[:, 3:4], 0.0), [smb], [smb])
                            pt, ptb = ptr.next()
                            act(lambda e, pt=pt, S=S, sm=sm, nk=nk: e.activation(out=pt[:, 0:nk], in_=S[:, 0:nk], func=AF.Exp, scale=SCALE,
                                                                               bias=sm[:, 2:3], accum_out=sm[:, 3:4]), Sb + [smb], [ptb, smb])
                            act(lambda e, sm=sm, h=h: e.activation(out=sm[:, 4:5], in_=sinkb[:, h:h + 1], func=AF.Exp, scale=1.0, bias=sm[:, 2:3]),
                                [smb, CB], [smb])
                            dve(lambda e, sm=sm: e.tensor_tensor(out=sm[:, 5:6], in0=sm[:, 3:4], in1=sm[:, 4:5], op=ALU.add), [smb], [smb])
                            dve(lambda e, sm=sm: e.reciprocal(out=sm[:, 6:7], in_=sm[:, 5:6]), [smb], [smb])
                            dg, dgb = dgr.next()
                            dve(lambda e, dg=dg, sm=sm: e.tensor_scalar(out=dg[:], in0=identb[:], scalar1=sm[:, 6:7], scalar2=None, op0=ALU.mult),
                                [smb, CB], [dgb])
                            return dict(j=j, hh=hh, po=po, pob=pob, pt=pt, ptb=ptb, dg=dg, dgb=dgb, nk=nk, vts=vts, psl=psl, qsl=qsl)

                        def att_C(u):
                            j, hh, po, pob, pt, ptb, dg, dgb, nk, vts, psl, qsl = (u[k_] for k_ in ("j", "hh", "po", "pob", "pt", "ptb", "dg", "dgb", "nk", "vts", "psl", "qsl"))
                            PT, PTb = bank2()
                            nkt = nk // 128

                            def tr(e, PT=PT, pt=pt, dg=dg, nkt=nkt):
                                ins = None
                                for k_ in range(nkt):
                                    ins = e.matmul(PT[:, k_ * 128:(k_ + 1) * 128], lhsT=pt[:, k_ * 128:(k_ + 1) * 128], rhs=dg[:], start=True, stop=True)
                                return ins
                            P.op("pe", tr, [ptb, dgb], PTb)
                            pts, ptsb = ptsr.next()
                            act(lambda e, pts=pts, PT=PT, nk=nk: e.activation(out=pts[:, 0:nk], in_=PT[:, 0:nk], func=AF.Copy), PTb, [ptsb])

                            def pv_(e, po=po, pts=pts, vts=vts, psl=psl, hh=hh):
                                ins = None
                                for k_, (vt, vi, _) in enumerate(vts):
                                    ins = e.matmul(po[psl, 0:128], lhsT=vt[:, vi, hh * 64:(hh + 1) * 64], rhs=pts[:, k_ * 128:(k_ + 1) * 128],
                                                   start=(k_ == 0), stop=(k_ == len(vts) - 1))
                                return ins
                            P.op("pe", pv_, [ptsb] + [v[2] for v in vts], [pob], skip_self=(hh == 1))
                            if hh == 1:
                                dve(lambda e, po=po, j=j, qsl=qsl: e.tensor_copy(out=mix[:, 4 + j, qsl], in_=po[:, 0:128]), [pob], [mixBq[j]])
                        pend = None
                        for qb in range(n // 128):
                            for j in range(4):
                                po, pob = lbank()
                                for hh in range(2):
                                    cur = att_A(qb, j, hh, po, pob)
                                    if pend is not None:
                                        att_C(pend)
                                    pend = cur
                        att_C(pend)
                        for f in range(KT):
                            py, pyb = bank()
                            mm(py[:, :n], [pyb], [(wout[:, mt, f * 128:(f + 1) * 128], mix[:, mt, :n]) for mt in range(KT)], [woB, mixA] + mixBq)
                            if kind == "ctx":
                                xa, xb_ = Xc[:, f, :], XcB[f]
                            else:
                                xa, xb_ = X[:, f, c * MC:(c + 1) * MC], XB[f][c * MC // CH]
                            dve(lambda e, xa=xa, py=py, n=n, g=modE[:, w, 16 + f:16 + f + 1]: e.scalar_tensor_tensor(
                                out=xa, in0=py[:, :n], scalar=g, in1=xa, op0=ALU.mult, op1=ALU.add), [pyb, xb_, CB], [xb_])
                    P.barrier()
                P.barrier()
            with ExitStack() as st:
                nr = norm_rings(st)
                h2T = sb("h2T", [128, KT, T], BF16, st)
                h2B = [Buf("h2_%d" % c) for c in range(NCH)]
                h2c = sb("h2c", [128, KT, LC], BF16, st)
                h2cB = Buf("h2c")
                for c in range(NCH):
                    norm_chunk(nr, xsrc(c), CH, 1, 0, (lambda kt: modE[:, 0, 24 + kt:24 + kt + 1]),
                               (lambda kt, c=c: (h2T[:, kt, c * CH:(c + 1) * CH], h2B[c])))
                norm_chunk(nr, xcsrc(), LC, 1, 1, (lambda kt: modE[:, 1, 24 + kt:24 + kt + 1]), (lambda kt: (h2c[:, kt, :], h2cB)))
                chunks = []
                for c in range(NCH):
                    chunks.append(((lambda kt, c=c: h2T[:, kt, c * CH:(c + 1) * CH]), h2B[c], CH,
                                   (lambda f, c=c: (X[:, f, c * CH:(c + 1) * CH], XB[f][c])),
                                   (lambda f: modE[:, 0, 40 + f:40 + f + 1]), c * CH))
                chunks.append(((lambda kt: h2c[:, kt, :]), h2cB, LC, (lambda f: (Xc[:, f, :], XcB[f])),
                               (lambda f: modE[:, 1, 40 + f:40 + f + 1]), 0))
                if not FAST:
                    ffn_pass(ffn_rings(st), I["ffn_w1"].rearrange("(kt p) n -> p kt n", p=128), I["ffn_w3"].rearrange("(kt p) n -> p kt n", p=128),
                             I["ffn_w2"].rearrange("(ft p) n -> p ft n", p=128), chunks)
                P.barrier()
        else:
            for kt in range(KT):
                for c in range(NCH):
                    P.dma("sp", X[:, kt, c * CH:(c + 1) * CH], I["x1T_in"][:, kt, c * CH:(c + 1) * CH], writes=[XB[kt][c]])
                if P2:
                    P.dma("sp", Xc[:, kt, :], I["xc1T_in"][:, kt, :], writes=[XcB[kt]])

        wio = I["w_in_o"].rearrange("(kt p) n -> p kt n", p=128)
        wio2 = WS["w_in_o"].rearrange("(kt p) n -> p kt n", p=128)
        if P1:
            with ExitStack() as st:
                nr = norm_rings(st)
                xb3 = sb("xb3", [128, KT, 4], F32, st)
                xb3B = Buf("xb3")
                hb3 = sb("hb3", [128, KT, 4], BF16, st)
                hb3B = Buf("hb3")
                brec = sb("brec", [128, NB, 3], F32, st)
                brB = Buf("brec")
                for kt in range(KT):
                    dve(lambda e, kt=kt: e.tensor_copy(out=xb3[:, kt, 0:1], in_=X[:, kt, 0:1]), [XB[kt][0]], [xb3B])
                    dve(lambda e, kt=kt: e.tensor_copy(out=xb3[:, kt, 1:3], in_=X[:, kt, T - 2:T]), [XB[kt][3]], [xb3B])
                norm_chunk(nr, (lambda kt: (xb3[:, kt, 0:3], xb3B)), 3, 2, 0, (lambda kt: modO[:, 0, kt:kt + 1]), (lambda kt: (hb3[:, kt, 0:3], hb3B)))
                wr = Ring(nc, st, "wrec", [128, KT, 256], BF16, 2)
                ps, pb = lbank()
                for piece in range(5):
                    wt, wb = wr.next()
                    P.dma("pool", wt[:], wio[:, :, DR + piece * 256:DR + (piece + 1) * 256], writes=[wb])
                    for m in range(2):
                        blk = piece * 2 + m
                        mm(ps[:, blk * 3:blk * 3 + 3], [pb], [(wt[:, kt, m * 128:(m + 1) * 128], hb3[:, kt, 0:3]) for kt in range(KT)], [wb, hb3B], skip_self=True)
                dve(lambda e: e.tensor_copy(out=brec[:].rearrange("p a b -> p (a b)"), in_=ps[:, 0:30]), [pb], [brB])
                if not FUSED:
                    P.dma("sp", O["brec"], brec[:], reads=[brB])
                    for kt in range(KT):
                        P.dma("sp", O["x1T"][:, kt, :], X[:, kt, :], reads=XB[kt])
                        P.dma("sp", O["xc1T"][:, kt, :], Xc[:, kt, :], reads=[XcB[kt]])
                else:
                    c1i, c1o = Buf("cc1i"), Buf("cc1o")
                    P.dma("sp", selm[:], I["selm"], writes=[CB])
                    P.dma("sp", cc1_in, brec[:].rearrange("p a b -> p (a b)"), reads=[brB], writes=[c1i])
                    P.cc((lambda e: e.collective_compute("AllGather", ALU.bypass, replica_groups=[list(range(8))],
                                                         ins=[cc1_in.opt()], outs=[cc1_out.opt()])), reads=[c1i], writes=[c1o])
                    g1 = sb("g1all", [128, 8, NB, 3], F32, st)
                    g1B = Buf("g1all")
                    P.dma("sp", g1[:].rearrange("p r a b -> p r (a b)"), cc1_out.rearrange("(r p) n -> p r n", p=128), reads=[c1o], writes=[g1B])
                    dve(lambda e: e.memset(halo[:], 0.0), [], [haloB])
                    for r in range(8):
                        dve(lambda e, r=r: e.scalar_tensor_tensor(out=halo[:, :, 0:2], in0=g1[:, r, :, 1:3], scalar=selm[:, r:r + 1], in1=halo[:, :, 0:2],
                                                                  op0=ALU.mult, op1=ALU.add), [g1B, haloB, CB], [haloB])
                        dve(lambda e, r=r: e.scalar_tensor_tensor(out=halo[:, :, 2:3], in0=g1[:, r, :, 0:1], scalar=selm[:, 8 + r:9 + r], in1=halo[:, :, 2:3],
                                                                  op0=ALU.mult, op1=ALU.add), [g1B, haloB, CB], [haloB])
                    if dbg:
                        P.dma("sp", O["dbg"][:, 400:430], brec[:].rearrange("p a b -> p (a b)"), reads=[brB])
                        P.dma("sp", O["dbg"][:, 512:752], g1[:].rearrange("p r a b -> p (r a b)"), reads=[g1B])
                        P.dma("sp", O["dbg"][:, 768:820], selm[:], reads=[CB])
                P.barrier()

        if P2 or P3:
            with ExitStack() as st:
                h1T = sb("h1T", [128, KT, T], BF16, st)
                h1B = [Buf("h1_%d" % c) for c in range(NCH)]
                if P2:
                    h1c = sb("h1c", [128, KT, LC], BF16, st)
                    h1cB = Buf("h1c")
                with ExitStack() as stn:
                    nr = norm_rings(stn)
                    for c in range(NCH):
                        norm_chunk(nr, xsrc(c), CH, 2, 0, (lambda kt: modO[:, 0, kt:kt + 1]), (lambda kt, c=c: (h1T[:, kt, c * CH:(c + 1) * CH], h1B[c])))
                    if P2:
                        norm_chunk(nr, xcsrc(), LC, 2, 1, (lambda kt: modO[:, 1, kt:kt + 1]), (lambda kt: (h1c[:, kt, :], h1cB)))
                    P.barrier()
                cw = sb("cw", [128, NB, 4], F32, st)
                cbv = sb("cbv", [128, NB], F32, st)
                lba = sb("lba", [128, 2, NB], F32, st)
                lbx = sb("lbx", [128, 2, NB], F32, st)
                lam = sb("lam", [128, 2, NB], F32, st)
                sc8 = sb("sc8", [128, 2, NB], F32, st)
                sc16 = sb("sc16", [128, 2, NB], F32, st)
                ltmp = sb("ltmp", [128, 2, NB], F32, st)
                for t_, n_ in [(cw, "convw"), (cbv, "convb"), (lba, "lru_ba"), (lbx, "lru_bx"), (lam, "lru_lam")]:
                    P.dma("sp", t_[:], I[n_], writes=[CB])
                if not P1:
                    P.dma("sp", halo[:], I["halo"], writes=[haloB])
                dve(lambda e: e.tensor_scalar(out=ltmp[:], in0=lam[:], scalar1=-1.0, scalar2=None, op0=ALU.mult), [CB], [CB])
                dve(lambda e: e.tensor_tensor(out=ltmp[:], in0=ltmp[:], in1=lam[:], op=ALU.max), [CB], [CB])
                act(lambda e: e.activation(out=ltmp[:], in_=ltmp[:], func=AF.Exp, scale=-1.0), [CB], [CB])
                act(lambda e: e.activation(out=ltmp[:], in_=ltmp[:], func=AF.Ln, bias=1.0, scale=1.0), [CB], [CB])
                dve(lambda e: e.tensor_scalar(out=sc8[:], in0=lam[:], scalar1=-1.0, scalar2=0.0, op0=ALU.mult, op1=ALU.max), [CB], [CB])
                dve(lambda e: e.tensor_tensor(out=sc8[:], in0=sc8[:], in1=ltmp[:], op=ALU.add), [CB], [CB])
                dve(lambda e: e.tensor_scalar(out=sc16[:], in0=sc8[:], scalar1=-16.0, scalar2=None, op0=ALU.mult), [CB], [CB])
                dve(lambda e: e.tensor_scalar(out=sc8[:], in0=sc8[:], scalar1=-8.0, scalar2=None, op0=ALU.mult), [CB], [CB])
                summ = sb("summ", [128, NB, 6], F32, st)
                summB = Buf("summ")
                rsum = sb("rsum", [128, NB, 2, NCH], F32, st)
                rsB = Buf("rsum")
                hin = sb("hin", [128, NB, 2], F32, st)
                hinB = Buf("hin")
                dve(lambda e: e.memset(hin[:], 0.0), [], [hinB])
                dve(lambda e: e.memset(rsum[:], 0.0), [], [rsB])

                def mixer_pass(final):
                    with ExitStack() as st2:
                        wr = Ring(nc, st2, "wrc", [128, KT, 128], BF16, 2)
                        wg = Ring(nc, st2, "wgt", [128, KT, 128], BF16, 2)
                        lw = Ring(nc, st2, "lw", [128, 4, 128], BF16, 2)
                        wo = Ring(nc, st2, "wo", [128, D], BF16, 2)
                        recp = sb("recp", [128, T + 3], F32, st2)
                        recpB = Buf("recp")
                        rc = sb("rc", [128, T], F32, st2)
                        rcB = Buf("rc")
                        rcb = sb("rcb", [128, T], BF16, st2)
                        rcbB = Buf("rcb")
                        hs = [sb("hs%d" % d_, [128, T], F32, st2) for d_ in range(2)]
                        hsB = [Buf("hs%d" % d_) for d_ in range(2)]
                        tr_ = Ring(nc, st2, "lt", [128, CH], F32, 8)
                        yb = Ring(nc, st2, "yblk", [128, T], BF16, 2)
                        if not final:
                            recpc = sb("recpc", [128, LC + 3], F32, st2)
                            rcc = sb("rcc", [128, LC], F32, st2)
                            rccb = sb("rccb", [128, LC], BF16, st2)
                            hsc = sb("hsc", [128, LC], F32, st2)
                            cB_ = Buf("ctxlru")
                            dve(lambda e: e.memset(recpc[:], 0.0), [], [cB_])
                        for blk in range(NB):
                            wt, wb = wr.next()
                            P.dma(WQ, wt[:], wio2[:, :, DR + blk * 128:DR + (blk + 1) * 128], reads=WRD, writes=[wb])
                            lt_, lb_ = lw.next()
                            for d_ in range(2):
                                P.dma(WQ, lt_[:, d_, :], WS["lru_wa"][d_, blk], reads=WRD, writes=[lb_])
                                P.dma(WQ, lt_[:, 2 + d_, :], WS["lru_wx"][d_, blk], reads=WRD, writes=[lb_])
                            if final:
                                gt_, gb_ = wg.next()
                                P.dma(WQ, gt_[:], wio2[:, :, blk * 128:(blk + 1) * 128], reads=WRD, writes=[gb_])
                                wot, wob = wo.next()
                                P.dma(WQ, wot[:], WS["w_out_o"][blk * 128:(blk + 1) * 128, :], reads=WRD, writes=[wob])
                            dve(lambda e, blk=blk: e.tensor_copy(out=recp[:, 0:2], in_=halo[:, blk, 0:2]), [haloB], [recpB])
                            dve(lambda e, blk=blk: e.tensor_copy(out=recp[:, T + 2:T + 3], in_=halo[:, blk, 2:3]), [haloB], [recpB])
                            for c in range(NCH):
                                pr, prb = bank()
                                mm(pr[:], [prb], [(wt[:, kt, :], h1T[:, kt, c * CH:(c + 1) * CH]) for kt in range(KT)], [wb, h1B[c]])
                                act(lambda e, pr=pr, c=c: e.activation(out=recp[:, 2 + c * CH:2 + (c + 1) * CH], in_=pr[:], func=AF.Copy), [prb], [recpB])
                            dve(lambda e, blk=blk: e.tensor_scalar(out=rc[:], in0=recp[:, 0:T], scalar1=cw[:, blk, 0:1], scalar2=cbv[:, blk:blk + 1],
                                                                    op0=ALU.mult, op1=ALU.add), [recpB, CB], [rcB])
                            for j in range(1, 4):
                                dve(lambda e, blk=blk, j=j: e.scalar_tensor_tensor(out=rc[:], in0=recp[:, j:j + T], scalar=cw[:, blk, j:j + 1], in1=rc[:],
                                                                                     op0=ALU.mult, op1=ALU.add), [recpB, rcB, CB], [rcB])
                            pool(lambda e: e.tensor_copy(out=rcb[:], in_=rc[:]), [rcB], [rcbB])
                            seqs = [("lat", rc, rcb, rcB, rcbB, T, hs)]
                            if not final:
                                pr, prb = bank()
                                mm(pr[:, :LC], [prb], [(wt[:, kt, :], h1c[:, kt, :]) for kt in range(KT)], [wb, h1cB])
                                act(lambda e, pr=pr: e.activation(out=recpc[:, 2:2 + LC], in_=pr[:, :LC], func=AF.Copy), [prb], [cB_])
                                dve(lambda e, blk=blk: e.tensor_scalar(out=rcc[:], in0=recpc[:, 0:LC], scalar1=cw[:, blk, 0:1], scalar2=cbv[:, blk:blk + 1],
                                                                        op0=ALU.mult, op1=ALU.add), [cB_, CB], [cB_])
                                for j in range(1, 4):
                                    dve(lambda e, blk=blk, j=j: e.scalar_tensor_tensor(out=rcc[:], in0=recpc[:, j:j + LC], scalar=cw[:, blk, j:j + 1], in1=rcc[:],
                                                                                         op0=ALU.mult, op1=ALU.add), [cB_, CB], [cB_])
                                pool(lambda e: e.tensor_copy(out=rccb[:], in_=rcc[:]), [cB_], [cB_])
                                seqs = [("ctx", rcc, rccb, cB_, cB_, LC, None)] + seqs
                            for kind, r32, rb16, r32B, rb16B, nt, hs_ in seqs:
                                for d_ in range(2):
                                    nch = (nt + CH - 1) // CH
                                    order = list(range(nch)) if d_ == 0 else list(range(nch - 1, -1, -1))
                                    for oi, c in enumerate(order):
                                        n = min(CH, nt - c * CH)
                                        csl = slice(c * CH, c * CH + n)
                                        pa, pab = bank()
                                        px, pxb = bank()
                                        mm(pa[:, :n], [pab], [(lt_[:, d_, :], rb16[:, csl])], [lb_, rb16B])
                                        mm(px[:, :n], [pxb], [(lt_[:, 2 + d_, :], rb16[:, csl])], [lb_, rb16B])
                                        r_, r_b = tr_.next()
                                        i_, i_b = tr_.next()
                                        a_, a_b = tr_.next()
                                        b_, b_b = tr_.next()
                                        if kind == "lat" and not final:
                                            act(lambda e, r_=r_, pa=pa, n=n, blk=blk, d_=d_, c=c: e.activation(
                                                out=r_[:, :n], in_=pa[:, :n], func=AF.Sigmoid, bias=lba[:, d_, blk:blk + 1], scale=1.0,
                                                accum_out=rsum[:, blk, d_, c:c + 1]), [pab, CB], [r_b, rsB])
                                        else:
                                            act(lambda e, r_=r_, pa=pa, n=n, blk=blk, d_=d_: e.activation(
                                                out=r_[:, :n], in_=pa[:, :n], func=AF.Sigmoid, bias=lba[:, d_, blk:blk + 1], scale=1.0), [pab, CB], [r_b])
                                        act(lambda e, i_=i_, px=px, n=n, blk=blk, d_=d_: e.activation(
                                            out=i_[:, :n], in_=px[:, :n], func=AF.Sigmoid, bias=lbx[:, d_, blk:blk + 1], scale=1.0), [pxb, CB], [i_b])
                                        act(lambda e, a_=a_, r_=r_, n=n, blk=blk, d_=d_: e.activation(
                                            out=a_[:, :n], in_=r_[:, :n], func=AF.Exp, scale=sc8[:, d_, blk:blk + 1]), [r_b, CB], [a_b])
                                        act(lambda e, b_=b_, r_=r_, n=n, blk=blk, d_=d_: e.activation(
                                            out=b_[:, :n], in_=r_[:, :n], func=AF.Exp, scale=sc16[:, d_, blk:blk + 1]), [r_b, CB], [b_b])
                                        act(lambda e, b_=b_, n=n: e.activation(out=b_[:, :n], in_=b_[:, :n], func=AF.Sqrt, scale=-1.0, bias=1.0), [b_b], [b_b])
                                        dve(lambda e, b_=b_, i_=i_, n=n: e.tensor_tensor(out=b_[:, :n], in0=b_[:, :n], in1=i_[:, :n], op=ALU.mult), [b_b, i_b], [b_b])
                                        pool(lambda e, b_=b_, r32=r32, csl=csl, n=n: e.tensor_tensor(out=b_[:, :n], in0=b_[:, :n], in1=r32[:, csl], op=ALU.mult),
                                             [b_b, r32B], [b_b])
                                        if kind == "ctx":
                                            ho, hoB = hsc, cB_
                                            init = 0.0
                                            initB = []
                                        else:
                                            ho, hoB = hs_[d_], hsB[d_]
                                            if oi == 0:
                                                init = hin[:, blk, d_:d_ + 1]
                                                initB = [hinB]
                                            elif d_ == 0:
                                                init = ho[:, c * CH - 1:c * CH]
                                                initB = []
                                            else:
                                                init = ho[:, (c + 1) * CH:(c + 1) * CH + 1]
                                                initB = []
                                        if d_ == 0:
                                            dve(lambda e, ho=ho, a_=a_, b_=b_, csl=csl, n=n, init=init: e.tensor_tensor_scan(
                                                out=ho[:, csl], data0=a_[:, :n], data1=b_[:, :n], initial=init, op0=ALU.mult, op1=ALU.add),
                                                [a_b, b_b, hoB] + initB, [hoB])
                                        else:
                                            lo = c * CH
                                            dve(lambda e, ho=ho, a_=a_, b_=b_, lo=lo, n=n, init=init: e.tensor_tensor_scan(
                                                out=ho[:, lo:lo + n][:, ::-1], data0=a_[:, 0:n][:, ::-1], data1=b_[:, 0:n][:, ::-1], initial=init,
                                                op0=ALU.mult, op1=ALU.add), [a_b, b_b, hoB] + initB, [hoB])
                                    if not final:
                                        if kind == "ctx":
                                            src_ = hsc[:, LC - 1:LC] if d_ == 0 else hsc[:, 0:1]
                                            dve(lambda e, blk=blk, d_=d_, src_=src_: e.tensor_copy(out=summ[:, blk, 4 + d_:5 + d_], in_=src_), [cB_], [summB])
                                        else:
                                            src_ = hs_[0][:, T - 1:T] if d_ == 0 else hs_[1][:, 0:1]
                                            dve(lambda e, blk=blk, d_=d_, src_=src_: e.tensor_copy(out=summ[:, blk, 2 * d_ + 1:2 * d_ + 2], in_=src_), [hsB[d_]], [summB])
                            if final:
                                yt, ytb = yb.next()
                                for c in range(NCH):
                                    csl = slice(c * CH, (c + 1) * CH)
                                    pg, pgb = bank()
                                    mm(pg[:], [pgb], [(gt_[:, kt, :], h1T[:, kt, csl]) for kt in range(KT)], [gb_, h1B[c]])
                                    g_, g_b = tr_.next()
                                    act(lambda e, g_=g_, pg=pg: e.activation(out=g_[:], in_=pg[:], func=AF.Gelu_apprx_tanh), [pgb], [g_b])
                                    s_, s_b = tr_.next()
                                    dve(lambda e, s_=s_, csl=csl: e.tensor_tensor(out=s_[:], in0=hs[0][:, csl], in1=hs[1][:, csl], op=ALU.add), hsB, [s_b])
                                    pool(lambda e, yt=yt, g_=g_, s_=s_, csl=csl: e.tensor_tensor(out=yt[:, csl], in0=g_[:], in1=s_[:], op=ALU.mult), [g_b, s_b], [ytb])
                                for c in range(NCH):
                                    csl = slice(c * CH, (c + 1) * CH)
                                    for f in range(KT):
                                        py, pyb = bank()
                                        mm(py[:], [pyb], [(wot[:, f * 128:(f + 1) * 128], yt[:, csl])], [wob, ytb])
                                        dve(lambda e, py=py, f=f, csl=csl: e.scalar_tensor_tensor(out=X[:, f, csl], in0=py[:], scalar=modO[:, 0, 16 + f:16 + f + 1],
                                                                                                   in1=X[:, f, csl], op0=ALU.mult, op1=ALU.add), [pyb, XB[f][c], CB], [XB[f][c]])
                        if not final:
                            for d_ in range(2):
                                dve(lambda e, d_=d_: e.tensor_reduce(out=summ[:, :, 2 * d_], in_=rsum[:, :, d_, :], axis=AX.X, op=ALU.add), [rsB], [summB])
                                dve(lambda e, d_=d_: e.tensor_tensor(out=summ[:, :, 2 * d_], in0=summ[:, :, 2 * d_], in1=sc8[:, d_, :], op=ALU.mult), [summB, CB], [summB])
                                act(lambda e, d_=d_: e.activation(out=summ[:, :, 2 * d_], in_=summ[:, :, 2 * d_], func=AF.Exp), [summB], [summB])
                        P.barrier()

                if P2:
                    mixer_pass(False)
                    if last == 2:
                        P.dma("sp", O["summ"], summ[:], reads=[summB])
                        P.barrier()
                if P3:
                    selt = sb("selt", [128, 4], F32, st)
                    cfw = sb("cfw", [128, 4, NB], F32, st)
                    cbw = sb("cbw", [128, 4, NB], F32, st)
                    P.dma("sp", selt[:], I["sel"], writes=[CB])
                    if not FUSED:
                        P.dma("sp", sa[:], I["summ_all"], writes=[CB])
                    else:
                        c2i, c2o = Buf("cc2i"), Buf("cc2o")
                        P.dma("sp", cc2_in, summ[:].rearrange("p a b -> p (a b)"), reads=[summB], writes=[c2i])
                        P.cc((lambda e: e.collective_compute("AllGather", ALU.bypass, replica_groups=[list(range(8))],
                                                             ins=[cc2_in.opt()], outs=[cc2_out.opt()])), reads=[c2i], writes=[c2o])
                        g2a = sb("g2all", [128, 8, NB * 6], F32, st)
                        g2B = Buf("g2all")
                        P.dma("sp", g2a[:], cc2_out.rearrange("(r p) n -> p r n", p=128), reads=[c2o], writes=[g2B])
                        dve(lambda e: e.memset(sa[:], 0.0), [], [CB])
                        for j in range(4):
                            for r in range(8):
                                dve(lambda e, j=j, r=r: e.scalar_tensor_tensor(
                                    out=sa[:, j].rearrange("p a b -> p (a b)"), in0=g2a[:, r, :], scalar=selm[:, 16 + j * 8 + r:17 + j * 8 + r],
                                    in1=sa[:, j].rearrange("p a b -> p (a b)"), op0=ALU.mult, op1=ALU.add), [g2B, CB], [CB])
                    dve(lambda e: e.tensor_copy(out=cfw[:, 0, :], in_=sa[:, 0, :, 4]), [CB], [CB])
                    for j in range(3):
                        dve(lambda e, j=j: e.tensor_tensor(out=cfw[:, j + 1, :], in0=sa[:, j, :, 0], in1=cfw[:, j, :], op=ALU.mult), [CB], [CB])
                        dve(lambda e, j=j: e.tensor_tensor(out=cfw[:, j + 1, :], in0=cfw[:, j + 1, :], in1=sa[:, j, :, 1], op=ALU.add), [CB], [CB])
                    dve(lambda e: e.tensor_copy(out=cbw[:, 3, :], in_=sa[:, 0, :, 5]), [CB], [CB])
                    for j in range(3, 0, -1):
                        dve(lambda e, j=j: e.tensor_tensor(out=cbw[:, j - 1, :], in0=sa[:, j, :, 2], in1=cbw[:, j, :], op=ALU.mult), [CB], [CB])
                        dve(lambda e, j=j: e.tensor_tensor(out=cbw[:, j - 1, :], in0=cbw[:, j - 1, :], in1=sa[:, j, :, 3], op=ALU.add), [CB], [CB])
                    for d_, cc in enumerate([cfw, cbw]):
                        dve(lambda e, d_=d_, cc=cc: e.tensor_scalar(out=hin[:, :, d_], in0=cc[:, 0, :], scalar1=selt[:, 0:1], scalar2=None, op0=ALU.mult), [CB, hinB], [hinB])
                        for j in range(1, 4):
                            dve(lambda e, d_=d_, cc=cc, j=j: e.scalar_tensor_tensor(out=hin[:, :, d_], in0=cc[:, j, :], scalar=selt[:, j:j + 1], in1=hin[:, :, d_],
                                                                                     op0=ALU.mult, op1=ALU.add), [CB, hinB], [hinB])
                    if dbg:
                        P.dma("sp", O["dbg"][:, 0:30], halo[:].rearrange("p a b -> p (a b)"), reads=[haloB])
                        P.dma("sp", O["dbg"][:, 32:32 + 240], sa[:].rearrange("p j a b -> p (j a b)"), reads=[CB])
                        P.dma("sp", O["dbg"][:, 272:292], hin[:].rearrange("p a b -> p (a b)"), reads=[hinB])
                        P.dma("sp", O["dbg"][:, 292:352], summ[:].rearrange("p a b -> p (a b)"), reads=[summB])
                    mixer_pass(True)
                P.barrier()

        if P3:
            with ExitStack() as st:
                h2T = sb("h2Tm", [128, KT, T], BF16, st)
                h2B = [Buf("h2m_%d" % c) for c in range(NCH)]
                gates = sb("gates", [128, T // 128, 8], F32, st)
                gatesB = Buf("gates")
                with ExitStack() as st2:
                    nr = norm_rings(st2)
                    h32 = sb("h32", [128, KT, CH], F32, st2)
                    h32B = Buf("h32")
                    rw = sb("rw", [128, KT, 8], F32, st2)
                    P.dma("sp", rw[:], I["router_w"], writes=[CB])
                    gr = Ring(nc, st2, "gr", [128, 40], F32, 2)
                    for c in range(NCH):
                        norm_chunk(nr, xsrc(c), CH, 3, 0, (lambda kt: modO[:, 0, 24 + kt:24 + kt + 1]),
                                   (lambda kt, c=c: (h2T[:, kt, c * CH:(c + 1) * CH], h2B[c])), dst32=(lambda kt: (h32[:, kt, :], h32B)))
                        for tt in range(4):
                            ti = c * 4 + tt
                            pl, plb = bank()
                            mm(pl[:, 0:8], [plb], [(h32[:, kt, tt * 128:(tt + 1) * 128], rw[:, kt, :]) for kt in range(KT)], [h32B, CB])
                            g_, g_b = gr.next()
                            dve(lambda e, g_=g_, pl=pl: e.tensor_copy(out=g_[:, 0:8], in_=pl[:, 0:8]), [plb], [g_b])
                            dve(lambda e, g_=g_: e.reduce_max(out=g_[:, 8:9], in_=g_[:, 0:8], axis=AX.X), [g_b], [g_b])
                            dve(lambda e, g_=g_: e.tensor_scalar(out=g_[:, 16:24], in0=g_[:, 0:8], scalar1=g_[:, 8:9], scalar2=NEG, op0=ALU.is_equal, op1=ALU.mult), [g_b], [g_b])
                            dve(lambda e, g_=g_: e.tensor_tensor(out=g_[:, 24:32], in0=g_[:, 16:24], in1=g_[:, 0:8], op=ALU.add), [g_b], [g_b])
                            dve(lambda e, g_=g_: e.reduce_max(out=g_[:, 9:10], in_=g_[:, 24:32], axis=AX.X), [g_b], [g_b])
                            dve(lambda e, g_=g_: e.tensor_scalar(out=g_[:, 10:11], in0=g_[:, 8:9], scalar1=-1.0, scalar2=None, op0=ALU.mult), [g_b], [g_b])
                            act(lambda e, g_=g_: e.activation(out=g_[:, 16:24], in_=g_[:, 0:8], func=AF.Exp, bias=g_[:, 10:11], scale=1.0), [g_b], [g_b])
                            dve(lambda e, g_=g_: e.tensor_scalar(out=g_[:, 24:32], in0=g_[:, 0:8], scalar1=g_[:, 9:10], scalar2=None, op0=ALU.is_ge), [g_b], [g_b])
                            dve(lambda e, g_=g_: e.tensor_tensor(out=g_[:, 16:24], in0=g_[:, 16:24], in1=g_[:, 24:32], op=ALU.mult), [g_b], [g_b])
                            dve(lambda e, g_=g_: e.reduce_sum(out=g_[:, 11:12], in_=g_[:, 16:24], axis=AX.X), [g_b], [g_b])
                            dve(lambda e, g_=g_: e.reciprocal(out=g_[:, 11:12], in_=g_[:, 11:12]), [g_b], [g_b])
                            dve(lambda e, g_=g_, ti=ti: e.tensor_scalar(out=gates[:, ti, :], in0=g_[:, 16:24], scalar1=g_[:, 11:12], scalar2=None, op0=ALU.mult), [g_b], [gatesB])
                    P.barrier()
                fr = ffn_rings(st)
                gbr = Ring(nc, st, "gbc", [128, T], F32, 2)
                ger = Ring(nc, st, "Ge", [128, 128], F32, 3)
                for ex in range(0 if FAST else 8):
                    gt, gtb = gbr.next()
                    for c in range(NCH):
                        pgb_, pgbb = lbank()
                        for tt in range(4):
                            ti = c * 4 + tt
                            ge, geb = ger.next()
                            dve(lambda e, ge=ge, ti=ti, ex=ex: e.tensor_scalar(out=ge[:], in0=ones32[:], scalar1=gates[:, ti, ex:ex + 1], scalar2=None, op0=ALU.mult),
                                [gatesB, CB], [geb])
                            mm(pgb_[:, tt * 128:(tt + 1) * 128], [pgbb], [(ge[:], ident32[:])], [geb, CB], skip_self=True)
                        act(lambda e, gt=gt, pgb_=pgb_, c=c: e.activation(out=gt[:, c * CH:(c + 1) * CH], in_=pgb_[:], func=AF.Copy), [pgbb], [gtb])
                    chunks = []
                    for c in range(NCH):
                        chunks.append(((lambda kt, c=c: h2T[:, kt, c * CH:(c + 1) * CH]), h2B[c], CH,
                                       (lambda f, c=c: (X[:, f, c * CH:(c + 1) * CH], XB[f][c])),
                                       (lambda f: modO[:, 0, 40 + f:40 + f + 1]), c * CH))
                    ffn_pass(fr, WS["moe_w1"][ex].rearrange("(kt p) n -> p kt n", p=128), WS["moe_w3"][ex].rearrange("(kt p) n -> p kt n", p=128),
                             WS["moe_w2"][ex].rearrange("(ft p) n -> p ft n", p=128), chunks, gbc=(gt, gtb), wq=WQ, wrd=WRD)
                P.barrier()
            with ExitStack() as st:
                nr = norm_rings(st)
                orr = Ring(nc, st, "ot", [128, CH], F32, 4)
                for c in range(NCH):
                    sqr, rsr, tmr = nr
                    ps, pb = lbank()
                    for kt in range(KT):
                        sq, sqb = sqr.next()
                        act(lambda e, sq=sq, kt=kt, c=c: e.activation(out=sq[:], in_=X[:, kt, c * CH:(c + 1) * CH], func=AF.Square), [XB[kt][c]], [sqb])
                        P.op("pe", (lambda e, ps=ps, sq=sq, kt=kt: e.matmul(ps[:], lhsT=onesb[:], rhs=sq[:], start=(kt == 0), stop=(kt == KT - 1))),
                             [sqb, CB], [pb], skip_self=(kt > 0))
                    rs, rsb = rsr.next()
                    act(lambda e, rs=rs, ps=ps: e.activation(out=rs[:], in_=ps[:], func=AF.Sqrt, scale=1.0 / D, bias=EPS), [pb], [rsb])
                    dve(lambda e, rs=rs: e.reciprocal(out=rs[:], in_=rs[:]), [rsb], [rsb])
                    for kt in range(KT):
                        ot, otb = orr.next()
                        dve(lambda e, ot=ot, kt=kt, c=c, rs=rs: e.scalar_tensor_tensor(out=ot[:], in0=X[:, kt, c * CH:(c + 1) * CH], scalar=vecs[:, 4, kt:kt + 1],
                                                                                       in1=rs[:], op0=ALU.mult, op1=ALU.mult), [XB[kt][c], rsb, CB], [otb])
                        P.dma("sp", O["outT"][:, kt, c * CH:(c + 1) * CH], ot[:], reads=[otb])
                P.barrier()
        P.barrier(final=True)
        P.emit(block)
    return nc


def _fm(v):
    v = np.asarray(v, np.float32)
    return np.ascontiguousarray(v.reshape(-1, 128).T)


def _tokT(a):
    a = np.asarray(a, np.float32)
    return np.ascontiguousarray(a.T.reshape(KT, 128, a.shape[0]).transpose(1, 0, 2))


def _rope_tables(t0):
    pos = np.arange(t0 - 128, t0 - 128 + TE)
    row = (pos // 64).astype(np.float32)
    col = (pos % 64).astype(np.float32)
    freqs = (np.float32(10000.0) ** (-np.arange(16, dtype=np.float32) / np.float32(16))).astype(np.float32)
    out = np.zeros((128, 2, TE), np.float32)
    for p in range(128):
        d = p % 64
        axis, half, f = d // 32, (d % 32) // 16, d % 16
        ang = ((row if axis == 0 else col) * freqs[f]).astype(np.float32)
        out[p, 0] = np.cos(ang)
        out[p, 1] = np.sin(ang) * (-1.0 if half == 0 else 1.0)
    return out


def _masks(s):
    q = np.arange(128)[:, None]
    koff = np.arange(384)[None, :] - 128
    band = np.abs(koff - q) <= 128
    m = np.zeros((128, 3, 384), np.float32)
    for i in range(3):
        v = band.copy()
        if i == 0 and s == 0:
            v &= (koff >= 0)
        if i == 2 and s == 3:
            v &= (koff < 128)
        m[:, i, :] = np.where(v, 0.0, NEG)
    return m


def _swap_idx():
    d = np.arange(64)
    axis, half, f = d // 32, (d % 32) // 16, d % 16
    return axis * 32 + (1 - half) * 16 + f


_CACHE = {}
_LAST = {}


DBG = False


def _prog(phases):
    key = tuple(phases)
    if key not in _CACHE:
        _CACHE[key] = build(list(phases), dbg=DBG)
    return _CACHE[key]


def _common(inp, b):
    f32 = lambda a: np.ascontiguousarray(np.asarray(a, np.float32))
    ccol = np.stack([inp["c"][b], inp["c_ctx"]], 0)
    ccol = np.ascontiguousarray(ccol.reshape(2, KT, 128).transpose(2, 1, 0))
    vecs = np.stack([_fm(inp["norm1_e"][0]), _fm(inp["norm2_e"][0]), _fm(inp["norm1_o"][0]), _fm(inp["norm2_o"][0]), _fm(inp["final_norm"])], 1)
    return {"ccol": f32(ccol), "vecs": f32(vecs), "ident": np.eye(128, dtype=np.float32),
            "ada_w_o": f32(inp["ada_w_o"][0]), "ada_b_o": _fm(inp["ada_b_o"][0]), "w_in_o": f32(inp["w_in_o"][0])}


def _lru_common(inp):
    f32 = lambda a: np.ascontiguousarray(np.asarray(a, np.float32))
    cw = np.asarray(inp["conv_w"][0], np.float32)
    convw = np.ascontiguousarray(cw.reshape(4, NB, 128).transpose(2, 1, 0))
    def d2(a):
        return np.ascontiguousarray(np.asarray(a, np.float32).reshape(2, NB, 128).transpose(2, 0, 1))
    return {"convw": convw, "convb": _fm(inp["conv_b"][0]), "lru_wa": f32(inp["lru_wa"][0]), "lru_wx": f32(inp["lru_wx"][0]),
            "lru_ba": d2(inp["lru_ba"][0]), "lru_bx": d2(inp["lru_bx"][0]), "lru_lam": d2(inp["lru_lambda"][0])}


def _maps1(inp):
    f32 = lambda a: np.ascontiguousarray(np.asarray(a, np.float32))
    x = np.asarray(inp["x"], np.float32)
    S = x.shape[1]
    w_in = np.asarray(inp["w_in_e"][0], np.float32)
    u, v, q, k, val = w_in[:, 0:512], w_in[:, 512:1024], w_in[:, 1024:1536], w_in[:, 1536:1664], w_in[:, 1664:1792]
    sw = _swap_idx()
    qp = np.concatenate([np.concatenate([q[:, j * 64:(j + 1) * 64], q[:, (j + 4) * 64:(j + 5) * 64]], 1) for j in range(4)], 1)
    qps = np.concatenate([np.concatenate([q[:, j * 64:(j + 1) * 64][:, sw], q[:, (j + 4) * 64:(j + 5) * 64][:, sw]], 1) for j in range(4)], 1)
    ks = np.concatenate([k[:, 0:64][:, sw], k[:, 64:128][:, sw]], 1)
    w_in_ext = f32(np.concatenate([u, v, qp, qps, k, ks, val], 1))
    wo = np.asarray(inp["w_out_e"][0], np.float32)
    rows = list(range(512))
    for j in range(4):
        rows += list(range(512 + j * 64, 512 + (j + 1) * 64)) + list(range(512 + (j + 4) * 64, 512 + (j + 5) * 64))
    w_out_p = f32(wo[rows, :])
    wsT = f32(np.asarray(inp["sgu_w"][0], np.float32).transpose(2, 0, 1))
    bsb = f32(np.broadcast_to(np.asarray(inp["sgu_b"][0], np.float32).reshape(1, 512), (128, 512)))
    sinkb = f32(np.broadcast_to(np.asarray(inp["attn_sink"][0], np.float32).reshape(1, 8), (128, 8)))
    shared = {"ada_w_e": f32(inp["ada_w_e"][0]), "ada_b_e": _fm(inp["ada_b_e"][0]), "w_in_e": w_in_ext, "wsT": wsT, "bsb": bsb,
              "sinkb": sinkb, "w_out_e": w_out_p, "ffn_w1": f32(inp["ffn_w1"][0]), "ffn_w3": f32(inp["ffn_w3"][0]), "ffn_w2": f32(inp["ffn_w2"][0])}
    maps = []
    for c in range(8):
        b, s = c // 4, c % 4
        t0 = s * T
        xh = np.zeros((256, D), np.float32)
        if s > 0:
            xh[0:128] = x[b, t0 - 128:t0]
        if t0 + T < S:
            xh[128:256] = x[b, t0 + T:t0 + T + 128]
        m = dict(shared)
        m.update(_common(inp, b))
        m.update({"xT": _tokT(x[b, t0:t0 + T]), "xhT": _tokT(xh), "ctxT": _tokT(inp["ctx"][b]), "ropeCS": _rope_tables(t0), "masks": _masks(s)})
        maps.append(m)
    return maps


def kernel_unfused(**inp):
    inp = {k: np.asarray(v) for k, v in inp.items()}
    cores = list(range(8))
    r1 = run_bass_kernel_spmd(_prog([1]), _maps1(inp), core_ids=cores).results
    lru = _lru_common(inp)
    halos = []
    for c in range(8):
        b, s = c // 4, c % 4
        h = np.zeros((128, NB, 3), np.float32)
        if s > 0:
            h[:, :, 0:2] = r1[c - 1]["brec"][:, :, 1:3]
        if s < 3:
            h[:, :, 2] = r1[c + 1]["brec"][:, :, 0]
        halos.append(h)
    maps2 = []
    for c in range(8):
        m = dict(lru)
        m.update(_common(inp, c // 4))
        m.update({"x1T_in": r1[c]["x1T"], "xc1T_in": r1[c]["xc1T"], "halo": halos[c]})
        maps2.append(m)
    r2 = run_bass_kernel_spmd(_prog([2]), maps2, core_ids=cores).results
    f32 = lambda a: np.ascontiguousarray(np.asarray(a, np.float32))
    rw = np.ascontiguousarray(np.asarray(inp["router_w"][0], np.float32).reshape(KT, 128, 8).transpose(1, 0, 2))
    shared3 = {"w_out_o": f32(inp["w_out_o"][0]), "router_w": rw, "moe_w1": f32(inp["moe_w1"][0]), "moe_w3": f32(inp["moe_w3"][0]),
               "moe_w2": f32(inp["moe_w2"][0])}
    maps3 = []
    for c in range(8):
        b, s = c // 4, c % 4
        m = dict(lru)
        m.update(shared3)
        m.update(_common(inp, b))
        sel = np.zeros((128, 4), np.float32)
        sel[:, s] = 1.0
        sa = np.ascontiguousarray(np.stack([r2[4 * b + j]["summ"] for j in range(4)], 1))
        m.update({"x1T_in": r1[c]["x1T"], "halo": halos[c], "sel": sel, "summ_all": sa})
        maps3.append(m)
    r3 = run_bass_kernel_spmd(_prog([3]), maps3, core_ids=cores).results
    out = np.zeros((2, 4 * T, D), np.float32)
    for c in range(8):
        b, s = c // 4, c % 4
        out[b, s * T:(s + 1) * T, :] = r3[c]["outT"].transpose(2, 1, 0).reshape(T, D)
    return out


def kernel_fused(**inp):
    inp = {k: np.asarray(v) for k, v in inp.items()}
    f32 = lambda a: np.ascontiguousarray(np.asarray(a, np.float32))
    maps = _maps1(inp)
    lru = _lru_common(inp)
    rw = np.ascontiguousarray(np.asarray(inp["router_w"][0], np.float32).reshape(KT, 128, 8).transpose(1, 0, 2))
    shared3 = {"w_out_o": f32(inp["w_out_o"][0]), "router_w": rw, "moe_w1": f32(inp["moe_w1"][0]), "moe_w3": f32(inp["moe_w3"][0]),
               "moe_w2": f32(inp["moe_w2"][0])}
    for c in range(8):
        b, s = c // 4, c % 4
        m = maps[c]
        m.update(lru)
        m.update(shared3)
        sel = np.zeros((128, 4), np.float32)
        sel[:, s] = 1.0
        selm = np.zeros((128, 52), np.float32)
        if s > 0:
            selm[:, c - 1] = 1.0
        if s < 3:
            selm[:, 8 + c + 1] = 1.0
        for j in range(4):
            selm[:, 16 + j * 8 + 4 * b + j] = 1.0
        m.update({"sel": sel, "selm": selm})
    r = run_bass_kernel_spmd(_prog([1, 2, 3]), maps, core_ids=list(range(8))).results
    _LAST["r"] = r
    out = np.zeros((2, 4 * T, D), np.float32)
    for c in range(8):
        b, s = c // 4, c % 4
        out[b, s * T:(s + 1) * T, :] = r[c]["outT"].transpose(2, 1, 0).reshape(T, D)
    return out


FUSED_MODE = False


def kernel(**inp):
    return kernel_fused(**inp) if FUSED_MODE else kernel_unfused(**inp)
```

```python
import numpy as np
import concourse.bass as bass
import concourse.mybir as mybir
from concourse.bass_utils import run_bass_kernel_spmd
from contextlib import ExitStack
import types

F32 = mybir.dt.float32
BF16 = mybir.dt.bfloat16
AF = mybir.ActivationFunctionType
ALU = mybir.AluOpType
AX = mybir.AxisListType

D = 1024
KT = 8
T = 2048
CH = 512
MC = 256
NCH = 4
LC = 256
DFF = 2816
NFF = 22
DR = 1280
NB = 10
EPS = 1e-6
SCALE = 0.125
NEG = -1e30
TE = T + 256


def _freeze(fn):
    if fn is None or fn.__closure__ is None:
        return fn
    cells = []
    for c in fn.__closure__:
        try:
            cells.append(types.CellType(c.cell_contents))
        except ValueError:
            cells.append(c)
    return types.FunctionType(fn.__code__, fn.__globals__, fn.__name__, fn.__defaults__, tuple(cells))


class Buf:
    __slots__ = ("w", "r", "name")

    def __init__(self, name=""):
        self.w = None
        self.r = {}
        self.name = name


class Prog:
    ENG = ["pe", "act", "dve", "pool", "sp"]

    def __init__(self, nc, es, n_dsem=48):
        self.nc = nc
        self.es = es
        self.LIMIT = 30000
        self.epoch = {e: 0 for e in self.ENG}
        self.sems = {e + "#0": es.enter_context(nc.semaphore("s_" + e)) for e in self.ENG}
        self.dsems = [es.enter_context(nc.semaphore("d%d" % i)) for i in range(n_dsem)]
        self.dcnt = [0] * n_dsem
        self.dnext = {"sw": 0, "hw": 0}
        self.cnt = {e: 0 for e in self.ENG}
        self.seen = {e: {} for e in self.ENG}
        self.q = {e: [] for e in self.ENG}
        self.ccsem = es.enter_context(nc.semaphore("ccsem"))
        self.cccnt = 0
        self.pcsem = es.enter_context(nc.semaphore("pcsem"))
        self.pccnt = 0

    def _semh(self, k):
        if k[0] == "c":
            return self.ccsem
        if k[0] == "p":
            return self.pcsem
        return self.sems[k[1]] if k[0] == "e" else self.dsems[k[1]]

    def cc(self, fn, reads=(), writes=()):
        waits = self._waits("pool", reads, writes)
        self.cccnt += 1
        tok = (("c", 0), self.cccnt)
        self.q["pool"].append((waits, _freeze(fn), ("c", 0)))
        self._mark(tok, reads, writes)
        return tok

    def _waits(self, eng, reads, writes, skip_self=False):
        need = {}

        def add(k, v):
            if skip_self and k[0] == "e" and k[1].split("#")[0] == eng:
                return
            if need.get(k, 0) < v:
                need[k] = v
        for b in reads:
            if b.w is not None:
                add(*b.w)
        for b in writes:
            if b.w is not None:
                add(*b.w)
            for k, v in b.r.items():
                add(k, v)
        out = []
        seen = self.seen[eng]
        for k, v in need.items():
            if seen.get(k, 0) < v:
                seen[k] = v
                out.append((k, v))
        return out

    def _mark(self, tok, reads, writes):
        k, v = tok
        for b in reads:
            if b.r.get(k, 0) < v:
                b.r[k] = v
        for b in writes:
            b.w = tok
            b.r = {}

    def op(self, eng, fn, reads=(), writes=(), skip_self=False):
        waits = self._waits(eng, reads, writes, skip_self)
        if self.cnt[eng] >= self.LIMIT:
            self.epoch[eng] += 1
            self.cnt[eng] = 0
            key = eng + "#%d" % self.epoch[eng]
            self.sems[key] = self.es.enter_context(self.nc.semaphore("s_%s_%d" % (eng, self.epoch[eng])))
        key = eng + "#%d" % self.epoch[eng]
        self.cnt[eng] += 1
        tok = (("e", key), self.cnt[eng])
        self.q[eng].append((waits, _freeze(fn), ("e", key)))
        self._mark(tok, reads, writes)
        return tok

    def dma(self, eng, out, in_, reads=(), writes=(), **kw):
        half = len(self.dsems) // 2
        cls = "sw" if eng == "pool" else "hw"
        i = self.dnext[cls] + (0 if cls == "sw" else half)
        self.dnext[cls] = (self.dnext[cls] + 1) % half
        waits = self._waits(eng, reads, writes)
        if self.dcnt[i] > 0:
            k = ("d", i)
            v = self.dcnt[i]
            if self.seen[eng].get(k, 0) < v:
                self.seen[eng][k] = v
                waits.append((k, v))
        self.dcnt[i] += 16
        tok = (("d", i), self.dcnt[i])
        self.q[eng].append((waits, (lambda e: e.dma_start(out=out, in_=in_, **kw)), ("d", i)))
        self._mark(tok, reads, writes)
        return tok

    def dma_bulk(self, eng, out, in_):
        self.pccnt += 16
        self.q[eng].append(([], (lambda e: e.dma_start(out=out, in_=in_)), ("p", 0)))

    def bulk_done(self, bufs):
        for b in bufs:
            b.w = (("p", 0), self.pccnt)
            b.r = {}

    def barrier(self, final=False):
        toks = [(("e", e + "#%d" % self.epoch[e]), self.cnt[e]) for e in self.ENG if self.cnt[e] > 0]
        toks += [(("d", i), c) for i, c in enumerate(self.dcnt) if c > 0]
        if self.cccnt > 0:
            toks.append((("c", 0), self.cccnt))
        if final and self.pccnt > 0:
            toks.append((("p", 0), self.pccnt))
        for e in self.ENG:
            waits = []
            for k, v in toks:
                if self.seen[e].get(k, 0) < v:
                    self.seen[e][k] = v
                    waits.append((k, v))
            if waits:
                self.q[e].append((waits, None, None))

    def emit(self, block):
        engobj = {"pe": "tensor", "act": "scalar", "dve": "vector", "pool": "gpsimd", "sp": "sync"}

        def make(ename):
            items = self.q[ename]

            def body(e):
                for waits, fn, inc in items:
                    for k, v in waits:
                        e.wait_ge(self._semh(k), v)
                    if fn is None:
                        continue
                    ins = fn(e)
                    if inc[0] == "e":
                        ins.then_inc(self.sems[inc[1]], 1)
                    elif inc[0] == "c":
                        ins.then_inc(self.ccsem, 1)
                    elif inc[0] == "p":
                        ins.then_inc(self.pcsem, 16)
                    else:
                        ins.then_inc(self.dsems[inc[1]], 16)
            return body
        for ename in self.ENG:
            getattr(block, engobj[ename])(make(ename))


class Ring:
    uid = 0

    def __init__(self, nc, es, name, shape, dtype, n):
        Ring.uid += 1
        self.tiles = [es.enter_context(nc.sbuf_tensor("%s%d_%d" % (name, i, Ring.uid), list(shape), dtype)) for i in range(n)]
        self.bufs = [Buf("%s%d" % (name, i)) for i in range(n)]
        self.i = 0

    def next(self):
        j = self.i % len(self.tiles)
        self.i += 1
        return self.tiles[j], self.bufs[j]


FAST = False


def build(phases, dbg=False):
    nc = bass.Bass("TRN2", target_bir_lowering=False)

    def din(name, shape):
        return nc.dram_tensor(name, list(shape), F32, kind="ExternalInput").ap()

    def dout(name, shape):
        return nc.dram_tensor(name, list(shape), F32, kind="ExternalOutput").ap()

    P1, P2, P3 = (1 in phases), (2 in phases), (3 in phases)
    first = min(phases)
    last = max(phases)
    I = {}
    I["ccol"] = din("ccol", [128, KT, 2])
    I["vecs"] = din("vecs", [128, 5, KT])
    I["ident"] = din("ident", [128, 128])
    if P1:
        for n_, s_ in [("xT", [128, KT, T]), ("xhT", [128, KT, 256]), ("ctxT", [128, KT, LC]), ("ada_w_e", [D, 6 * D]),
                       ("ada_b_e", [128, 48]), ("w_in_e", [D, 2432]), ("wsT", [128, 4, 128]), ("bsb", [128, 512]),
                       ("sinkb", [128, 8]), ("w_out_e", [D, D]), ("ffn_w1", [D, DFF]), ("ffn_w3", [D, DFF]),
                       ("ffn_w2", [DFF, D]), ("ropeCS", [128, 2, TE]), ("masks", [128, 3, 384])]:
            I[n_] = din(n_, s_)
    I["ada_w_o"] = din("ada_w_o", [D, 6 * D])
    I["ada_b_o"] = din("ada_b_o", [128, 48])
    I["w_in_o"] = din("w_in_o", [D, 2 * DR])
    if first > 1:
        I["x1T_in"] = din("x1T_in", [128, KT, T])
    if P2 and first > 1:
        I["xc1T_in"] = din("xc1T_in", [128, KT, LC])
    if P2 or P3:
        for n_, s_ in [("convw", [128, NB, 4]), ("convb", [128, NB]), ("lru_wa", [2, NB, 128, 128]), ("lru_wx", [2, NB, 128, 128]),
                       ("lru_ba", [128, 2, NB]), ("lru_bx", [128, 2, NB]), ("lru_lam", [128, 2, NB])]:
            I[n_] = din(n_, s_)
        if not P1:
            I["halo"] = din("halo", [128, NB, 3])
        else:
            I["selm"] = din("selm", [128, 52])
    if P3:
        for n_, s_ in [("w_out_o", [DR, D]), ("router_w", [128, KT, 8]), ("moe_w1", [8, D, DFF]), ("moe_w3", [8, D, DFF]),
                       ("moe_w2", [8, DFF, D]), ("sel", [128, 4])]:
            I[n_] = din(n_, s_)
        if not P2:
            I["summ_all"] = din("summ_all", [128, 4, NB, 6])
    FUSED = P1 and P2 and P3
    if FUSED:
        cc1_in = nc.dram_tensor("cc1_in", [128, NB * 3], F32).ap()
        cc1_out = nc.dram_tensor("cc1_out", [8 * 128, NB * 3], F32).ap()
        cc2_in = nc.dram_tensor("cc2_in", [128, NB * 6], F32).ap()
        cc2_out = nc.dram_tensor("cc2_out", [8 * 128, NB * 6], F32).ap()
        SCR = {"w_in_o": nc.dram_tensor("w_in_o_bf", [D, 2 * DR], BF16).ap(),
               "lru_wa": nc.dram_tensor("lru_wa_bf", [2, NB, 128, 128], BF16).ap(),
               "lru_wx": nc.dram_tensor("lru_wx_bf", [2, NB, 128, 128], BF16).ap(),
               "w_out_o": nc.dram_tensor("w_out_o_bf", [DR, D], BF16).ap(),
               "moe_w1": nc.dram_tensor("moe_w1_bf", [8, D, DFF], BF16).ap(),
               "moe_w3": nc.dram_tensor("moe_w3_bf", [8, D, DFF], BF16).ap(),
               "moe_w2": nc.dram_tensor("moe_w2_bf", [8, DFF, D], BF16).ap()}
    O = {}
    if last == 1:
        O["x1T"] = dout("x1T", [128, KT, T])
        O["xc1T"] = dout("xc1T", [128, KT, LC])
        O["brec"] = dout("brec", [128, NB, 3])
    if last == 2:
        O["summ"] = dout("summ", [128, NB, 6])
    if last == 3:
        O["outT"] = dout("outT", [128, KT, T])
    if dbg:
        O["dbg"] = dout("dbg", [128, 4096])

    with ExitStack() as es:
        P = Prog(nc, es)

        def sb(name, shape, dt=F32, st=es):
            Ring.uid += 1
            return st.enter_context(nc.sbuf_tensor("%s_%d" % (name, Ring.uid), list(shape), dt))

        X = sb("X", [128, KT, T])
        XB = [[Buf("X%d_%d" % (kt, c)) for c in range(NCH)] for kt in range(KT)]
        Xc = sb("Xc", [128, KT, LC])
        XcB = [Buf("Xc%d" % kt) for kt in range(KT)]
        pst = es.enter_context(nc.psum_tensor("pst", [128, 4096], F32))
        PSB = [Buf("ps%d" % i) for i in range(8)]
        pstate = {"i": 0, "l": 0}

        def bank():
            i = pstate["i"] % 6
            pstate["i"] += 1
            return pst[:, i * 512:(i + 1) * 512], PSB[i]

        def bank2():
            if pstate["i"] % 2:
                pstate["i"] += 1
            i = pstate["i"] % 6
            pstate["i"] += 2
            return pst[:, i * 512:(i + 2) * 512], [PSB[i], PSB[i + 1]]

        def lbank():
            i = 6 + pstate["l"] % 2
            pstate["l"] += 1
            return pst[:, i * 512:(i + 1) * 512], PSB[i]

        ident32 = sb("ident32", [128, 128])
        identb = sb("identb", [128, 128], BF16)
        onesb = sb("onesb", [128, 128], BF16)
        ones32 = sb("ones32", [128, 128])
        vecs = sb("vecs_sb", [128, 5, KT])
        ccol = sb("ccol_sb", [128, KT, 2])
        cact = sb("cact", [128, KT, 2], BF16)
        modE = sb("modE", [128, 2, 48])
        modO = sb("modO", [128, 2, 48])
        gam = sb("gam", [128, 4, 2, KT])
        CB = Buf("consts")
        scrB = Buf("scratch")
        WQ = "sp" if FUSED else "pool"
        WS = SCR if FUSED else I
        WRD = [scrB] if FUSED else []

        def precast():
            RP = 256
            for nm in ("w_in_o", "w_out_o"):
                rows = SCR[nm].shape[0]
                for r0 in range(0, rows, RP):
                    P.dma_bulk("pool", SCR[nm][r0:r0 + RP, :], I[nm][r0:r0 + RP, :])
            for nm in ("lru_wa", "lru_wx"):
                P.dma_bulk("pool", SCR[nm].rearrange("d b i j -> (d b) (i j)"), I[nm].rearrange("d b i j -> (d b) (i j)"))
            for ex in range(8):
                for nm in ("moe_w1", "moe_w3", "moe_w2"):
                    rows = SCR[nm].shape[1]
                    for r0 in range(0, rows, RP):
                        P.dma_bulk("pool", SCR[nm][ex, r0:r0 + RP, :], I[nm][ex, r0:r0 + RP, :])
            P.bulk_done([scrB])
        halo = sb("halo", [128, NB, 3])
        haloB = Buf("halo")
        sa = sb("summ_all", [128, 4, NB, 6])
        saB = Buf("sa")
        selm = sb("selm", [128, 52])
        block = es.enter_context(nc.Block())

        def mm(out, outbufs, pairs, reads, skip_self=False):
            def f(e, out=out, pairs=pairs):
                n = len(pairs)
                ins = None
                for i, (l, r) in enumerate(pairs):
                    ins = e.matmul(out, lhsT=l, rhs=r, start=(i == 0), stop=(i == n - 1))
                return ins
            P.op("pe", f, reads=reads, writes=outbufs, skip_self=skip_self)

        def dve(f, reads, writes):
            P.op("dve", f, reads, writes)

        def act(f, reads, writes):
            P.op("act", f, reads, writes)

        def pool(f, reads, writes):
            P.op("pool", f, reads, writes)

        P.dma("sp", ident32[:], I["ident"], writes=[CB])
        P.dma("pool", identb[:], I["ident"], writes=[CB])
        P.dma("sp", vecs[:], I["vecs"], writes=[CB])
        P.dma("sp", ccol[:], I["ccol"], writes=[CB])
        dve(lambda e: e.memset(onesb[:], 1.0), [], [CB])
        dve(lambda e: e.memset(ones32[:], 1.0), [], [CB])
        act(lambda e: e.activation(out=cact[:], in_=ccol[:], func=AF.Silu), [CB], [CB])

        def setup_mods(wname, bname, dst):
            with ExitStack() as st:
                wr = Ring(nc, st, "adaw", [128, KT, 768], BF16, 2)
                adab = sb("adab", [128, 48], F32, st)
                ab = Buf()
                P.dma("sp", adab[:], I[bname], writes=[ab])
                ps, pb = lbank()
                wv = I[wname].rearrange("(kt p) n -> p kt n", p=128)
                for piece in range(8):
                    wt, wb = wr.next()
                    P.dma("pool", wt[:], wv[:, :, piece * 768:(piece + 1) * 768], writes=[wb])
                    for m in range(6):
                        col = piece * 6 + m
                        mm(ps[:, col * 2:col * 2 + 2], [pb],
                           [(wt[:, kt, m * 128:(m + 1) * 128], cact[:, kt, :]) for kt in range(KT)], [wb, CB], skip_self=True)
                psv = ps[:, 0:96].rearrange("p (m w) -> p m w", w=2)
                for w in range(2):
                    dve(lambda e, w=w: e.tensor_tensor(out=dst[:, w, :], in0=psv[:, :, w], in1=adab[:], op=ALU.add), [pb, ab], [CB])
                P.barrier()

        def setup_gam(idx, mod, nvec_idx, jsc):
            for w in range(2):
                dve(lambda e, w=w: e.scalar_tensor_tensor(out=gam[:, idx, w, :], in0=mod[:, w, jsc * 8:(jsc + 1) * 8], scalar=1.0,
                                                          in1=vecs[:, nvec_idx, :], op0=ALU.add, op1=ALU.mult), [CB], [CB])

        if P1:
            setup_mods("ada_w_e", "ada_b_e", modE)
            setup_gam(0, modE, 0, 1)
            setup_gam(1, modE, 1, 4)
        setup_mods("ada_w_o", "ada_b_o", modO)
        setup_gam(2, modO, 2, 1)
        setup_gam(3, modO, 3, 4)

        def norm_chunk(st_rings, src, n, gidx, w, beta, dst, dst32=None):
            sqr, rsr, tmr = st_rings
            ps, pb = lbank()
            for kt in range(KT):
                sa, sbuf_ = src(kt)
                sq, sqb = sqr.next()
                act(lambda e, sa=sa, sq=sq: e.activation(out=sq[:, :n], in_=sa, func=AF.Square), [sbuf_], [sqb])
                P.op("pe", (lambda e, ps=ps, sq=sq, kt=kt: e.matmul(ps[:, :n], lhsT=onesb[:], rhs=sq[:, :n], start=(kt == 0), stop=(kt == KT - 1))),
                     [sqb, CB], [pb], skip_self=(kt > 0))
            rs, rsb = rsr.next()
            act(lambda e, rs=rs, ps=ps: e.activation(out=rs[:, :n], in_=ps[:, :n], func=AF.Sqrt, scale=1.0 / D, bias=EPS), [pb], [rsb])
            dve(lambda e, rs=rs: e.reciprocal(out=rs[:, :n], in_=rs[:, :n]), [rsb], [rsb])
            for kt in range(KT):
                sa, sbuf_ = src(kt)
                tm, tmb = tmr.next()
                dve(lambda e, tm=tm, sa=sa, rs=rs: e.tensor_tensor(out=tm[:, :n], in0=sa, in1=rs[:, :n], op=ALU.mult), [sbuf_, rsb], [tmb])
                da, db = dst(kt)
                g = gam[:, gidx, w, kt:kt + 1] if gidx is not None else vecs[:, 4, kt:kt + 1]
                if dst32 is None:
                    if beta is not None:
                        act(lambda e, da=da, tm=tm, g=g, bt=beta(kt): e.activation(out=da, in_=tm[:, :n], func=AF.Identity, scale=g, bias=bt),
                            [tmb, CB], [db])
                    else:
                        act(lambda e, da=da, tm=tm, g=g: e.activation(out=da, in_=tm[:, :n], func=AF.Identity, scale=g), [tmb, CB], [db])
                else:
                    d32, d32b = dst32(kt)
                    act(lambda e, d32=d32, tm=tm, g=g, bt=beta(kt): e.activation(out=d32, in_=tm[:, :n], func=AF.Identity, scale=g, bias=bt),
                        [tmb, CB], [d32b])
                    pool(lambda e, da=da, d32=d32: e.tensor_copy(out=da, in_=d32), [d32b], [db])

        def norm_rings(st):
            return (Ring(nc, st, "nsq", [128, CH], BF16, 3), Ring(nc, st, "nrs", [128, CH], F32, 2), Ring(nc, st, "ntm", [128, CH], F32, 3))

        def xsrc(c, n=CH):
            return lambda kt: (X[:, kt, c * CH:c * CH + n], XB[kt][c])

        def xcsrc():
            return lambda kt: (Xc[:, kt, :], XcB[kt])

        def ffn_pass(st, w1v, w3v, w2v, chunks, gbc=None, wq="pool", wrd=()):
            groups = [(0, 4), (4, 4), (8, 4), (12, 4), (16, 4), (20, 2)]
            w1r, w3r, w2r, actr, sr, sgr = st
            stages = []
            for gi, (f0, G) in enumerate(groups):
                for ci in range(len(chunks)):
                    stages.append((gi, ci))
            wcur = {}

            def load_group(gi):
                f0, G = groups[gi]
                a, ab = w1r.next()
                b, bb = w3r.next()
                c, cb = w2r.next()
                P.dma(wq, a[:, :, :G * 128], w1v[:, :, f0 * 128:(f0 + G) * 128], reads=list(wrd), writes=[ab])
                P.dma(wq, b[:, :, :G * 128], w3v[:, :, f0 * 128:(f0 + G) * 128], reads=list(wrd), writes=[bb])
                P.dma(wq, c[:, :G, :], w2v[:, f0:f0 + G, :], reads=list(wrd), writes=[cb])
                wcur[gi] = (a, ab, b, bb, c, cb)

            def gu_stage(s):
                gi, ci = stages[s]
                f0, G = groups[gi]
                if gi not in wcur:
                    load_group(gi)
                a, ab, b, bb, c, cb = wcur[gi]
                h2, hb, n, xdst, g2, col0 = chunks[ci]
                at, atb = actr.next()
                for i in range(G):
                    pg, pgb = bank()
                    pu, pub = bank()
                    mm(pg[:, :n], [pgb], [(a[:, kt, i * 128:(i + 1) * 128], h2(kt)) for kt in range(KT)], [ab, hb])
                    mm(pu[:, :n], [pub], [(b[:, kt, i * 128:(i + 1) * 128], h2(kt)) for kt in range(KT)], [bb, hb])
                    s_, s_b = sr.next()
                    act(lambda e, s_=s_, pg=pg: e.activation(out=s_[:, :n], in_=pg[:, :n], func=AF.Silu), [pgb], [s_b])
                    if gbc is not None:
                        gt, gtb = gbc
                        sg, sgb = sgr.next()
                        pool(lambda e, sg=sg, s_=s_, gt=gt: e.tensor_tensor(out=sg[:, :n], in0=s_[:, :n], in1=gt[:, col0:col0 + n], op=ALU.mult),
                             [s_b, gtb], [sgb])
                        s_, s_b = sg, sgb
                    dve(lambda e, at=at, i=i, s_=s_, pu=pu: e.tensor_tensor(out=at[:, i, :n], in0=s_[:, :n], in1=pu[:, :n], op=ALU.mult),
                        [s_b, pub], [atb])
                return (at, atb)

            def y_stage(s, atp):
                gi, ci = stages[s]
                f0, G = groups[gi]
                a, ab, b, bb, c, cb = wcur[gi]
                h2, hb, n, xdst, g2, col0 = chunks[ci]
                at, atb = atp
                for f in range(KT):
                    py, pyb = bank()
                    mm(py[:, :n], [pyb], [(c[:, i, f * 128:(f + 1) * 128], at[:, i, :n]) for i in range(G)], [cb, atb])
                    xa, xb_ = xdst(f)
                    dve(lambda e, xa=xa, py=py, g=g2(f): e.scalar_tensor_tensor(out=xa, in0=py[:, :n], scalar=g, in1=xa, op0=ALU.mult, op1=ALU.add),
                        [pyb, xb_, CB], [xb_])
            prev = None
            for s in range(len(stages)):
                cur = gu_stage(s)
                if prev is not None:
                    y_stage(s - 1, prev)
                prev = cur
            y_stage(len(stages) - 1, prev)

        def ffn_rings(st):
            return (Ring(nc, st, "w1g", [128, KT, 512], BF16, 2), Ring(nc, st, "w3g", [128, KT, 512], BF16, 2),
                    Ring(nc, st, "w2g", [128, 4, D], BF16, 2), Ring(nc, st, "actg", [128, 4, CH], BF16, 2),
                    Ring(nc, st, "fs", [128, CH], F32, 3), Ring(nc, st, "fsg", [128, CH], F32, 3))

        if P1:
            for kt in range(KT):
                for c in range(NCH):
                    P.dma("sp", X[:, kt, c * CH:(c + 1) * CH], I["xT"][:, kt, c * CH:(c + 1) * CH], writes=[XB[kt][c]])
                P.dma("sp", Xc[:, kt, :], I["ctxT"][:, kt, :], writes=[XcB[kt]])
            with ExitStack() as st:
                kTe = sb("kTe", [128, TE], BF16, st)
                kTeB = [Buf("kTe%d" % i) for i in range(TE // 128)]
                kcT = sb("kcT", [128, LC], BF16, st)
                kcB = Buf("kcT")
                Ve = sb("Ve", [128, TE // 128, 128], BF16, st)
                VeB = [Buf("Ve%d" % i) for i in range(TE // 128)]
                Vc = sb("Vc", [128, 2, 128], BF16, st)
                VcB = Buf("Vc")
                ropeC = sb("ropeC", [128, TE], F32, st)
                ropeS = sb("ropeS", [128, TE], F32, st)
                maskb = sb("maskb", [128, 3, 384], BF16, st)
                wsT = sb("wsT", [128, 4, 128], BF16, st)
                bsb = sb("bsb", [128, 512], F32, st)
                sinkb = sb("sinkb", [128, 8], F32, st)
                P.dma("sp", ropeC[:], I["ropeCS"][:, 0, :], writes=[CB])
                P.dma("sp", ropeS[:], I["ropeCS"][:, 1, :], writes=[CB])
                P.dma("pool", maskb[:], I["masks"], writes=[CB])
                P.dma("pool", wsT[:], I["wsT"], writes=[CB])
                P.dma("sp", bsb[:], I["bsb"], writes=[CB])
                P.dma("sp", sinkb[:], I["sinkb"], writes=[CB])
                wv = I["w_in_e"].rearrange("(kt p) n -> p kt n", p=128)
                with ExitStack() as st2:
                    Xh = sb("Xh", [128, KT, 256], F32, st2)
                    XhB = Buf("Xh")
                    P.dma("sp", Xh[:], I["xhT"], writes=[XhB])
                    nr = norm_rings(st2)
                    hT = sb("hT", [128, KT, CH], BF16, st2)
                    hTB = Buf("hT")
                    rt = Ring(nc, st2, "ropet", [128, CH], F32, 4)
                    wpre = sb("wpre", [128, KT, 384], BF16, st2)
                    wpB = Buf("wpre")
                    P.dma("pool", wpre[:], wv[:, :, 2048:2432], writes=[wpB])
                    pre = [("ctx", xcsrc(), LC, 1, None, None),
                           ("hl", (lambda kt: (Xh[:, kt, 0:128], XhB)), 128, 0, 0, 0),
                           ("hr", (lambda kt: (Xh[:, kt, 128:256], XhB)), 128, 0, TE - 128, TE // 128 - 1)]
                    for c in range(NCH):
                        pre.append(("lat", xsrc(c), CH, 0, 128 + c * CH, 1 + c * 4))
                    for kind, src, n, w, e0, vt0 in ([] if FAST else pre):
                        norm_chunk(nr, src, n, 0, w, (lambda kt, w=w: modE[:, w, 0 * 8 + kt:0 * 8 + kt + 1]),
                                   (lambda kt: (hT[:, kt, :n], hTB)))
                        pk, pkb = bank()
                        mm(pk[:, :n], [pkb], [(wpre[:, kt, 0:128], hT[:, kt, :n]) for kt in range(KT)], [wpB, hTB])
                        if kind == "ctx":
                            act(lambda e, pk=pk: e.activation(out=kcT[:, :], in_=pk[:, :LC], func=AF.Copy), [pkb], [kcB])
                        else:
                            pks, pksb = bank()
                            mm(pks[:, :n], [pksb], [(wpre[:, kt, 128:256], hT[:, kt, :n]) for kt in range(KT)], [wpB, hTB])
                            t1, t1b = rt.next()
                            t2, t2b = rt.next()
                            dve(lambda e, t1=t1, pk=pk, e0=e0, n=n: e.tensor_tensor(out=t1[:, :n], in0=pk[:, :n], in1=ropeC[:, e0:e0 + n], op=ALU.mult),
                                [pkb, CB], [t1b])
                            dve(lambda e, t2=t2, pks=pks, e0=e0, n=n: e.tensor_tensor(out=t2[:, :n], in0=pks[:, :n], in1=ropeS[:, e0:e0 + n], op=ALU.mult),
                                [pksb, CB], [t2b])
                            kb_ = kTeB[e0 // 128:(e0 + n) // 128]
                            pool(lambda e, t1=t1, t2=t2, e0=e0, n=n: e.tensor_tensor(out=kTe[:, e0:e0 + n], in0=t1[:, :n], in1=t2[:, :n], op=ALU.add),
                                 [t1b, t2b], kb_)
                        for tt in range(n // 128):
                            pv, pvb = bank()
                            mm(pv[:, 0:128], [pvb], [(hT[:, kt, tt * 128:(tt + 1) * 128], wpre[:, kt, 256:384]) for kt in range(KT)], [wpB, hTB])
                            if kind == "ctx":
                                act(lambda e, pv=pv, tt=tt: e.activation(out=Vc[:, tt, :], in_=pv[:, 0:128], func=AF.Copy), [pvb], [VcB])
                            else:
                                act(lambda e, pv=pv, vi=vt0 + tt: e.activation(out=Ve[:, vi, :], in_=pv[:, 0:128], func=AF.Copy), [pvb], [VeB[vt0 + tt]])
                    P.barrier()
                with ExitStack() as st2:
                    wmain = sb("wmain", [128, KT, 2048], BF16, st2)
                    wmB = Buf("wmain")
                    for q4 in range(4):
                        P.dma("pool", wmain[:, :, q4 * 512:(q4 + 1) * 512], wv[:, :, q4 * 512:(q4 + 1) * 512], writes=[wmB])
                    wout = sb("wout", [128, KT, D], BF16, st2)
                    woB = Buf("wout")
                    P.dma("pool", wout[:], I["w_out_e"].rearrange("(kt p) n -> p kt n", p=128), writes=[woB])
                    if FUSED:
                        precast()
                    nr = (Ring(nc, st2, "nsq2", [128, MC], BF16, 2), Ring(nc, st2, "nrs2", [128, MC], F32, 2), Ring(nc, st2, "ntm2", [128, MC], F32, 2))
                    hT = sb("hT2", [128, KT, MC], BF16, st2)
                    hTB = Buf("hT2")
                    rt = Ring(nc, st2, "ropet2", [128, 512], F32, 3)
                    gu = sb("gu", [128, 4, MC], BF16, st2)
                    guB = Buf("gu")
                    qT = sb("qT", [128, 4, MC], BF16, st2)
                    qTB = Buf("qT")
                    mix = sb("mix", [128, KT, MC], BF16, st2)
                    mixA = Buf("mixA")
                    mixBq = [Buf("mixB%d" % i) for i in range(4)]
                    gvr = Ring(nc, st2, "gv", [128, 512], F32, 2)
                    vnr = Ring(nc, st2, "vn", [128, 512], BF16, 2)
                    str_ = Ring(nc, st2, "bnst", [128, 4, 6], F32, 2)
                    mvr = Ring(nc, st2, "bnmv", [128, 4, 2], F32, 2)
                    sdr = Ring(nc, st2, "sd", [128, 4], F32, 2)
                    ptr = Ring(nc, st2, "Pt", [128, 640], BF16, 3)
                    ptsr = Ring(nc, st2, "PTs", [128, 640], BF16, 2)
                    dgr = Ring(nc, st2, "Dg", [128, 128], BF16, 3)
                    smr = Ring(nc, st2, "sm", [128, 8], F32, 4)
                    main = [("ctx", xcsrc(), LC, 1, None)] + [("lat", (lambda kt, c2=c2: (X[:, kt, c2 * MC:(c2 + 1) * MC], XB[kt][c2 * MC // CH])), MC, 0, c2)
                                                          for c2 in range(T // MC)]
                    for kind, src, n, w, c in ([] if FAST else main):
                        norm_chunk(nr, src, n, 0, w, (lambda kt, w=w: modE[:, w, kt:kt + 1]), (lambda kt: (hT[:, kt, :n], hTB)))
                        for m in range(4):
                            pu, pub = bank()
                            mm(pu[:, :n], [pub], [(wmain[:, kt, m * 128:(m + 1) * 128], hT[:, kt, :n]) for kt in range(KT)], [wmB, hTB])
                            act(lambda e, pu=pu, m=m, n=n: e.activation(out=gu[:, m, :n], in_=pu[:, :n], func=AF.Gelu_apprx_tanh), [pub], [guB])
                        for j in range(4):
                            pq, pqb = bank()
                            mm(pq[:, :n], [pqb], [(wmain[:, kt, 1024 + j * 128:1024 + (j + 1) * 128], hT[:, kt, :n]) for kt in range(KT)], [wmB, hTB])
                            if kind == "ctx":
                                act(lambda e, pq=pq, j=j, n=n: e.activation(out=qT[:, j, :n], in_=pq[:, :n], func=AF.Copy), [pqb], [qTB])
                            else:
                                pqs, pqsb = bank()
                                mm(pqs[:, :n], [pqsb], [(wmain[:, kt, 1536 + j * 128:1536 + (j + 1) * 128], hT[:, kt, :n]) for kt in range(KT)], [wmB, hTB])
                                e0 = 128 + c * MC
                                t1, t1b = rt.next()
                                t2, t2b = rt.next()
                                dve(lambda e, t1=t1, pq=pq, e0=e0, n=n: e.tensor_tensor(out=t1[:, :n], in0=pq[:, :n], in1=ropeC[:, e0:e0 + n], op=ALU.mult),
                                    [pqb, CB], [t1b])
                                dve(lambda e, t2=t2, pqs=pqs, e0=e0, n=n: e.tensor_tensor(out=t2[:, :n], in0=pqs[:, :n], in1=ropeS[:, e0:e0 + n], op=ALU.mult),
                                    [pqsb, CB], [t2b])
                                pool(lambda e, t1=t1, t2=t2, j=j, n=n: e.tensor_tensor(out=qT[:, j, :n], in0=t1[:, :n], in1=t2[:, :n], op=ALU.add),
                                     [t1b, t2b], [qTB])
                        for tt in range(n // 128):
                            tsl = slice(tt * 128, (tt + 1) * 128)
                            pv, pvb = bank()
                            mm(pv[:, :], [pvb], [(hT[:, kt, tsl], wmain[:, kt, 512:1024]) for kt in range(KT)], [wmB, hTB])
                            gv, gvb = gvr.next()
                            act(lambda e, gv=gv, pv=pv: e.activation(out=gv[:], in_=pv[:], func=AF.Gelu_apprx_tanh), [pvb], [gvb])
                            stt, stb = str_.next()
                            mv, mvb = mvr.next()
                            for g in range(4):
                                dve(lambda e, stt=stt, gv=gv, g=g: e.bn_stats(out=stt[:, g, :], in_=gv[:, g * 128:(g + 1) * 128]), [gvb], [stb])
                                dve(lambda e, stt=stt, mv=mv, g=g: e.bn_aggr(out=mv[:, g, :], in_=stt[:, g, :]), [stb], [mvb])
                            sd, sdb = sdr.next()
                            act(lambda e, sd=sd, mv=mv: e.activation(out=sd[:], in_=mv[:, :, 1], func=AF.Sqrt, bias=EPS, scale=1.0), [mvb], [sdb])
                            dve(lambda e, sd=sd: e.reciprocal(out=sd[:], in_=sd[:]), [sdb], [sdb])
                            vn, vnb = vnr.next()
                            for g in range(4):
                                dve(lambda e, vn=vn, gv=gv, mv=mv, sd=sd, g=g: e.tensor_scalar(
                                    out=vn[:, g * 128:(g + 1) * 128], in0=gv[:, g * 128:(g + 1) * 128], scalar1=mv[:, g, 0:1],
                                    scalar2=sd[:, g:g + 1], op0=ALU.subtract, op1=ALU.mult), [gvb, mvb, sdb], [vnb])
                            pm, pmb = bank()

                            def mixmm(e, pm=pm, vn=vn):
                                ins = None
                                for g in range(4):
                                    ins = e.matmul(pm[:, g * 128:(g + 1) * 128], lhsT=vn[:, g * 128:(g + 1) * 128], rhs=wsT[:, g, :], start=True, stop=True)
                                return ins
                            P.op("pe", mixmm, [vnb, CB], [pmb])
                            t1, t1b = rt.next()
                            dve(lambda e, t1=t1, pm=pm: e.tensor_tensor(out=t1[:], in0=pm[:], in1=bsb[:], op=ALU.add), [pmb, CB], [t1b])
                            pool(lambda e, t1=t1, tsl=tsl: e.tensor_tensor(out=mix[:, 0:4, tsl], in0=t1[:].rearrange("p (g t) -> p g t", g=4),
                                                                            in1=gu[:, 0:4, tsl], op=ALU.mult), [t1b, guB], [mixA])
                        def att_A(qb, j, hh, po, pob):
                            qsl = slice(qb * 128, (qb + 1) * 128)
                            h = j + 4 * hh
                            psl = slice(hh * 64, (hh + 1) * 64)
                            S, Sb = bank2()
                            if kind == "ctx":
                                nk = 256
                                P.op("pe", (lambda e, S=S, j=j, psl=psl, qsl=qsl: e.matmul(S[:, 0:256], lhsT=qT[psl, j, qsl], rhs=kcT[psl, :], start=True, stop=True)),
                                     [qTB, kcB], Sb)
                                vts = [(Vc, 0, VcB), (Vc, 1, VcB)]
                            else:
                                nk = 640
                                E0 = c * MC + qb * 128
                                gb = c * (MC // 128) + qb
                                mi = 0 if (c == 0 and qb == 0) else (2 if (c == T // MC - 1 and qb == MC // 128 - 1) else 1)

                                def qk(e, S=S, j=j, psl=psl, qsl=qsl, E0=E0, mi=mi):
                                    e.matmul(S[:, 0:384], lhsT=qT[psl, j, qsl], rhs=kTe[psl, E0:E0 + 384], start=True, stop=False)
                                    e.matmul(S[:, 0:384], lhsT=identb[:], rhs=maskb[:, mi, :], start=False, stop=True)
                                    e.matmul(S[:, 384:512], lhsT=qT[psl, j, qsl], rhs=kcT[psl, 0:128], start=True, stop=True)
                                    return e.matmul(S[:, 512:640], lhsT=qT[psl, j, qsl], rhs=kcT[psl, 128:256], start=True, stop=True)
                                P.op("pe", qk, [qTB, kcB, CB] + kTeB[gb:gb + 3], Sb)
                                vts = [(Ve, gb, VeB[gb]), (Ve, gb + 1, VeB[gb + 1]), (Ve, gb + 2, VeB[gb + 2]), (Vc, 0, VcB), (Vc, 1, VcB)]
                            sm, smb = smr.next()
                            dve(lambda e, sm=sm, S=S, nk=nk: e.reduce_max(out=sm[:, 0:1], in_=S[:, 0:nk], axis=AX.X), Sb, [smb])
                            dve(lambda e, sm=sm, h=h: e.tensor_scalar(out=sm[:, 1:2], in0=sm[:, 0:1], scalar1=SCALE, scalar2=sinkb[:, h:h + 1],
                                                                      op0=ALU.mult, op1=ALU.max), [smb, CB], [smb])
                            dve(lambda e, sm=sm: e.tensor_scalar(out=sm[:, 2:3], in0=sm[:, 1:2], scalar1=-1.0, scalar2=None, op0=ALU.mult), [smb], [smb])
                            dve(lambda e, sm=sm: e.memset(sm[:, 3:4], 0.0), [smb], [smb])
                            pt, ptb = ptr.next()
                            act(lambda e, pt=pt, S=S, sm=sm, nk=nk: e.activation(out=pt[:, 0:nk], in_=S[:, 0:nk], func=AF.Exp, scale=SCALE,
                                                                               bias=sm[:, 2:3], accum_out=sm[:, 3:4]), Sb + [smb], [ptb, smb])
                            act(lambda e, sm=sm, h=h: e.activation(out=sm[:, 4:5], in_=sinkb[:, h:h + 1], func=AF.Exp, scale=1.0, bias=sm[:, 2:3]),
                                [smb, CB], [smb])
                            dve(lambda e, sm=sm: e.tensor_tensor(out=sm[:, 5:6], in0=sm[:, 3:4], in1=sm[:, 4:5], op=ALU.add), [smb], [smb])
                            dve(lambda e, sm=sm: e.reciprocal(out=sm[:, 6:7], in_=sm[:, 5:6]), [smb], [smb])
                            dg, dgb = dgr.next()
                            dve(lambda e, dg=dg, sm=sm: e.tensor_scalar(out=dg[:], in0=identb[:], scalar1=sm[:, 6:7], scalar2=None, op0=ALU.mult),
                                [smb, CB], [dgb])
                            return dict(j=j, hh=hh, po=po, pob=pob, pt=pt, ptb=ptb, dg=dg, dgb=dgb, nk=nk, vts=vts, psl=psl, qsl=qsl)

                        def att_C(u):
                            j, hh, po, pob, pt, ptb, dg, dgb, nk, vts, psl, qsl = (u[k_] for k_ in ("j", "hh", "po", "pob", "pt", "ptb", "dg", "dgb", "nk", "vts", "psl", "qsl"))
                            PT, PTb = bank2()
                            nkt = nk // 128

                            def tr(e, PT=PT, pt=pt, dg=dg, nkt=nkt):
                                ins = None
                                for k_ in range(nkt):
                                    ins = e.matmul(PT[:, k_ * 128:(k_ + 1) * 128], lhsT=pt[:, k_ * 128:(k_ + 1) * 128], rhs=dg[:], start=True, stop=True)
                                return ins
                            P.op("pe", tr, [ptb, dgb], PTb)
                            pts, ptsb = ptsr.next()
                            act(lambda e, pts=pts, PT=PT, nk=nk: e.activation(out=pts[:, 0:nk], in_=PT[:, 0:nk], func=AF.Copy), PTb, [ptsb])

                            def pv_(e, po=po, pts=pts, vts=vts, psl=psl, hh=hh):
                                ins = None
                                for k_, (vt, vi, _) in enumerate(vts):
                                    ins = e.matmul(po[psl, 0:128], lhsT=vt[:, vi, hh * 64:(hh + 1) * 64], rhs=pts[:, k_ * 128:(k_ + 1) * 128],
                                                   start=(k_ == 0), stop=(k_ == len(vts) - 1))
                                return ins
                            P.op("pe", pv_, [ptsb] + [v[2] for v in vts], [pob], skip_self=(hh == 1))
                            if hh == 1:
                                dve(lambda e, po=po, j=j, qsl=qsl: e.tensor_copy(out=mix[:, 4 + j, qsl], in_=po[:, 0:128]), [pob], [mixBq[j]])
                        pend = None
                        for qb in range(n // 128):
                            for j in range(4):
                                po, pob = lbank()
                                for hh in range(2):
                                    cur = att_A(qb, j, hh, po, pob)
                                    if pend is not None:
                                        att_C(pend)
                                    pend = cur
                        att_C(pend)
                        for f in range(KT):
                            py, pyb = bank()
                            mm(py[:, :n], [pyb], [(wout[:, mt, f * 128:(f + 1) * 128], mix[:, mt, :n]) for mt in range(KT)], [woB, mixA] + mixBq)
                            if kind == "ctx":
                                xa, xb_ = Xc[:, f, :], XcB[f]
                            else:
                                xa, xb_ = X[:, f, c * MC:(c + 1) * MC], XB[f][c * MC // CH]
                            dve(lambda e, xa=xa, py=py, n=n, g=modE[:, w, 16 + f:16 + f + 1]: e.scalar_tensor_tensor(
                                out=xa, in0=py[:, :n], scalar=g, in1=xa, op0=ALU.mult, op1=ALU.add), [pyb, xb_, CB], [xb_])
                    P.barrier()
                P.barrier()
            with ExitStack() as st:
                nr = norm_rings(st)
                h2T = sb("h2T", [128, KT, T], BF16, st)
                h2B = [Buf("h2_%d" % c) for c in range(NCH)]
                h2c = sb("h2c", [128, KT, LC], BF16, st)
                h2cB = Buf("h2c")
                for c in range(NCH):
                    norm_chunk(nr, xsrc(c), CH, 1, 0, (lambda kt: modE[:, 0, 24 + kt:24 + kt + 1]),
                               (lambda kt, c=c: (h2T[:, kt, c * CH:(c + 1) * CH], h2B[c])))
                norm_chunk(nr, xcsrc(), LC, 1, 1, (lambda kt: modE[:, 1, 24 + kt:24 + kt + 1]), (lambda kt: (h2c[:, kt, :], h2cB)))
                chunks = []
                for c in range(NCH):
                    chunks.append(((lambda kt, c=c: h2T[:, kt, c * CH:(c + 1) * CH]), h2B[c], CH,
                                   (lambda f, c=c: (X[:, f, c * CH:(c + 1) * CH], XB[f][c])),
                                   (lambda f: modE[:, 0, 40 + f:40 + f + 1]), c * CH))
                chunks.append(((lambda kt: h2c[:, kt, :]), h2cB, LC, (lambda f: (Xc[:, f, :], XcB[f])),
                               (lambda f: modE[:, 1, 40 + f:40 + f + 1]), 0))
                if not FAST:
                    ffn_pass(ffn_rings(st), I["ffn_w1"].rearrange("(kt p) n -> p kt n", p=128), I["ffn_w3"].rearrange("(kt p) n -> p kt n", p=128),
                             I["ffn_w2"].rearrange("(ft p) n -> p ft n", p=128), chunks)
                P.barrier()
        else:
            for kt in range(KT):
                for c in range(NCH):
                    P.dma("sp", X[:, kt, c * CH:(c + 1) * CH], I["x1T_in"][:, kt, c * CH:(c + 1) * CH], writes=[XB[kt][c]])
                if P2:
                    P.dma("sp", Xc[:, kt, :], I["xc1T_in"][:, kt, :], writes=[XcB[kt]])

        wio = I["w_in_o"].rearrange("(kt p) n -> p kt n", p=128)
        wio2 = WS["w_in_o"].rearrange("(kt p) n -> p kt n", p=128)
        if P1:
            with ExitStack() as st:
                nr = norm_rings(st)
                xb3 = sb("xb3", [128, KT, 4], F32, st)
                xb3B = Buf("xb3")
                hb3 = sb("hb3", [128, KT, 4], BF16, st)
                hb3B = Buf("hb3")
                brec = sb("brec", [128, NB, 3], F32, st)
                brB = Buf("brec")
                for kt in range(KT):
                    dve(lambda e, kt=kt: e.tensor_copy(out=xb3[:, kt, 0:1], in_=X[:, kt, 0:1]), [XB[kt][0]], [xb3B])
                    dve(lambda e, kt=kt: e.tensor_copy(out=xb3[:, kt, 1:3], in_=X[:, kt, T - 2:T]), [XB[kt][3]], [xb3B])
                norm_chunk(nr, (lambda kt: (xb3[:, kt, 0:3], xb3B)), 3, 2, 0, (lambda kt: modO[:, 0, kt:kt + 1]), (lambda kt: (hb3[:, kt, 0:3], hb3B)))
                wr = Ring(nc, st, "wrec", [128, KT, 256], BF16, 2)
                ps, pb = lbank()
                for piece in range(5):
                    wt, wb = wr.next()
                    P.dma("pool", wt[:], wio[:, :, DR + piece * 256:DR + (piece + 1) * 256], writes=[wb])
                    for m in range(2):
                        blk = piece * 2 + m
                        mm(ps[:, blk * 3:blk * 3 + 3], [pb], [(wt[:, kt, m * 128:(m + 1) * 128], hb3[:, kt, 0:3]) for kt in range(KT)], [wb, hb3B], skip_self=True)
                dve(lambda e: e.tensor_copy(out=brec[:].rearrange("p a b -> p (a b)"), in_=ps[:, 0:30]), [pb], [brB])
                if not FUSED:
                    P.dma("sp", O["brec"], brec[:], reads=[brB])
                    for kt in range(KT):
                        P.dma("sp", O["x1T"][:, kt, :], X[:, kt, :], reads=XB[kt])
                        P.dma("sp", O["xc1T"][:, kt, :], Xc[:, kt, :], reads=[XcB[kt]])
                else:
                    c1i, c1o = Buf("cc1i"), Buf("cc1o")
                    P.dma("sp", selm[:], I["selm"], writes=[CB])
                    P.dma("sp", cc1_in, brec[:].rearrange("p a b -> p (a b)"), reads=[brB], writes=[c1i])
                    P.cc((lambda e: e.collective_compute("AllGather", ALU.bypass, replica_groups=[list(range(8))],
                                                         ins=[cc1_in.opt()], outs=[cc1_out.opt()])), reads=[c1i], writes=[c1o])
                    g1 = sb("g1all", [128, 8, NB, 3], F32, st)
                    g1B = Buf("g1all")
                    P.dma("sp", g1[:].rearrange("p r a b -> p r (a b)"), cc1_out.rearrange("(r p) n -> p r n", p=128), reads=[c1o], writes=[g1B])
                    dve(lambda e: e.memset(halo[:], 0.0), [], [haloB])
                    for r in range(8):
                        dve(lambda e, r=r: e.scalar_tensor_tensor(out=halo[:, :, 0:2], in0=g1[:, r, :, 1:3], scalar=selm[:, r:r + 1], in1=halo[:, :, 0:2],
                                                                  op0=ALU.mult, op1=ALU.add), [g1B, haloB, CB], [haloB])
                        dve(lambda e, r=r: e.scalar_tensor_tensor(out=halo[:, :, 2:3], in0=g1[:, r, :, 0:1], scalar=selm[:, 8 + r:9 + r], in1=halo[:, :, 2:3],
                                                                  op0=ALU.mult, op1=ALU.add), [g1B, haloB, CB], [haloB])
                    if dbg:
                        P.dma("sp", O["dbg"][:, 400:430], brec[:].rearrange("p a b -> p (a b)"), reads=[brB])
                        P.dma("sp", O["dbg"][:, 512:752], g1[:].rearrange("p r a b -> p (r a b)"), reads=[g1B])
                        P.dma("sp", O["dbg"][:, 768:820], selm[:], reads=[CB])
                P.barrier()

        if P2 or P3:
            with ExitStack() as st:
                h1T = sb("h1T", [128, KT, T], BF16, st)
                h1B = [Buf("h1_%d" % c) for c in range(NCH)]
                if P2:
                    h1c = sb("h1c", [128, KT, LC], BF16, st)
                    h1cB = Buf("h1c")
                with ExitStack() as stn:
                    nr = norm_rings(stn)
                    for c in range(NCH):
                        norm_chunk(nr, xsrc(c), CH, 2, 0, (lambda kt: modO[:, 0, kt:kt + 1]), (lambda kt, c=c: (h1T[:, kt, c * CH:(c + 1) * CH], h1B[c])))
                    if P2:
                        norm_chunk(nr, xcsrc(), LC, 2, 1, (lambda kt: modO[:, 1, kt:kt + 1]), (lambda kt: (h1c[:, kt, :], h1cB)))
                    P.barrier()
                cw = sb("cw", [128, NB, 4], F32, st)
                cbv = sb("cbv", [128, NB], F32, st)
                lba = sb("lba", [128, 2, NB], F32, st)
                lbx = sb("lbx", [128, 2, NB], F32, st)
                lam = sb("lam", [128, 2, NB], F32, st)
                sc8 = sb("sc8", [128, 2, NB], F32, st)
                sc16 = sb("sc16", [128, 2, NB], F32, st)
                ltmp = sb("ltmp", [128, 2, NB], F32, st)
                for t_, n_ in [(cw, "convw"), (cbv, "convb"), (lba, "lru_ba"), (lbx, "lru_bx"), (lam, "lru_lam")]:
                    P.dma("sp", t_[:], I[n_], writes=[CB])
                if not P1:
                    P.dma("sp", halo[:], I["halo"], writes=[haloB])
                dve(lambda e: e.tensor_scalar(out=ltmp[:], in0=lam[:], scalar1=-1.0, scalar2=None, op0=ALU.mult), [CB], [CB])
                dve(lambda e: e.tensor_tensor(out=ltmp[:], in0=ltmp[:], in1=lam[:], op=ALU.max), [CB], [CB])
                act(lambda e: e.activation(out=ltmp[:], in_=ltmp[:], func=AF.Exp, scale=-1.0), [CB], [CB])
                act(lambda e: e.activation(out=ltmp[:], in_=ltmp[:], func=AF.Ln, bias=1.0, scale=1.0), [CB], [CB])
                dve(lambda e: e.tensor_scalar(out=sc8[:], in0=lam[:], scalar1=-1.0, scalar2=0.0, op0=ALU.mult, op1=ALU.max), [CB], [CB])
                dve(lambda e: e.tensor_tensor(out=sc8[:], in0=sc8[:], in1=ltmp[:], op=ALU.add), [CB], [CB])
                dve(lambda e: e.tensor_scalar(out=sc16[:], in0=sc8[:], scalar1=-16.0, scalar2=None, op0=ALU.mult), [CB], [CB])
                dve(lambda e: e.tensor_scalar(out=sc8[:], in0=sc8[:], scalar1=-8.0, scalar2=None, op0=ALU.mult), [CB], [CB])
                summ = sb("summ", [128, NB, 6], F32, st)
                summB = Buf("summ")
                rsum = sb("rsum", [128, NB, 2, NCH], F32, st)
                rsB = Buf("rsum")
                hin = sb("hin", [128, NB, 2], F32, st)
                hinB = Buf("hin")
                dve(lambda e: e.memset(hin[:], 0.0), [], [hinB])
                dve(lambda e: e.memset(rsum[:], 0.0), [], [rsB])

                def mixer_pass(final):
                    with ExitStack() as st2:
                        wr = Ring(nc, st2, "wrc", [128, KT, 128], BF16, 2)
                        wg = Ring(nc, st2, "wgt", [128, KT, 128], BF16, 2) if final else None
                        lw = Ring(nc, st2, "lw", [128, 4, 128], BF16, 2)
                        wo = Ring(nc, st2, "wo", [128, D], BF16, 2) if final else None
                        recp = sb("recp", [128, T + 3], F32, st2)
                        recpB = Buf("recp")
                        rc = sb("rc", [128, T], F32, st2)
                        rcB = Buf("rc")
                        rcb = sb("rcb", [128, T], BF16, st2)
                        rcbB = Buf("rcb")
                        hs = [sb("hs%d" % d_, [128, T], F32, st2) for d_ in range(2)]
                        hsB = [Buf("hs%d" % d_) for d_ in range(2)]
                        tr_ = Ring(nc, st2, "lt", [128, CH], F32, 16 if final else 20)
                        yb = Ring(nc, st2, "yblk", [128, T], BF16, 2) if final else None
                        if not final:
                            recpc = sb("recpc", [128, LC + 3], F32, st2)
                            rcc = sb("rcc", [128, LC], F32, st2)
                            rccb = sb("rccb", [128, LC], BF16, st2)
                            hsc = sb("hsc", [128, LC], F32, st2)
                            cB_ = Buf("ctxlru")
                            dve(lambda e: e.memset(recpc[:], 0.0), [], [cB_])
                        for blk in range(NB):
                            wt, wb = wr.next()
                            P.dma(WQ, wt[:], wio2[:, :, DR + blk * 128:DR + (blk + 1) * 128], reads=WRD, writes=[wb])
                            lt_, lb_ = lw.next()
                            for d_ in range(2):
                                P.dma(WQ, lt_[:, d_, :], WS["lru_wa"][d_, blk], reads=WRD, writes=[lb_])
                                P.dma(WQ, lt_[:, 2 + d_, :], WS["lru_wx"][d_, blk], reads=WRD, writes=[lb_])
                            if final:
                                gt_, gb_ = wg.next()
                                P.dma(WQ, gt_[:], wio2[:, :, blk * 128:(blk + 1) * 128], reads=WRD, writes=[gb_])
                                wot, wob = wo.next()
                                P.dma(WQ, wot[:], WS["w_out_o"][blk * 128:(blk + 1) * 128, :], reads=WRD, writes=[wob])
                            dve(lambda e, blk=blk: e.tensor_copy(out=recp[:, 0:2], in_=halo[:, blk, 0:2]), [haloB], [recpB])
                            dve(lambda e, blk=blk: e.tensor_copy(out=recp[:, T + 2:T + 3], in_=halo[:, blk, 2:3]), [haloB], [recpB])
                            for c in range(NCH):
                                pr, prb = bank()
                                mm(pr[:], [prb], [(wt[:, kt, :], h1T[:, kt, c * CH:(c + 1) * CH]) for kt in range(KT)], [wb, h1B[c]])
                                act(lambda e, pr=pr, c=c: e.activation(out=recp[:, 2 + c * CH:2 + (c + 1) * CH], in_=pr[:], func=AF.Copy), [prb], [recpB])
                            dve(lambda e, blk=blk: e.tensor_scalar(out=rc[:], in0=recp[:, 0:T], scalar1=cw[:, blk, 0:1], scalar2=cbv[:, blk:blk + 1],
                                                                    op0=ALU.mult, op1=ALU.add), [recpB, CB], [rcB])
                            for j in range(1, 4):
                                dve(lambda e, blk=blk, j=j: e.scalar_tensor_tensor(out=rc[:], in0=recp[:, j:j + T], scalar=cw[:, blk, j:j + 1], in1=rc[:],
                                                                                     op0=ALU.mult, op1=ALU.add), [recpB, rcB, CB], [rcB])
                            pool(lambda e: e.tensor_copy(out=rcb[:], in_=rc[:]), [rcB], [rcbB])
                            seqs = [("lat", rc, rcb, rcB, rcbB, T, hs)]
                            if not final:
                                pr, prb = bank()
                                mm(pr[:, :LC], [prb], [(wt[:, kt, :], h1c[:, kt, :]) for kt in range(KT)], [wb, h1cB])
                                act(lambda e, pr=pr: e.activation(out=recpc[:, 2:2 + LC], in_=pr[:, :LC], func=AF.Copy), [prb], [cB_])
                                dve(lambda e, blk=blk: e.tensor_scalar(out=rcc[:], in0=recpc[:, 0:LC], scalar1=cw[:, blk, 0:1], scalar2=cbv[:, blk:blk + 1],
                                                                        op0=ALU.mult, op1=ALU.add), [cB_, CB], [cB_])
                                for j in range(1, 4):
                                    dve(lambda e, blk=blk, j=j: e.scalar_tensor_tensor(out=rcc[:], in0=recpc[:, j:j + LC], scalar=cw[:, blk, j:j + 1], in1=rcc[:],
                                                                                         op0=ALU.mult, op1=ALU.add), [cB_, CB], [cB_])
                                pool(lambda e: e.tensor_copy(out=rccb[:], in_=rcc[:]), [cB_], [cB_])
                                seqs = [("ctx", rcc, rccb, cB_, cB_, LC, None)] + seqs
                            for kind, r32, rb16, r32B, rb16B, nt, hs_ in seqs:
                                for d_ in range(2):
                                    nch = (nt + CH - 1) // CH
                                    order = list(range(nch)) if d_ == 0 else list(range(nch - 1, -1, -1))
                                    items = []
                                    for oi, c in enumerate(order):
                                        n = min(CH, nt - c * CH)
                                        csl = slice(c * CH, c * CH + n)
                                        pa, pab = bank()
                                        px, pxb = bank()
                                        mm(pa[:, :n], [pab], [(lt_[:, d_, :], rb16[:, csl])], [lb_, rb16B])
                                        mm(px[:, :n], [pxb], [(lt_[:, 2 + d_, :], rb16[:, csl])], [lb_, rb16B])
                                        r_, r_b = tr_.next()
                                        i_, i_b = tr_.next()
                                        a_, a_b = tr_.next()
                                        b_, b_b = tr_.next()
                                        if kind == "lat" and not final:
                                            act(lambda e, r_=r_, pa=pa, n=n, blk=blk, d_=d_, c=c: e.activation(
                                                out=r_[:, :n], in_=pa[:, :n], func=AF.Sigmoid, bias=lba[:, d_, blk:blk + 1], scale=1.0,
                                                accum_out=rsum[:, blk, d_, c:c + 1]), [pab, CB], [r_b, rsB])
                                        else:
                                            act(lambda e, r_=r_, pa=pa, n=n, blk=blk, d_=d_: e.activation(
                                                out=r_[:, :n], in_=pa[:, :n], func=AF.Sigmoid, bias=lba[:, d_, blk:blk + 1], scale=1.0), [pab, CB], [r_b])
                                        act(lambda e, i_=i_, px=px, n=n, blk=blk, d_=d_: e.activation(
                                            out=i_[:, :n], in_=px[:, :n], func=AF.Sigmoid, bias=lbx[:, d_, blk:blk + 1], scale=1.0), [pxb, CB], [i_b])
                                        items.append((oi, c, n, csl, r_, r_b, i_, i_b, a_, a_b, b_, b_b))
                                    for (oi, c, n, csl, r_, r_b, i_, i_b, a_, a_b, b_, b_b) in items:
                                        act(lambda e, a_=a_, r_=r_, n=n, blk=blk, d_=d_: e.activation(
                                            out=a_[:, :n], in_=r_[:, :n], func=AF.Exp, scale=sc8[:, d_, blk:blk + 1]), [r_b, CB], [a_b])
                                        act(lambda e, b_=b_, r_=r_, n=n, blk=blk, d_=d_: e.activation(
                                            out=b_[:, :n], in_=r_[:, :n], func=AF.Exp, scale=sc16[:, d_, blk:blk + 1]), [r_b, CB], [b_b])
                                    for (oi, c, n, csl, r_, r_b, i_, i_b, a_, a_b, b_, b_b) in items:
                                        act(lambda e, b_=b_, n=n: e.activation(out=b_[:, :n], in_=b_[:, :n], func=AF.Sqrt, scale=-1.0, bias=1.0), [b_b], [b_b])
                                    for (oi, c, n, csl, r_, r_b, i_, i_b, a_, a_b, b_, b_b) in items:
                                        dve(lambda e, b_=b_, i_=i_, n=n: e.tensor_tensor(out=b_[:, :n], in0=b_[:, :n], in1=i_[:, :n], op=ALU.mult), [b_b, i_b], [b_b])
                                        pool(lambda e, b_=b_, r32=r32, csl=csl, n=n: e.tensor_tensor(out=b_[:, :n], in0=b_[:, :n], in1=r32[:, csl], op=ALU.mult),
                                             [b_b, r32B], [b_b])
                                        if kind == "ctx":
                                            ho, hoB = hsc, cB_
                                            init = 0.0
                                            initB = []
                                        else:
                                            ho, hoB = hs_[d_], hsB[d_]
                                            if oi == 0:
                                                init = hin[:, blk, d_:d_ + 1]
                                                initB = [hinB]
                                            elif d_ == 0:
                                                init = ho[:, c * CH - 1:c * CH]
                                                initB = []
                                            else:
                                                init = ho[:, (c + 1) * CH:(c + 1) * CH + 1]
                                                initB = []
                                        if d_ == 0:
                                            dve(lambda e, ho=ho, a_=a_, b_=b_, csl=csl, n=n, init=init: e.tensor_tensor_scan(
                                                out=ho[:, csl], data0=a_[:, :n], data1=b_[:, :n], initial=init, op0=ALU.mult, op1=ALU.add),
                                                [a_b, b_b, hoB] + initB, [hoB])
                                        else:
                                            lo = c * CH
                                            dve(lambda e, ho=ho, a_=a_, b_=b_, lo=lo, n=n, init=init: e.tensor_tensor_scan(
                                                out=ho[:, lo:lo + n][:, ::-1], data0=a_[:, 0:n][:, ::-1], data1=b_[:, 0:n][:, ::-1], initial=init,
                                                op0=ALU.mult, op1=ALU.add), [a_b, b_b, hoB] + initB, [hoB])
                                    if not final:
                                        if kind == "ctx":
                                            src_ = hsc[:, LC - 1:LC] if d_ == 0 else hsc[:, 0:1]
                                            dve(lambda e, blk=blk, d_=d_, src_=src_: e.tensor_copy(out=summ[:, blk, 4 + d_:5 + d_], in_=src_), [cB_], [summB])
                                        else:
                                            src_ = hs_[0][:, T - 1:T] if d_ == 0 else hs_[1][:, 0:1]
                                            dve(lambda e, blk=blk, d_=d_, src_=src_: e.tensor_copy(out=summ[:, blk, 2 * d_ + 1:2 * d_ + 2], in_=src_), [hsB[d_]], [summB])
                            if final:
                                yt, ytb = yb.next()
                                for c in range(NCH):
                                    csl = slice(c * CH, (c + 1) * CH)
                                    pg, pgb = bank()
                                    mm(pg[:], [pgb], [(gt_[:, kt, :], h1T[:, kt, csl]) for kt in range(KT)], [gb_, h1B[c]])
                                    g_, g_b = tr_.next()
                                    act(lambda e, g_=g_, pg=pg: e.activation(out=g_[:], in_=pg[:], func=AF.Gelu_apprx_tanh), [pgb], [g_b])
                                    s_, s_b = tr_.next()
                                    dve(lambda e, s_=s_, csl=csl: e.tensor_tensor(out=s_[:], in0=hs[0][:, csl], in1=hs[1][:, csl], op=ALU.add), hsB, [s_b])
                                    pool(lambda e, yt=yt, g_=g_, s_=s_, csl=csl: e.tensor_tensor(out=yt[:, csl], in0=g_[:], in1=s_[:], op=ALU.mult), [g_b, s_b], [ytb])
                                for c in range(NCH):
                                    csl = slice(c * CH, (c + 1) * CH)
                                    for f in range(KT):
                                        py, pyb = bank()
                                        mm(py[:], [pyb], [(wot[:, f * 128:(f + 1) * 128], yt[:, csl])], [wob, ytb])
                                        dve(lambda e, py=py, f=f, csl=csl: e.scalar_tensor_tensor(out=X[:, f, csl], in0=py[:], scalar=modO[:, 0, 16 + f:16 + f + 1],
                                                                                                   in1=X[:, f, csl], op0=ALU.mult, op1=ALU.add), [pyb, XB[f][c], CB], [XB[f][c]])
                        if not final:
                            for d_ in range(2):
                                dve(lambda e, d_=d_: e.tensor_reduce(out=summ[:, :, 2 * d_], in_=rsum[:, :, d_, :], axis=AX.X, op=ALU.add), [rsB], [summB])
                                dve(lambda e, d_=d_: e.tensor_tensor(out=summ[:, :, 2 * d_], in0=summ[:, :, 2 * d_], in1=sc8[:, d_, :], op=ALU.mult), [summB, CB], [summB])
                                act(lambda e, d_=d_: e.activation(out=summ[:, :, 2 * d_], in_=summ[:, :, 2 * d_], func=AF.Exp), [summB], [summB])
                        P.barrier()

                if P2:
                    mixer_pass(False)
                    if last == 2:
                        P.dma("sp", O["summ"], summ[:], reads=[summB])
                        P.barrier()
                if P3:
                    selt = sb("selt", [128, 4], F32, st)
                    cfw = sb("cfw", [128, 4, NB], F32, st)
                    cbw = sb("cbw", [128, 4, NB], F32, st)
                    P.dma("sp", selt[:], I["sel"], writes=[CB])
                    if not FUSED:
                        P.dma("sp", sa[:], I["summ_all"], writes=[CB])
                    else:
                        c2i, c2o = Buf("cc2i"), Buf("cc2o")
                        P.dma("sp", cc2_in, summ[:].rearrange("p a b -> p (a b)"), reads=[summB], writes=[c2i])
                        P.cc((lambda e: e.collective_compute("AllGather", ALU.bypass, replica_groups=[list(range(8))],
                                                             ins=[cc2_in.opt()], outs=[cc2_out.opt()])), reads=[c2i], writes=[c2o])
                        g2a = sb("g2all", [128, 8, NB * 6], F32, st)
                        g2B = Buf("g2all")
                        P.dma("sp", g2a[:], cc2_out.rearrange("(r p) n -> p r n", p=128), reads=[c2o], writes=[g2B])
                        dve(lambda e: e.memset(sa[:], 0.0), [], [CB])
                        for j in range(4):
                            for r in range(8):
                                dve(lambda e, j=j, r=r: e.scalar_tensor_tensor(
                                    out=sa[:, j].rearrange("p a b -> p (a b)"), in0=g2a[:, r, :], scalar=selm[:, 16 + j * 8 + r:17 + j * 8 + r],
                                    in1=sa[:, j].rearrange("p a b -> p (a b)"), op0=ALU.mult, op1=ALU.add), [g2B, CB], [CB])
                    dve(lambda e: e.tensor_copy(out=cfw[:, 0, :], in_=sa[:, 0, :, 4]), [CB], [CB])
                    for j in range(3):
                        dve(lambda e, j=j: e.tensor_tensor(out=cfw[:, j + 1, :], in0=sa[:, j, :, 0], in1=cfw[:, j, :], op=ALU.mult), [CB], [CB])
                        dve(lambda e, j=j: e.tensor_tensor(out=cfw[:, j + 1, :], in0=cfw[:, j + 1, :], in1=sa[:, j, :, 1], op=ALU.add), [CB], [CB])
                    dve(lambda e: e.tensor_copy(out=cbw[:, 3, :], in_=sa[:, 0, :, 5]), [CB], [CB])
                    for j in range(3, 0, -1):
                        dve(lambda e, j=j: e.tensor_tensor(out=cbw[:, j - 1, :], in0=sa[:, j, :, 2], in1=cbw[:, j, :], op=ALU.mult), [CB], [CB])
                        dve(lambda e, j=j: e.tensor_tensor(out=cbw[:, j - 1, :], in0=cbw[:, j - 1, :], in1=sa[:, j, :, 3], op=ALU.add), [CB], [CB])
                    for d_, cc in enumerate([cfw, cbw]):
                        dve(lambda e, d_=d_, cc=cc: e.tensor_scalar(out=hin[:, :, d_], in0=cc[:, 0, :], scalar1=selt[:, 0:1], scalar2=None, op0=ALU.mult), [CB, hinB], [hinB])
                        for j in range(1, 4):
                            dve(lambda e, d_=d_, cc=cc, j=j: e.scalar_tensor_tensor(out=hin[:, :, d_], in0=cc[:, j, :], scalar=selt[:, j:j + 1], in1=hin[:, :, d_],
                                                                                     op0=ALU.mult, op1=ALU.add), [CB, hinB], [hinB])
                    if dbg:
                        P.dma("sp", O["dbg"][:, 0:30], halo[:].rearrange("p a b -> p (a b)"), reads=[haloB])
                        P.dma("sp", O["dbg"][:, 32:32 + 240], sa[:].rearrange("p j a b -> p (j a b)"), reads=[CB])
                        P.dma("sp", O["dbg"][:, 272:292], hin[:].rearrange("p a b -> p (a b)"), reads=[hinB])
                        P.dma("sp", O["dbg"][:, 292:352], summ[:].rearrange("p a b -> p (a b)"), reads=[summB])
                    mixer_pass(True)
                P.barrier()

        if P3:
            with ExitStack() as st:
                h2T = sb("h2Tm", [128, KT, T], BF16, st)
                h2B = [Buf("h2m_%d" % c) for c in range(NCH)]
                gates = sb("gates", [128, T // 128, 8], F32, st)
                gatesB = Buf("gates")
                with ExitStack() as st2:
                    nr = norm_rings(st2)
                    h32 = sb("h32", [128, KT, CH], F32, st2)
                    h32B = Buf("h32")
                    rw = sb("rw", [128, KT, 8], F32, st2)
                    P.dma("sp", rw[:], I["router_w"], writes=[CB])
                    gr = Ring(nc, st2, "gr", [128, 40], F32, 2)
                    for c in range(NCH):
                        norm_chunk(nr, xsrc(c), CH, 3, 0, (lambda kt: modO[:, 0, 24 + kt:24 + kt + 1]),
                                   (lambda kt, c=c: (h2T[:, kt, c * CH:(c + 1) * CH], h2B[c])), dst32=(lambda kt: (h32[:, kt, :], h32B)))
                        for tt in range(4):
                            ti = c * 4 + tt
                            pl, plb = bank()
                            mm(pl[:, 0:8], [plb], [(h32[:, kt, tt * 128:(tt + 1) * 128], rw[:, kt, :]) for kt in range(KT)], [h32B, CB])
                            g_, g_b = gr.next()
                            dve(lambda e, g_=g_, pl=pl: e.tensor_copy(out=g_[:, 0:8], in_=pl[:, 0:8]), [plb], [g_b])
                            dve(lambda e, g_=g_: e.reduce_max(out=g_[:, 8:9], in_=g_[:, 0:8], axis=AX.X), [g_b], [g_b])
                            dve(lambda e, g_=g_: e.tensor_scalar(out=g_[:, 16:24], in0=g_[:, 0:8], scalar1=g_[:, 8:9], scalar2=NEG, op0=ALU.is_equal, op1=ALU.mult), [g_b], [g_b])
                            dve(lambda e, g_=g_: e.tensor_tensor(out=g_[:, 24:32], in0=g_[:, 16:24], in1=g_[:, 0:8], op=ALU.add), [g_b], [g_b])
                            dve(lambda e, g_=g_: e.reduce_max(out=g_[:, 9:10], in_=g_[:, 24:32], axis=AX.X), [g_b], [g_b])
                            dve(lambda e, g_=g_: e.tensor_scalar(out=g_[:, 10:11], in0=g_[:, 8:9], scalar1=-1.0, scalar2=None, op0=ALU.mult), [g_b], [g_b])
                            act(lambda e, g_=g_: e.activation(out=g_[:, 16:24], in_=g_[:, 0:8], func=AF.Exp, bias=g_[:, 10:11], scale=1.0), [g_b], [g_b])
                            dve(lambda e, g_=g_: e.tensor_scalar(out=g_[:, 24:32], in0=g_[:, 0:8], scalar1=g_[:, 9:10], scalar2=None, op0=ALU.is_ge), [g_b], [g_b])
                            dve(lambda e, g_=g_: e.tensor_tensor(out=g_[:, 16:24], in0=g_[:, 16:24], in1=g_[:, 24:32], op=ALU.mult), [g_b], [g_b])
                            dve(lambda e, g_=g_: e.reduce_sum(out=g_[:, 11:12], in_=g_[:, 16:24], axis=AX.X), [g_b], [g_b])
                            dve(lambda e, g_=g_: e.reciprocal(out=g_[:, 11:12], in_=g_[:, 11:12]), [g_b], [g_b])
                            dve(lambda e, g_=g_, ti=ti: e.tensor_scalar(out=gates[:, ti, :], in0=g_[:, 16:24], scalar1=g_[:, 11:12], scalar2=None, op0=ALU.mult), [g_b], [gatesB])
                    P.barrier()
                fr = ffn_rings(st)
                gbr = Ring(nc, st, "gbc", [128, T], F32, 2)
                ger = Ring(nc, st, "Ge", [128, 128], F32, 3)
                for ex in range(0 if FAST else 8):
                    gt, gtb = gbr.next()
                    for c in range(NCH):
                        pgb_, pgbb = lbank()
                        for tt in range(4):
                            ti = c * 4 + tt
                            ge, geb = ger.next()
                            dve(lambda e, ge=ge, ti=ti, ex=ex: e.tensor_scalar(out=ge[:], in0=ones32[:], scalar1=gates[:, ti, ex:ex + 1], scalar2=None, op0=ALU.mult),
                                [gatesB, CB], [geb])
                            mm(pgb_[:, tt * 128:(tt + 1) * 128], [pgbb], [(ge[:], ident32[:])], [geb, CB], skip_self=True)
                        act(lambda e, gt=gt, pgb_=pgb_, c=c: e.activation(out=gt[:, c * CH:(c + 1) * CH], in_=pgb_[:], func=AF.Copy), [pgbb], [gtb])
                    chunks = []
                    for c in range(NCH):
                        chunks.append(((lambda kt, c=c: h2T[:, kt, c * CH:(c + 1) * CH]), h2B[c], CH,
                                       (lambda f, c=c: (X[:, f, c * CH:(c + 1) * CH], XB[f][c])),
                                       (lambda f: modO[:, 0, 40 + f:40 + f + 1]), c * CH))
                    ffn_pass(fr, WS["moe_w1"][ex].rearrange("(kt p) n -> p kt n", p=128), WS["moe_w3"][ex].rearrange("(kt p) n -> p kt n", p=128),
                             WS["moe_w2"][ex].rearrange("(ft p) n -> p ft n", p=128), chunks, gbc=(gt, gtb), wq=WQ, wrd=WRD)
                P.barrier()
            with ExitStack() as st:
                nr = norm_rings(st)
                orr = Ring(nc, st, "ot", [128, CH], F32, 4)
                for c in range(NCH):
                    sqr, rsr, tmr = nr
                    ps, pb = lbank()
                    for kt in range(KT):
                        sq, sqb = sqr.next()
                        act(lambda e, sq=sq, kt=kt, c=c: e.activation(out=sq[:], in_=X[:, kt, c * CH:(c + 1) * CH], func=AF.Square), [XB[kt][c]], [sqb])
                        P.op("pe", (lambda e, ps=ps, sq=sq, kt=kt: e.matmul(ps[:], lhsT=onesb[:], rhs=sq[:], start=(kt == 0), stop=(kt == KT - 1))),
                             [sqb, CB], [pb], skip_self=(kt > 0))
                    rs, rsb = rsr.next()
                    act(lambda e, rs=rs, ps=ps: e.activation(out=rs[:], in_=ps[:], func=AF.Sqrt, scale=1.0 / D, bias=EPS), [pb], [rsb])
                    dve(lambda e, rs=rs: e.reciprocal(out=rs[:], in_=rs[:]), [rsb], [rsb])
                    for kt in range(KT):
                        ot, otb = orr.next()
                        dve(lambda e, ot=ot, kt=kt, c=c, rs=rs: e.scalar_tensor_tensor(out=ot[:], in0=X[:, kt, c * CH:(c + 1) * CH], scalar=vecs[:, 4, kt:kt + 1],
                                                                                       in1=rs[:], op0=ALU.mult, op1=ALU.mult), [XB[kt][c], rsb, CB], [otb])
                        P.dma("sp", O["outT"][:, kt, c * CH:(c + 1) * CH], ot[:], reads=[otb])
                P.barrier()
        P.barrier(final=True)
        P.emit(block)
    return nc


def _fm(v):
    v = np.asarray(v, np.float32)
    return np.ascontiguousarray(v.reshape(-1, 128).T)


def _tokT(a):
    a = np.asarray(a, np.float32)
    return np.ascontiguousarray(a.T.reshape(KT, 128, a.shape[0]).transpose(1, 0, 2))


def _rope_tables(t0):
    pos = np.arange(t0 - 128, t0 - 128 + TE)
    row = (pos // 64).astype(np.float32)
    col = (pos % 64).astype(np.float32)
    freqs = (np.float32(10000.0) ** (-np.arange(16, dtype=np.float32) / np.float32(16))).astype(np.float32)
    out = np.zeros((128, 2, TE), np.float32)
    for p in range(128):
        d = p % 64
        axis, half, f = d // 32, (d % 32) // 16, d % 16
        ang = ((row if axis == 0 else col) * freqs[f]).astype(np.float32)
        out[p, 0] = np.cos(ang)
        out[p, 1] = np.sin(ang) * (-1.0 if half == 0 else 1.0)
    return out


def _masks(s):
    q = np.arange(128)[:, None]
    koff = np.arange(384)[None, :] - 128
    band = np.abs(koff - q) <= 128
    m = np.zeros((128, 3, 384), np.float32)
    for i in range(3):
        v = band.copy()
        if i == 0 and s == 0:
            v &= (koff >= 0)
        if i == 2 and s == 3:
            v &= (koff < 128)
        m[:, i, :] = np.where(v, 0.0, NEG)
    return m


def _swap_idx():
    d = np.arange(64)
    axis, half, f = d // 32, (d % 32) // 16, d % 16
    return axis * 32 + (1 - half) * 16 + f


_CACHE = {}
_LAST = {}


DBG = False


def _prog(phases):
    key = tuple(phases)
    if key not in _CACHE:
        _CACHE[key] = build(list(phases), dbg=DBG)
    return _CACHE[key]


def _common(inp, b):
    f32 = lambda a: np.ascontiguousarray(np.asarray(a, np.float32))
    ccol = np.stack([inp["c"][b], inp["c_ctx"]], 0)
    ccol = np.ascontiguousarray(ccol.reshape(2, KT, 128).transpose(2, 1, 0))
    vecs = np.stack([_fm(inp["norm1_e"][0]), _fm(inp["norm2_e"][0]), _fm(inp["norm1_o"][0]), _fm(inp["norm2_o"][0]), _fm(inp["final_norm"])], 1)
    return {"ccol": f32(ccol), "vecs": f32(vecs), "ident": np.eye(128, dtype=np.float32),
            "ada_w_o": f32(inp["ada_w_o"][0]), "ada_b_o": _fm(inp["ada_b_o"][0]), "w_in_o": f32(inp["w_in_o"][0])}


def _lru_common(inp):
    f32 = lambda a: np.ascontiguousarray(np.asarray(a, np.float32))
    cw = np.asarray(inp["conv_w"][0], np.float32)
    convw = np.ascontiguousarray(cw.reshape(4, NB, 128).transpose(2, 1, 0))
    def d2(a):
        return np.ascontiguousarray(np.asarray(a, np.float32).reshape(2, NB, 128).transpose(2, 0, 1))
    return {"convw": convw, "convb": _fm(inp["conv_b"][0]), "lru_wa": f32(inp["lru_wa"][0]), "lru_wx": f32(inp["lru_wx"][0]),
            "lru_ba": d2(inp["lru_ba"][0]), "lru_bx": d2(inp["lru_bx"][0]), "lru_lam": d2(inp["lru_lambda"][0])}


def _maps1(inp):
    f32 = lambda a: np.ascontiguousarray(np.asarray(a, np.float32))
    x = np.asarray(inp["x"], np.float32)
    S = x.shape[1]
    w_in = np.asarray(inp["w_in_e"][0], np.float32)
    u, v, q, k, val = w_in[:, 0:512], w_in[:, 512:1024], w_in[:, 1024:1536], w_in[:, 1536:1664], w_in[:, 1664:1792]
    sw = _swap_idx()
    qp = np.concatenate([np.concatenate([q[:, j * 64:(j + 1) * 64], q[:, (j + 4) * 64:(j + 5) * 64]], 1) for j in range(4)], 1)
    qps = np.concatenate([np.concatenate([q[:, j * 64:(j + 1) * 64][:, sw], q[:, (j + 4) * 64:(j + 5) * 64][:, sw]], 1) for j in range(4)], 1)
    ks = np.concatenate([k[:, 0:64][:, sw], k[:, 64:128][:, sw]], 1)
    w_in_ext = f32(np.concatenate([u, v, qp, qps, k, ks, val], 1))
    wo = np.asarray(inp["w_out_e"][0], np.float32)
    rows = list(range(512))
    for j in range(4):
        rows += list(range(512 + j * 64, 512 + (j + 1) * 64)) + list(range(512 + (j + 4) * 64, 512 + (j + 5) * 64))
    w_out_p = f32(wo[rows, :])
    wsT = f32(np.asarray(inp["sgu_w"][0], np.float32).transpose(2, 0, 1))
    bsb = f32(np.broadcast_to(np.asarray(inp["sgu_b"][0], np.float32).reshape(1, 512), (128, 512)))
    sinkb = f32(np.broadcast_to(np.asarray(inp["attn_sink"][0], np.float32).reshape(1, 8), (128, 8)))
    shared = {"ada_w_e": f32(inp["ada_w_e"][0]), "ada_b_e": _fm(inp["ada_b_e"][0]), "w_in_e": w_in_ext, "wsT": wsT, "bsb": bsb,
              "sinkb": sinkb, "w_out_e": w_out_p, "ffn_w1": f32(inp["ffn_w1"][0]), "ffn_w3": f32(inp["ffn_w3"][0]), "ffn_w2": f32(inp["ffn_w2"][0])}
    maps = []
    for c in range(8):
        b, s = c // 4, c % 4
        t0 = s * T
        xh = np.zeros((256, D), np.float32)
        if s > 0:
            xh[0:128] = x[b, t0 - 128:t0]
        if t0 + T < S:
            xh[128:256] = x[b, t0 + T:t0 + T + 128]
        m = dict(shared)
        m.update(_common(inp, b))
        m.update({"xT": _tokT(x[b, t0:t0 + T]), "xhT": _tokT(xh), "ctxT": _tokT(inp["ctx"][b]), "ropeCS": _rope_tables(t0), "masks": _masks(s)})
        maps.append(m)
    return maps


def kernel_unfused(**inp):
    inp = {k: np.asarray(v) for k, v in inp.items()}
    cores = list(range(8))
    r1 = run_bass_kernel_spmd(_prog([1]), _maps1(inp), core_ids=cores).results
    lru = _lru_common(inp)
    halos = []
    for c in range(8):
        b, s = c // 4, c % 4
        h = np.zeros((128, NB, 3), np.float32)
        if s > 0:
            h[:, :, 0:2] = r1[c - 1]["brec"][:, :, 1:3]
        if s < 3:
            h[:, :, 2] = r1[c + 1]["brec"][:, :, 0]
        halos.append(h)
    maps2 = []
    for c in range(8):
        m = dict(lru)
        m.update(_common(inp, c // 4))
        m.update({"x1T_in": r1[c]["x1T"], "xc1T_in": r1[c]["xc1T"], "halo": halos[c]})
        maps2.append(m)
    r2 = run_bass_kernel_spmd(_prog([2]), maps2, core_ids=cores).results
    f32 = lambda a: np.ascontiguousarray(np.asarray(a, np.float32))
    rw = np.ascontiguousarray(np.asarray(inp["router_w"][0], np.float32).reshape(KT, 128, 8).transpose(1, 0, 2))
    shared3 = {"w_out_o": f32(inp["w_out_o"][0]), "router_w": rw, "moe_w1": f32(inp["moe_w1"][0]), "moe_w3": f32(inp["moe_w3"][0]),
               "moe_w2": f32(inp["moe_w2"][0])}
    maps3 = []
    for c in range(8):
        b, s = c // 4, c % 4
        m = dict(lru)
        m.update(shared3)
        m.update(_common(inp, b))
        sel = np.zeros((128, 4), np.float32)
        sel[:, s] = 1.0
        sa = np.ascontiguousarray(np.stack([r2[4 * b + j]["summ"] for j in range(4)], 1))
        m.update({"x1T_in": r1[c]["x1T"], "halo": halos[c], "sel": sel, "summ_all": sa})
        maps3.append(m)
    r3 = run_bass_kernel_spmd(_prog([3]), maps3, core_ids=cores).results
    out = np.zeros((2, 4 * T, D), np.float32)
    for c in range(8):
        b, s = c // 4, c % 4
        out[b, s * T:(s + 1) * T, :] = r3[c]["outT"].transpose(2, 1, 0).reshape(T, D)
    return out


def kernel_fused(**inp):
    inp = {k: np.asarray(v) for k, v in inp.items()}
    f32 = lambda a: np.ascontiguousarray(np.asarray(a, np.float32))
    maps = _maps1(inp)
    lru = _lru_common(inp)
    rw = np.ascontiguousarray(np.asarray(inp["router_w"][0], np.float32).reshape(KT, 128, 8).transpose(1, 0, 2))
    shared3 = {"w_out_o": f32(inp["w_out_o"][0]), "router_w": rw, "moe_w1": f32(inp["moe_w1"][0]), "moe_w3": f32(inp["moe_w3"][0]),
               "moe_w2": f32(inp["moe_w2"][0])}
    for c in range(8):
        b, s = c // 4, c % 4
        m = maps[c]
        m.update(lru)
        m.update(shared3)
        sel = np.zeros((128, 4), np.float32)
        sel[:, s] = 1.0
        selm = np.zeros((128, 52), np.float32)
        if s > 0:
            selm[:, c - 1] = 1.0
        if s < 3:
            selm[:, 8 + c + 1] = 1.0
        for j in range(4):
            selm[:, 16 + j * 8 + 4 * b + j] = 1.0
        m.update({"sel": sel, "selm": selm})
    r = run_bass_kernel_spmd(_prog([1, 2, 3]), maps, core_ids=list(range(8))).results
    _LAST["r"] = r
    out = np.zeros((2, 4 * T, D), np.float32)
    for c in range(8):
        b, s = c // 4, c % 4
        out[b, s * T:(s + 1) * T, :] = r[c]["outT"].transpose(2, 1, 0).reshape(T, D)
    return out


FUSED_MODE = False


def kernel(**inp):
    return kernel_fused(**inp) if FUSED_MODE else kernel_unfused(**inp)
```

```python
import numpy as np
import concourse.bass as bass
import concourse.mybir as mybir
from concourse.bass_utils import run_bass_kernel_spmd
from contextlib import ExitStack
import types

F32 = mybir.dt.float32
BF16 = mybir.dt.bfloat16
AF = mybir.ActivationFunctionType
ALU = mybir.AluOpType
AX = mybir.AxisListType

D = 1024
KT = 8
T = 2048
CH = 512
MC = 256
NCH = 4
LC = 256
DFF = 2816
NFF = 22
DR = 1280
NB = 10
EPS = 1e-6
SCALE = 0.125
NEG = -1e30
TE = T + 256


def _freeze(fn):
    if fn is None or fn.__closure__ is None:
        return fn
    cells = []
    for c in fn.__closure__:
        try:
            cells.append(types.CellType(c.cell_contents))
        except ValueError:
            cells.append(c)
    return types.FunctionType(fn.__code__, fn.__globals__, fn.__name__, fn.__defaults__, tuple(cells))


class Buf:
    __slots__ = ("w", "r", "name")

    def __init__(self, name=""):
        self.w = None
        self.r = {}
        self.name = name


class Prog:
    ENG = ["pe", "act", "dve", "pool", "sp"]

    def __init__(self, nc, es, n_dsem=48):
        self.nc = nc
        self.es = es
        self.LIMIT = 30000
        self.epoch = {e: 0 for e in self.ENG}
        self.sems = {e + "#0": es.enter_context(nc.semaphore("s_" + e)) for e in self.ENG}
        self.dsems = [es.enter_context(nc.semaphore("d%d" % i)) for i in range(n_dsem)]
        self.dcnt = [0] * n_dsem
        self.dnext = {"sw": 0, "hw": 0}
        self.cnt = {e: 0 for e in self.ENG}
        self.seen = {e: {} for e in self.ENG}
        self.q = {e: [] for e in self.ENG}
        self.ccsem = es.enter_context(nc.semaphore("ccsem"))
        self.cccnt = 0
        self.pcsem = es.enter_context(nc.semaphore("pcsem"))
        self.pccnt = 0

    def _semh(self, k):
        if k[0] == "c":
            return self.ccsem
        if k[0] == "p":
            return self.pcsem
        return self.sems[k[1]] if k[0] == "e" else self.dsems[k[1]]

    def cc(self, fn, reads=(), writes=()):
        waits = self._waits("pool", reads, writes)
        self.cccnt += 1
        tok = (("c", 0), self.cccnt)
        self.q["pool"].append((waits, _freeze(fn), ("c", 0)))
        self._mark(tok, reads, writes)
        return tok

    def _waits(self, eng, reads, writes, skip_self=False):
        need = {}

        def add(k, v):
            if skip_self and k[0] == "e" and k[1].split("#")[0] == eng:
                return
            if need.get(k, 0) < v:
                need[k] = v
        for b in reads:
            if b.w is not None:
                add(*b.w)
        for b in writes:
            if b.w is not None:
                add(*b.w)
            for k, v in b.r.items():
                add(k, v)
        out = []
        seen = self.seen[eng]
        for k, v in need.items():
            if seen.get(k, 0) < v:
                seen[k] = v
                out.append((k, v))
        return out

    def _mark(self, tok, reads, writes):
        k, v = tok
        for b in reads:
            if b.r.get(k, 0) < v:
                b.r[k] = v
        for b in writes:
            b.w = tok
            b.r = {}

    def op(self, eng, fn, reads=(), writes=(), skip_self=False):
        waits = self._waits(eng, reads, writes, skip_self)
        if self.cnt[eng] >= self.LIMIT:
            self.epoch[eng] += 1
            self.cnt[eng] = 0
            key = eng + "#%d" % self.epoch[eng]
            self.sems[key] = self.es.enter_context(self.nc.semaphore("s_%s_%d" % (eng, self.epoch[eng])))
        key = eng + "#%d" % self.epoch[eng]
        self.cnt[eng] += 1
        tok = (("e", key), self.cnt[eng])
        self.q[eng].append((waits, _freeze(fn), ("e", key)))
        self._mark(tok, reads, writes)
        return tok

    def dma(self, eng, out, in_, reads=(), writes=(), **kw):
        half = len(self.dsems) // 2
        cls = "sw" if eng == "pool" else "hw"
        i = self.dnext[cls] + (0 if cls == "sw" else half)
        self.dnext[cls] = (self.dnext[cls] + 1) % half
        waits = self._waits(eng, reads, writes)
        if self.dcnt[i] > 0:
            k = ("d", i)
            v = self.dcnt[i]
            if self.seen[eng].get(k, 0) < v:
                self.seen[eng][k] = v
                waits.append((k, v))
        self.dcnt[i] += 16
        tok = (("d", i), self.dcnt[i])
        self.q[eng].append((waits, (lambda e: e.dma_start(out=out, in_=in_, **kw)), ("d", i)))
        self._mark(tok, reads, writes)
        return tok

    def dma_bulk(self, eng, out, in_):
        self.pccnt += 16
        self.q[eng].append(([], (lambda e: e.dma_start(out=out, in_=in_)), ("p", 0)))

    def bulk_done(self, bufs):
        for b in bufs:
            b.w = (("p", 0), self.pccnt)
            b.r = {}

    def barrier(self, final=False):
        toks = [(("e", e + "#%d" % self.epoch[e]), self.cnt[e]) for e in self.ENG if self.cnt[e] > 0]
        toks += [(("d", i), c) for i, c in enumerate(self.dcnt) if c > 0]
        if self.cccnt > 0:
            toks.append((("c", 0), self.cccnt))
        if final and self.pccnt > 0:
            toks.append((("p", 0), self.pccnt))
        for e in self.ENG:
            waits = []
            for k, v in toks:
                if self.seen[e].get(k, 0) < v:
                    self.seen[e][k] = v
                    waits.append((k, v))
            if waits:
                self.q[e].append((waits, None, None))

    def emit(self, block):
        engobj = {"pe": "tensor", "act": "scalar", "dve": "vector", "pool": "gpsimd", "sp": "sync"}

        def make(ename):
            items = self.q[ename]

            def body(e):
                for waits, fn, inc in items:
                    for k, v in waits:
                        e.wait_ge(self._semh(k), v)
                    if fn is None:
                        continue
                    ins = fn(e)
                    if inc[0] == "e":
                        ins.then_inc(self.sems[inc[1]], 1)
                    elif inc[0] == "c":
                        ins.then_inc(self.ccsem, 1)
                    elif inc[0] == "p":
                        ins.then_inc(self.pcsem, 16)
                    else:
                        ins.then_inc(self.dsems[inc[1]], 16)
            return body
        for ename in self.ENG:
            getattr(block, engobj[ename])(make(ename))


class Ring:
    uid = 0

    def __init__(self, nc, es, name, shape, dtype, n):
        Ring.uid += 1
        self.tiles = [es.enter_context(nc.sbuf_tensor("%s%d_%d" % (name, i, Ring.uid), list(shape), dtype)) for i in range(n)]
        self.bufs = [Buf("%s%d" % (name, i)) for i in range(n)]
        self.i = 0

    def next(self):
        j = self.i % len(self.tiles)
        self.i += 1
        return self.tiles[j], self.bufs[j]


FAST = False


def build(phases, dbg=False):
    nc = bass.Bass("TRN2", target_bir_lowering=False)

    def din(name, shape):
        return nc.dram_tensor(name, list(shape), F32, kind="ExternalInput").ap()

    def dout(name, shape):
        return nc.dram_tensor(name, list(shape), F32, kind="ExternalOutput").ap()

    P1, P2, P3 = (1 in phases), (2 in phases), (3 in phases)
    first = min(phases)
    last = max(phases)
    I = {}
    I["ccol"] = din("ccol", [128, KT, 2])
    I["vecs"] = din("vecs", [128, 5, KT])
    I["ident"] = din("ident", [128, 128])
    if P1:
        for n_, s_ in [("xT", [128, KT, T]), ("xhT", [128, KT, 256]), ("ctxT", [128, KT, LC]), ("ada_w_e", [D, 6 * D]),
                       ("ada_b_e", [128, 48]), ("w_in_e", [D, 2432]), ("wsT", [128, 4, 128]), ("bsb", [128, 512]),
                       ("sinkb", [128, 8]), ("w_out_e", [D, D]), ("ffn_w1", [D, DFF]), ("ffn_w3", [D, DFF]),
                       ("ffn_w2", [DFF, D]), ("ropeCS", [128, 2, TE]), ("masks", [128, 3, 384])]:
            I[n_] = din(n_, s_)
    I["ada_w_o"] = din("ada_w_o", [D, 6 * D])
    I["ada_b_o"] = din("ada_b_o", [128, 48])
    I["w_in_o"] = din("w_in_o", [D, 2 * DR])
    if first > 1:
        I["x1T_in"] = din("x1T_in", [128, KT, T])
    if P2 and first > 1:
        I["xc1T_in"] = din("xc1T_in", [128, KT, LC])
    if P2 or P3:
        for n_, s_ in [("convw", [128, NB, 4]), ("convb", [128, NB]), ("lru_wa", [2, NB, 128, 128]), ("lru_wx", [2, NB, 128, 128]),
                       ("lru_ba", [128, 2, NB]), ("lru_bx", [128, 2, NB]), ("lru_lam", [128, 2, NB])]:
            I[n_] = din(n_, s_)
        if not P1:
            I["halo"] = din("halo", [128, NB, 3])
        else:
            I["selm"] = din("selm", [128, 52])
    if P3:
        for n_, s_ in [("w_out_o", [DR, D]), ("router_w", [128, KT, 8]), ("moe_w1", [8, D, DFF]), ("moe_w3", [8, D, DFF]),
                       ("moe_w2", [8, DFF, D]), ("sel", [128, 4])]:
            I[n_] = din(n_, s_)
        if not P2:
            I["summ_all"] = din("summ_all", [128, 4, NB, 6])
    FUSED = P1 and P2 and P3
    if FUSED:
        cc1_in = nc.dram_tensor("cc1_in", [128, NB * 3], F32).ap()
        cc1_out = nc.dram_tensor("cc1_out", [8 * 128, NB * 3], F32).ap()
        cc2_in = nc.dram_tensor("cc2_in", [128, NB * 6], F32).ap()
        cc2_out = nc.dram_tensor("cc2_out", [8 * 128, NB * 6], F32).ap()
        SCR = {"w_in_o": nc.dram_tensor("w_in_o_bf", [D, 2 * DR], BF16).ap(),
               "lru_wa": nc.dram_tensor("lru_wa_bf", [2, NB, 128, 128], BF16).ap(),
               "lru_wx": nc.dram_tensor("lru_wx_bf", [2, NB, 128, 128], BF16).ap(),
               "w_out_o": nc.dram_tensor("w_out_o_bf", [DR, D], BF16).ap(),
               "moe_w1": nc.dram_tensor("moe_w1_bf", [8, D, DFF], BF16).ap(),
               "moe_w3": nc.dram_tensor("moe_w3_bf", [8, D, DFF], BF16).ap(),
               "moe_w2": nc.dram_tensor("moe_w2_bf", [8, DFF, D], BF16).ap()}
    O = {}
    if last == 1:
        O["x1T"] = dout("x1T", [128, KT, T])
        O["xc1T"] = dout("xc1T", [128, KT, LC])
        O["brec"] = dout("brec", [128, NB, 3])
    if last == 2:
        O["summ"] = dout("summ", [128, NB, 6])
    if last == 3:
        O["outT"] = dout("outT", [128, KT, T])
    if dbg:
        O["dbg"] = dout("dbg", [128, 4096])

    with ExitStack() as es:
        P = Prog(nc, es)

        def sb(name, shape, dt=F32, st=es):
            Ring.uid += 1
            return st.enter_context(nc.sbuf_tensor("%s_%d" % (name, Ring.uid), list(shape), dt))

        X = sb("X", [128, KT, T])
        XB = [[Buf("X%d_%d" % (kt, c)) for c in range(NCH)] for kt in range(KT)]
        Xc = sb("Xc", [128, KT, LC])
        XcB = [Buf("Xc%d" % kt) for kt in range(KT)]
        pst = es.enter_context(nc.psum_tensor("pst", [128, 4096], F32))
        PSB = [Buf("ps%d" % i) for i in range(8)]
        pstate = {"i": 0, "l": 0}

        def bank():
            i = pstate["i"] % 6
            pstate["i"] += 1
            return pst[:, i * 512:(i + 1) * 512], PSB[i]

        def bank2():
            if pstate["i"] % 2:
                pstate["i"] += 1
            i = pstate["i"] % 6
            pstate["i"] += 2
            return pst[:, i * 512:(i + 2) * 512], [PSB[i], PSB[i + 1]]

        def lbank():
            i = 6 + pstate["l"] % 2
            pstate["l"] += 1
            return pst[:, i * 512:(i + 1) * 512], PSB[i]

        ident32 = sb("ident32", [128, 128])
        identb = sb("identb", [128, 128], BF16)
        onesb = sb("onesb", [128, 128], BF16)
        ones32 = sb("ones32", [128, 128])
        vecs = sb("vecs_sb", [128, 5, KT])
        ccol = sb("ccol_sb", [128, KT, 2])
        cact = sb("cact", [128, KT, 2], BF16)
        modE = sb("modE", [128, 2, 48])
        modO = sb("modO", [128, 2, 48])
        gam = sb("gam", [128, 4, 2, KT])
        CB = Buf("consts")
        scrB = Buf("scratch")
        WQ = "sp" if FUSED else "pool"
        WS = SCR if FUSED else I
        WRD = [scrB] if FUSED else []

        def precast():
            RP = 256
            for nm in ("w_in_o", "w_out_o"):
                rows = SCR[nm].shape[0]
                for r0 in range(0, rows, RP):
                    P.dma_bulk("pool", SCR[nm][r0:r0 + RP, :], I[nm][r0:r0 + RP, :])
            for nm in ("lru_wa", "lru_wx"):
                P.dma_bulk("pool", SCR[nm].rearrange("d b i j -> (d b) (i j)"), I[nm].rearrange("d b i j -> (d b) (i j)"))
            for ex in range(8):
                for nm in ("moe_w1", "moe_w3", "moe_w2"):
                    rows = SCR[nm].shape[1]
                    for r0 in range(0, rows, RP):
                        P.dma_bulk("pool", SCR[nm][ex, r0:r0 + RP, :], I[nm][ex, r0:r0 + RP, :])
            P.bulk_done([scrB])
        halo = sb("halo", [128, NB, 3])
        haloB = Buf("halo")
        sa = sb("summ_all", [128, 4, NB, 6])
        saB = Buf("sa")
        selm = sb("selm", [128, 52])
        block = es.enter_context(nc.Block())

        def mm(out, outbufs, pairs, reads, skip_self=False):
            def f(e, out=out, pairs=pairs):
                n = len(pairs)
                ins = None
                for i, (l, r) in enumerate(pairs):
                    ins = e.matmul(out, lhsT=l, rhs=r, start=(i == 0), stop=(i == n - 1))
                return ins
            P.op("pe", f, reads=reads, writes=outbufs, skip_self=skip_self)

        def dve(f, reads, writes):
            P.op("dve", f, reads, writes)

        def act(f, reads, writes):
            P.op("act", f, reads, writes)

        def pool(f, reads, writes):
            P.op("pool", f, reads, writes)

        P.dma("sp", ident32[:], I["ident"], writes=[CB])
        P.dma("pool", identb[:], I["ident"], writes=[CB])
        P.dma("sp", vecs[:], I["vecs"], writes=[CB])
        P.dma("sp", ccol[:], I["ccol"], writes=[CB])
        dve(lambda e: e.memset(onesb[:], 1.0), [], [CB])
        dve(lambda e: e.memset(ones32[:], 1.0), [], [CB])
        act(lambda e: e.activation(out=cact[:], in_=ccol[:], func=AF.Silu), [CB], [CB])

        def setup_mods(wname, bname, dst, need=(0, 1, 2, 3, 4, 5)):
            with ExitStack() as st:
                wr = Ring(nc, st, "adaw", [128, KT, 1024], BF16, 2)
                adab = sb("adab", [128, 48], F32, st)
                ab = Buf()
                P.dma("sp", adab[:], I[bname], writes=[ab])
                dve(lambda e: e.memset(dst[:], 0.0), [CB], [CB])
                ps, pb = lbank()
                wv = I[wname].rearrange("(kt p) n -> p kt n", p=128)
                for j in need:
                    wt, wb = wr.next()
                    P.dma("pool", wt[:], wv[:, :, j * 1024:(j + 1) * 1024], writes=[wb])
                    for m in range(8):
                        col = j * 8 + m
                        mm(ps[:, col * 2:col * 2 + 2], [pb],
                           [(wt[:, kt, m * 128:(m + 1) * 128], cact[:, kt, :]) for kt in range(KT)], [wb, CB], skip_self=True)
                psv = ps[:, 0:96].rearrange("p (m w) -> p m w", w=2)
                for j in need:
                    for w in range(2):
                        dve(lambda e, w=w, j=j: e.tensor_tensor(out=dst[:, w, j * 8:(j + 1) * 8], in0=psv[:, j * 8:(j + 1) * 8, w],
                                                                in1=adab[:, j * 8:(j + 1) * 8], op=ALU.add), [pb, ab, CB], [CB])
                P.barrier()

        def setup_gam(idx, mod, nvec_idx, jsc):
            for w in range(2):
                dve(lambda e, w=w: e.scalar_tensor_tensor(out=gam[:, idx, w, :], in0=mod[:, w, jsc * 8:(jsc + 1) * 8], scalar=1.0,
                                                          in1=vecs[:, nvec_idx, :], op0=ALU.add, op1=ALU.mult), [CB], [CB])

        if P1:
            setup_mods("ada_w_e", "ada_b_e", modE)
            setup_gam(0, modE, 0, 1)
            setup_gam(1, modE, 1, 4)
        setup_mods("ada_w_o", "ada_b_o", modO, need=((0, 1, 2, 3, 4, 5) if P3 else (0, 1)))
        setup_gam(2, modO, 2, 1)
        setup_gam(3, modO, 3, 4)

        def norm_chunk(st_rings, src, n, gidx, w, beta, dst, dst32=None):
            sqr, rsr, tmr = st_rings
            ps, pb = lbank()
            for kt in range(KT):
                sa, sbuf_ = src(kt)
                sq, sqb = sqr.next()
                act(lambda e, sa=sa, sq=sq: e.activation(out=sq[:, :n], in_=sa, func=AF.Square), [sbuf_], [sqb])
                P.op("pe", (lambda e, ps=ps, sq=sq, kt=kt: e.matmul(ps[:, :n], lhsT=onesb[:], rhs=sq[:, :n], start=(kt == 0), stop=(kt == KT - 1))),
                     [sqb, CB], [pb], skip_self=(kt > 0))
            rs, rsb = rsr.next()
            act(lambda e, rs=rs, ps=ps: e.activation(out=rs[:, :n], in_=ps[:, :n], func=AF.Sqrt, scale=1.0 / D, bias=EPS), [pb], [rsb])
            dve(lambda e, rs=rs: e.reciprocal(out=rs[:, :n], in_=rs[:, :n]), [rsb], [rsb])
            for kt in range(KT):
                sa, sbuf_ = src(kt)
                tm, tmb = tmr.next()
                dve(lambda e, tm=tm, sa=sa, rs=rs: e.tensor_tensor(out=tm[:, :n], in0=sa, in1=rs[:, :n], op=ALU.mult), [sbuf_, rsb], [tmb])
                da, db = dst(kt)
                g = gam[:, gidx, w, kt:kt + 1] if gidx is not None else vecs[:, 4, kt:kt + 1]
                if dst32 is None:
                    if beta is not None:
                        act(lambda e, da=da, tm=tm, g=g, bt=beta(kt): e.activation(out=da, in_=tm[:, :n], func=AF.Identity, scale=g, bias=bt),
                            [tmb, CB], [db])
                    else:
                        act(lambda e, da=da, tm=tm, g=g: e.activation(out=da, in_=tm[:, :n], func=AF.Identity, scale=g), [tmb, CB], [db])
                else:
                    d32, d32b = dst32(kt)
                    act(lambda e, d32=d32, tm=tm, g=g, bt=beta(kt): e.activation(out=d32, in_=tm[:, :n], func=AF.Identity, scale=g, bias=bt),
                        [tmb, CB], [d32b])
                    pool(lambda e, da=da, d32=d32: e.tensor_copy(out=da, in_=d32), [d32b], [db])

        def norm_rings(st):
            return (Ring(nc, st, "nsq", [128, CH], BF16, 3), Ring(nc, st, "nrs", [128, CH], F32, 2), Ring(nc, st, "ntm", [128, CH], F32, 3))

        def xsrc(c, n=CH):
            return lambda kt: (X[:, kt, c * CH:c * CH + n], XB[kt][c])

        def xcsrc():
            return lambda kt: (Xc[:, kt, :], XcB[kt])

        def ffn_pass(st, w1v, w3v, w2v, chunks, gbc=None, wq="pool", wrd=()):
            groups = [(0, 4), (4, 4), (8, 4), (12, 4), (16, 4), (20, 2)]
            w1r, w3r, w2r, actr, sr, sgr = st
            stages = []
            for gi, (f0, G) in enumerate(groups):
                for ci in range(len(chunks)):
                    stages.append((gi, ci))
            wcur = {}

            def load_group(gi):
                f0, G = groups[gi]
                a, ab = w1r.next()
                b, bb = w3r.next()
                c, cb = w2r.next()
                P.dma(wq, a[:, :, :G * 128], w1v[:, :, f0 * 128:(f0 + G) * 128], reads=list(wrd), writes=[ab])
                P.dma(wq, b[:, :, :G * 128], w3v[:, :, f0 * 128:(f0 + G) * 128], reads=list(wrd), writes=[bb])
                P.dma(wq, c[:, :G, :], w2v[:, f0:f0 + G, :], reads=list(wrd), writes=[cb])
                wcur[gi] = (a, ab, b, bb, c, cb)

            def gu_stage(s):
                gi, ci = stages[s]
                f0, G = groups[gi]
                if gi not in wcur:
                    load_group(gi)
                a, ab, b, bb, c, cb = wcur[gi]
                h2, hb, n, xdst, g2, col0 = chunks[ci]
                at, atb = actr.next()
                for i in range(G):
                    pg, pgb = bank()
                    pu, pub = bank()
                    mm(pg[:, :n], [pgb], [(a[:, kt, i * 128:(i + 1) * 128], h2(kt)) for kt in range(KT)], [ab, hb])
                    mm(pu[:, :n], [pub], [(b[:, kt, i * 128:(i + 1) * 128], h2(kt)) for kt in range(KT)], [bb, hb])
                    s_, s_b = sr.next()
                    act(lambda e, s_=s_, pg=pg: e.activation(out=s_[:, :n], in_=pg[:, :n], func=AF.Silu), [pgb], [s_b])
                    if gbc is not None:
                        gt, gtb = gbc
                        sg, sgb = sgr.next()
                        pool(lambda e, sg=sg, s_=s_, gt=gt: e.tensor_tensor(out=sg[:, :n], in0=s_[:, :n], in1=gt[:, col0:col0 + n], op=ALU.mult),
                             [s_b, gtb], [sgb])
                        s_, s_b = sg, sgb
                    dve(lambda e, at=at, i=i, s_=s_, pu=pu: e.tensor_tensor(out=at[:, i, :n], in0=s_[:, :n], in1=pu[:, :n], op=ALU.mult),
                        [s_b, pub], [atb])
                return (at, atb)

            def y_stage(s, atp):
                gi, ci = stages[s]
                f0, G = groups[gi]
                a, ab, b, bb, c, cb = wcur[gi]
                h2, hb, n, xdst, g2, col0 = chunks[ci]
                at, atb = atp
                for f in range(KT):
                    py, pyb = bank()
                    mm(py[:, :n], [pyb], [(c[:, i, f * 128:(f + 1) * 128], at[:, i, :n]) for i in range(G)], [cb, atb])
                    xa, xb_ = xdst(f)
                    dve(lambda e, xa=xa, py=py, g=g2(f): e.scalar_tensor_tensor(out=xa, in0=py[:, :n], scalar=g, in1=xa, op0=ALU.mult, op1=ALU.add),
                        [pyb, xb_, CB], [xb_])
            prev = None
            for s in range(len(stages)):
                cur = gu_stage(s)
                if prev is not None:
                    y_stage(s - 1, prev)
                prev = cur
            y_stage(len(stages) - 1, prev)

        def ffn_rings(st):
            return (Ring(nc, st, "w1g", [128, KT, 512], BF16, 2), Ring(nc, st, "w3g", [128, KT, 512], BF16, 2),
                    Ring(nc, st, "w2g", [128, 4, D], BF16, 2), Ring(nc, st, "actg", [128, 4, CH], BF16, 2),
                    Ring(nc, st, "fs", [128, CH], F32, 3), Ring(nc, st, "fsg", [128, CH], F32, 3))

        if P1:
            for kt in range(KT):
                for c in range(NCH):
                    P.dma("sp", X[:, kt, c * CH:(c + 1) * CH], I["xT"][:, kt, c * CH:(c + 1) * CH], writes=[XB[kt][c]])
                P.dma("sp", Xc[:, kt, :], I["ctxT"][:, kt, :], writes=[XcB[kt]])
            with ExitStack() as st:
                kTe = sb("kTe", [128, TE], BF16, st)
                kTeB = [Buf("kTe%d" % i) for i in range(TE // 128)]
                kcT = sb("kcT", [128, LC], BF16, st)
                kcB = Buf("kcT")
                Ve = sb("Ve", [128, TE // 128, 128], BF16, st)
                VeB = [Buf("Ve%d" % i) for i in range(TE // 128)]
                Vc = sb("Vc", [128, 2, 128], BF16, st)
                VcB = Buf("Vc")
                ropeC = sb("ropeC", [128, TE], F32, st)
                ropeS = sb("ropeS", [128, TE], F32, st)
                maskb = sb("maskb", [128, 3, 384], BF16, st)
                wsT = sb("wsT", [128, 4, 128], BF16, st)
                bsb = sb("bsb", [128, 512], F32, st)
                sinkb = sb("sinkb", [128, 8], F32, st)
                P.dma("sp", ropeC[:], I["ropeCS"][:, 0, :], writes=[CB])
                P.dma("sp", ropeS[:], I["ropeCS"][:, 1, :], writes=[CB])
                P.dma("pool", maskb[:], I["masks"], writes=[CB])
                P.dma("pool", wsT[:], I["wsT"], writes=[CB])
                P.dma("sp", bsb[:], I["bsb"], writes=[CB])
                P.dma("sp", sinkb[:], I["sinkb"], writes=[CB])
                wv = I["w_in_e"].rearrange("(kt p) n -> p kt n", p=128)
                with ExitStack() as st2:
                    Xh = sb("Xh", [128, KT, 256], F32, st2)
                    XhB = Buf("Xh")
                    P.dma("sp", Xh[:], I["xhT"], writes=[XhB])
                    nr = norm_rings(st2)
                    hT = sb("hT", [128, KT, CH], BF16, st2)
                    hTB = Buf("hT")
                    rt = Ring(nc, st2, "ropet", [128, CH], F32, 4)
                    wpre = sb("wpre", [128, KT, 384], BF16, st2)
                    wpB = Buf("wpre")
                    P.dma("pool", wpre[:], wv[:, :, 2048:2432], writes=[wpB])
                    pre = [("ctx", xcsrc(), LC, 1, None, None),
                           ("hl", (lambda kt: (Xh[:, kt, 0:128], XhB)), 128, 0, 0, 0),
                           ("hr", (lambda kt: (Xh[:, kt, 128:256], XhB)), 128, 0, TE - 128, TE // 128 - 1)]
                    for c in range(NCH):
                        pre.append(("lat", xsrc(c), CH, 0, 128 + c * CH, 1 + c * 4))
                    for kind, src, n, w, e0, vt0 in ([] if FAST else pre):
                        norm_chunk(nr, src, n, 0, w, (lambda kt, w=w: modE[:, w, 0 * 8 + kt:0 * 8 + kt + 1]),
                                   (lambda kt: (hT[:, kt, :n], hTB)))
                        pk, pkb = bank()
                        mm(pk[:, :n], [pkb], [(wpre[:, kt, 0:128], hT[:, kt, :n]) for kt in range(KT)], [wpB, hTB])
                        if kind == "ctx":
                            act(lambda e, pk=pk: e.activation(out=kcT[:, :], in_=pk[:, :LC], func=AF.Copy), [pkb], [kcB])
                        else:
                            pks, pksb = bank()
                            mm(pks[:, :n], [pksb], [(wpre[:, kt, 128:256], hT[:, kt, :n]) for kt in range(KT)], [wpB, hTB])
                            t1, t1b = rt.next()
                            t2, t2b = rt.next()
                            dve(lambda e, t1=t1, pk=pk, e0=e0, n=n: e.tensor_tensor(out=t1[:, :n], in0=pk[:, :n], in1=ropeC[:, e0:e0 + n], op=ALU.mult),
                                [pkb, CB], [t1b])
                            dve(lambda e, t2=t2, pks=pks, e0=e0, n=n: e.tensor_tensor(out=t2[:, :n], in0=pks[:, :n], in1=ropeS[:, e0:e0 + n], op=ALU.mult),
                                [pksb, CB], [t2b])
                            kb_ = kTeB[e0 // 128:(e0 + n) // 128]
                            pool(lambda e, t1=t1, t2=t2, e0=e0, n=n: e.tensor_tensor(out=kTe[:, e0:e0 + n], in0=t1[:, :n], in1=t2[:, :n], op=ALU.add),
                                 [t1b, t2b], kb_)
                        for tt in range(n // 128):
                            pv, pvb = bank()
                            mm(pv[:, 0:128], [pvb], [(hT[:, kt, tt * 128:(tt + 1) * 128], wpre[:, kt, 256:384]) for kt in range(KT)], [wpB, hTB])
                            if kind == "ctx":
                                act(lambda e, pv=pv, tt=tt: e.activation(out=Vc[:, tt, :], in_=pv[:, 0:128], func=AF.Copy), [pvb], [VcB])
                            else:
                                act(lambda e, pv=pv, vi=vt0 + tt: e.activation(out=Ve[:, vi, :], in_=pv[:, 0:128], func=AF.Copy), [pvb], [VeB[vt0 + tt]])
                    P.barrier()
                with ExitStack() as st2:
                    wmain = sb("wmain", [128, KT, 2048], BF16, st2)
                    wmB = Buf("wmain")
                    for q4 in range(4):
                        P.dma("pool", wmain[:, :, q4 * 512:(q4 + 1) * 512], wv[:, :, q4 * 512:(q4 + 1) * 512], writes=[wmB])
                    wout = sb("wout", [128, KT, D], BF16, st2)
                    woB = Buf("wout")
                    P.dma("pool", wout[:], I["w_out_e"].rearrange("(kt p) n -> p kt n", p=128), writes=[woB])
                    if FUSED:
                        precast()
                    nr = (Ring(nc, st2, "nsq2", [128, MC], BF16, 2), Ring(nc, st2, "nrs2", [128, MC], F32, 2), Ring(nc, st2, "ntm2", [128, MC], F32, 2))
                    hT = sb("hT2", [128, KT, MC], BF16, st2)
                    hTB = Buf("hT2")
                    rt = Ring(nc, st2, "ropet2", [128, 512], F32, 3)
                    gu = sb("gu", [128, 4, MC], BF16, st2)
                    guB = Buf("gu")
                    qT = sb("qT", [128, 4, MC], BF16, st2)
                    qTB = Buf("qT")
                    mix = sb("mix", [128, KT, MC], BF16, st2)
                    mixA = Buf("mixA")
                    mixBq = [Buf("mixB%d" % i) for i in range(4)]
                    gvr = Ring(nc, st2, "gv", [128, 512], F32, 2)
                    vnr = Ring(nc, st2, "vn", [128, 512], BF16, 2)
                    str_ = Ring(nc, st2, "bnst", [128, 4, 6], F32, 2)
                    mvr = Ring(nc, st2, "bnmv", [128, 4, 2], F32, 2)
                    sdr = Ring(nc, st2, "sd", [128, 4], F32, 2)
                    ptr = Ring(nc, st2, "Pt", [128, 640], BF16, 3)
                    ptsr = Ring(nc, st2, "PTs", [128, 640], BF16, 2)
                    dgr = Ring(nc, st2, "Dg", [128, 128], BF16, 3)
                    smr = Ring(nc, st2, "sm", [128, 8], F32, 4)
                    main = [("ctx", xcsrc(), LC, 1, None)] + [("lat", (lambda kt, c2=c2: (X[:, kt, c2 * MC:(c2 + 1) * MC], XB[kt][c2 * MC // CH])), MC, 0, c2)
                                                          for c2 in range(T // MC)]
                    for kind, src, n, w, c in ([] if FAST else main):
                        norm_chunk(nr, src, n, 0, w, (lambda kt, w=w: modE[:, w, kt:kt + 1]), (lambda kt: (hT[:, kt, :n], hTB)))
                        for m in range(4):
                            pu, pub = bank()
                            mm(pu[:, :n], [pub], [(wmain[:, kt, m * 128:(m + 1) * 128], hT[:, kt, :n]) for kt in range(KT)], [wmB, hTB])
                            act(lambda e, pu=pu, m=m, n=n: e.activation(out=gu[:, m, :n], in_=pu[:, :n], func=AF.Gelu_apprx_tanh), [pub], [guB])
                        for j in range(4):
                            pq, pqb = bank()
                            mm(pq[:, :n], [pqb], [(wmain[:, kt, 1024 + j * 128:1024 + (j + 1) * 128], hT[:, kt, :n]) for kt in range(KT)], [wmB, hTB])
                            if kind == "ctx":
                                act(lambda e, pq=pq, j=j, n=n: e.activation(out=qT[:, j, :n], in_=pq[:, :n], func=AF.Copy), [pqb], [qTB])
                            else:
                                pqs, pqsb = bank()
                                mm(pqs[:, :n], [pqsb], [(wmain[:, kt, 1536 + j * 128:1536 + (j + 1) * 128], hT[:, kt, :n]) for kt in range(KT)], [wmB, hTB])
                                e0 = 128 + c * MC
                                t1, t1b = rt.next()
                                t2, t2b = rt.next()
                                dve(lambda e, t1=t1, pq=pq, e0=e0, n=n: e.tensor_tensor(out=t1[:, :n], in0=pq[:, :n], in1=ropeC[:, e0:e0 + n], op=ALU.mult),
                                    [pqb, CB], [t1b])
                                dve(lambda e, t2=t2, pqs=pqs, e0=e0, n=n: e.tensor_tensor(out=t2[:, :n], in0=pqs[:, :n], in1=ropeS[:, e0:e0 + n], op=ALU.mult),
                                    [pqsb, CB], [t2b])
                                pool(lambda e, t1=t1, t2=t2, j=j, n=n: e.tensor_tensor(out=qT[:, j, :n], in0=t1[:, :n], in1=t2[:, :n], op=ALU.add),
                                     [t1b, t2b], [qTB])
                        for tt in range(n // 128):
                            tsl = slice(tt * 128, (tt + 1) * 128)
                            pv, pvb = bank()
                            mm(pv[:, :], [pvb], [(hT[:, kt, tsl], wmain[:, kt, 512:1024]) for kt in range(KT)], [wmB, hTB])
                            gv, gvb = gvr.next()
                            act(lambda e, gv=gv, pv=pv: e.activation(out=gv[:], in_=pv[:], func=AF.Gelu_apprx_tanh), [pvb], [gvb])
                            stt, stb = str_.next()
                            mv, mvb = mvr.next()
                            for g in range(4):
                                dve(lambda e, stt=stt, gv=gv, g=g: e.bn_stats(out=stt[:, g, :], in_=gv[:, g * 128:(g + 1) * 128]), [gvb], [stb])
                                dve(lambda e, stt=stt, mv=mv, g=g: e.bn_aggr(out=mv[:, g, :], in_=stt[:, g, :]), [stb], [mvb])
                            sd, sdb = sdr.next()
                            act(lambda e, sd=sd, mv=mv: e.activation(out=sd[:], in_=mv[:, :, 1], func=AF.Sqrt, bias=EPS, scale=1.0), [mvb], [sdb])
                            dve(lambda e, sd=sd: e.reciprocal(out=sd[:], in_=sd[:]), [sdb], [sdb])
                            vn, vnb = vnr.next()
                            for g in range(4):
                                dve(lambda e, vn=vn, gv=gv, mv=mv, sd=sd, g=g: e.tensor_scalar(
                                    out=vn[:, g * 128:(g + 1) * 128], in0=gv[:, g * 128:(g + 1) * 128], scalar1=mv[:, g, 0:1],
                                    scalar2=sd[:, g:g + 1], op0=ALU.subtract, op1=ALU.mult), [gvb, mvb, sdb], [vnb])
                            pm, pmb = bank()

                            def mixmm(e, pm=pm, vn=vn):
                                ins = None
                                for g in range(4):
                                    ins = e.matmul(pm[:, g * 128:(g + 1) * 128], lhsT=vn[:, g * 128:(g + 1) * 128], rhs=wsT[:, g, :], start=True, stop=True)
                                return ins
                            P.op("pe", mixmm, [vnb, CB], [pmb])
                            t1, t1b = rt.next()
                            dve(lambda e, t1=t1, pm=pm: e.tensor_tensor(out=t1[:], in0=pm[:], in1=bsb[:], op=ALU.add), [pmb, CB], [t1b])
                            pool(lambda e, t1=t1, tsl=tsl: e.tensor_tensor(out=mix[:, 0:4, tsl], in0=t1[:].rearrange("p (g t) -> p g t", g=4),
                                                                            in1=gu[:, 0:4, tsl], op=ALU.mult), [t1b, guB], [mixA])
                        def att_A(qb, j, hh, po, pob):
                            qsl = slice(qb * 128, (qb + 1) * 128)
                            h = j + 4 * hh
                            psl = slice(hh * 64, (hh + 1) * 64)
                            S, Sb = bank2()
                            if kind == "ctx":
                                nk = 256
                                P.op("pe", (lambda e, S=S, j=j, psl=psl, qsl=qsl: e.matmul(S[:, 0:256], lhsT=qT[psl, j, qsl], rhs=kcT[psl, :], start=True, stop=True)),
                                     [qTB, kcB], Sb)
                                vts = [(Vc, 0, VcB), (Vc, 1, VcB)]
                            else:
                                nk = 640
                                E0 = c * MC + qb * 128
                                gb = c * (MC // 128) + qb
                                mi = 0 if (c == 0 and qb == 0) else (2 if (c == T // MC - 1 and qb == MC // 128 - 1) else 1)

                                def qk(e, S=S, j=j, psl=psl, qsl=qsl, E0=E0, mi=mi):
                                    e.matmul(S[:, 0:384], lhsT=qT[psl, j, qsl], rhs=kTe[psl, E0:E0 + 384], start=True, stop=False)
                                    e.matmul(S[:, 0:384], lhsT=identb[:], rhs=maskb[:, mi, :], start=False, stop=True)
                                    e.matmul(S[:, 384:512], lhsT=qT[psl, j, qsl], rhs=kcT[psl, 0:128], start=True, stop=True)
                                    return e.matmul(S[:, 512:640], lhsT=qT[psl, j, qsl], rhs=kcT[psl, 128:256], start=True, stop=True)
                                P.op("pe", qk, [qTB, kcB, CB] + kTeB[gb:gb + 3], Sb)
                                vts = [(Ve, gb, VeB[gb]), (Ve, gb + 1, VeB[gb + 1]), (Ve, gb + 2, VeB[gb + 2]), (Vc, 0, VcB), (Vc, 1, VcB)]
                            sm, smb = smr.next()
                            dve(lambda e, sm=sm, S=S, nk=nk: e.reduce_max(out=sm[:, 0:1], in_=S[:, 0:nk], axis=AX.X), Sb, [smb])
                            dve(lambda e, sm=sm, h=h: e.tensor_scalar(out=sm[:, 1:2], in0=sm[:, 0:1], scalar1=SCALE, scalar2=sinkb[:, h:h + 1],
                                                                      op0=ALU.mult, op1=ALU.max), [smb, CB], [smb])
                            dve(lambda e, sm=sm: e.tensor_scalar(out=sm[:, 2:3], in0=sm[:, 1:2], scalar1=-1.0, scalar2=None, op0=ALU.mult), [smb], [smb])
                            dve(lambda e, sm=sm: e.memset(sm[:, 3:4], 0.0), [smb], [smb])
                            pt, ptb = ptr.next()
                            act(lambda e, pt=pt, S=S, sm=sm, nk=nk: e.activation(out=pt[:, 0:nk], in_=S[:, 0:nk], func=AF.Exp, scale=SCALE,
                                                                               bias=sm[:, 2:3], accum_out=sm[:, 3:4]), Sb + [smb], [ptb, smb])
                            act(lambda e, sm=sm, h=h: e.activation(out=sm[:, 4:5], in_=sinkb[:, h:h + 1], func=AF.Exp, scale=1.0, bias=sm[:, 2:3]),
                                [smb, CB], [smb])
                            dve(lambda e, sm=sm: e.tensor_tensor(out=sm[:, 5:6], in0=sm[:, 3:4], in1=sm[:, 4:5], op=ALU.add), [smb], [smb])
                            dve(lambda e, sm=sm: e.reciprocal(out=sm[:, 6:7], in_=sm[:, 5:6]), [smb], [smb])
                            dg, dgb = dgr.next()
                            dve(lambda e, dg=dg, sm=sm: e.tensor_scalar(out=dg[:], in0=identb[:], scalar1=sm[:, 6:7], scalar2=None, op0=ALU.mult),
                                [smb, CB], [dgb])
                            return dict(j=j, hh=hh, po=po, pob=pob, pt=pt, ptb=ptb, dg=dg, dgb=dgb, nk=nk, vts=vts, psl=psl, qsl=qsl)

                        def att_C(u):
                            j, hh, po, pob, pt, ptb, dg, dgb, nk, vts, psl, qsl = (u[k_] for k_ in ("j", "hh", "po", "pob", "pt", "ptb", "dg", "dgb", "nk", "vts", "psl", "qsl"))
                            PT, PTb = bank2()
                            nkt = nk // 128

                            def tr(e, PT=PT, pt=pt, dg=dg, nkt=nkt):
                                ins = None
                                for k_ in range(nkt):
                                    ins = e.matmul(PT[:, k_ * 128:(k_ + 1) * 128], lhsT=pt[:, k_ * 128:(k_ + 1) * 128], rhs=dg[:], start=True, stop=True)
                                return ins
                            P.op("pe", tr, [ptb, dgb], PTb)
                            pts, ptsb = ptsr.next()
                            act(lambda e, pts=pts, PT=PT, nk=nk: e.activation(out=pts[:, 0:nk], in_=PT[:, 0:nk], func=AF.Copy), PTb, [ptsb])

                            def pv_(e, po=po, pts=pts, vts=vts, psl=psl, hh=hh):
                                ins = None
                                for k_, (vt, vi, _) in enumerate(vts):
                                    ins = e.matmul(po[psl, 0:128], lhsT=vt[:, vi, hh * 64:(hh + 1) * 64], rhs=pts[:, k_ * 128:(k_ + 1) * 128],
                                                   start=(k_ == 0), stop=(k_ == len(vts) - 1))
                                return ins
                            P.op("pe", pv_, [ptsb] + [v[2] for v in vts], [pob], skip_self=(hh == 1))
                            if hh == 1:
                                dve(lambda e, po=po, j=j, qsl=qsl: e.tensor_copy(out=mix[:, 4 + j, qsl], in_=po[:, 0:128]), [pob], [mixBq[j]])
                        pend = None
                        for qb in range(n // 128):
                            for j in range(4):
                                po, pob = lbank()
                                for hh in range(2):
                                    cur = att_A(qb, j, hh, po, pob)
                                    if pend is not None:
                                        att_C(pend)
                                    pend = cur
                        att_C(pend)
                        for f in range(KT):
                            py, pyb = bank()
                            mm(py[:, :n], [pyb], [(wout[:, mt, f * 128:(f + 1) * 128], mix[:, mt, :n]) for mt in range(KT)], [woB, mixA] + mixBq)
                            if kind == "ctx":
                                xa, xb_ = Xc[:, f, :], XcB[f]
                            else:
                                xa, xb_ = X[:, f, c * MC:(c + 1) * MC], XB[f][c * MC // CH]
                            dve(lambda e, xa=xa, py=py, n=n, g=modE[:, w, 16 + f:16 + f + 1]: e.scalar_tensor_tensor(
                                out=xa, in0=py[:, :n], scalar=g, in1=xa, op0=ALU.mult, op1=ALU.add), [pyb, xb_, CB], [xb_])
                    P.barrier()
                P.barrier()
            with ExitStack() as st:
                nr = norm_rings(st)
                h2T = sb("h2T", [128, KT, T], BF16, st)
                h2B = [Buf("h2_%d" % c) for c in range(NCH)]
                h2c = sb("h2c", [128, KT, LC], BF16, st)
                h2cB = Buf("h2c")
                for c in range(NCH):
                    norm_chunk(nr, xsrc(c), CH, 1, 0, (lambda kt: modE[:, 0, 24 + kt:24 + kt + 1]),
                               (lambda kt, c=c: (h2T[:, kt, c * CH:(c + 1) * CH], h2B[c])))
                norm_chunk(nr, xcsrc(), LC, 1, 1, (lambda kt: modE[:, 1, 24 + kt:24 + kt + 1]), (lambda kt: (h2c[:, kt, :], h2cB)))
                chunks = []
                for c in range(NCH):
                    chunks.append(((lambda kt, c=c: h2T[:, kt, c * CH:(c + 1) * CH]), h2B[c], CH,
                                   (lambda f, c=c: (X[:, f, c * CH:(c + 1) * CH], XB[f][c])),
                                   (lambda f: modE[:, 0, 40 + f:40 + f + 1]), c * CH))
                chunks.append(((lambda kt: h2c[:, kt, :]), h2cB, LC, (lambda f: (Xc[:, f, :], XcB[f])),
                               (lambda f: modE[:, 1, 40 + f:40 + f + 1]), 0))
                if not FAST:
                    ffn_pass(ffn_rings(st), I["ffn_w1"].rearrange("(kt p) n -> p kt n", p=128), I["ffn_w3"].rearrange("(kt p) n -> p kt n", p=128),
                             I["ffn_w2"].rearrange("(ft p) n -> p ft n", p=128), chunks)
                P.barrier()
        else:
            for kt in range(KT):
                for c in range(NCH):
                    P.dma("sp", X[:, kt, c * CH:(c + 1) * CH], I["x1T_in"][:, kt, c * CH:(c + 1) * CH], writes=[XB[kt][c]])
                if P2:
                    P.dma("sp", Xc[:, kt, :], I["xc1T_in"][:, kt, :], writes=[XcB[kt]])

        wio = I["w_in_o"].rearrange("(kt p) n -> p kt n", p=128)
        wio2 = WS["w_in_o"].rearrange("(kt p) n -> p kt n", p=128)
        if P1:
            with ExitStack() as st:
                nr = norm_rings(st)
                xb3 = sb("xb3", [128, KT, 4], F32, st)
                xb3B = Buf("xb3")
                hb3 = sb("hb3", [128, KT, 4], BF16, st)
                hb3B = Buf("hb3")
                brec = sb("brec", [128, NB, 3], F32, st)
                brB = Buf("brec")
                for kt in range(KT):
                    dve(lambda e, kt=kt: e.tensor_copy(out=xb3[:, kt, 0:1], in_=X[:, kt, 0:1]), [XB[kt][0]], [xb3B])
                    dve(lambda e, kt=kt: e.tensor_copy(out=xb3[:, kt, 1:3], in_=X[:, kt, T - 2:T]), [XB[kt][3]], [xb3B])
                norm_chunk(nr, (lambda kt: (xb3[:, kt, 0:3], xb3B)), 3, 2, 0, (lambda kt: modO[:, 0, kt:kt + 1]), (lambda kt: (hb3[:, kt, 0:3], hb3B)))
                wr = Ring(nc, st, "wrec", [128, KT, 256], BF16, 2)
                ps, pb = lbank()
                for piece in range(5):
                    wt, wb = wr.next()
                    P.dma("pool", wt[:], wio[:, :, DR + piece * 256:DR + (piece + 1) * 256], writes=[wb])
                    for m in range(2):
                        blk = piece * 2 + m
                        mm(ps[:, blk * 3:blk * 3 + 3], [pb], [(wt[:, kt, m * 128:(m + 1) * 128], hb3[:, kt, 0:3]) for kt in range(KT)], [wb, hb3B], skip_self=True)
                dve(lambda e: e.tensor_copy(out=brec[:].rearrange("p a b -> p (a b)"), in_=ps[:, 0:30]), [pb], [brB])
                if not FUSED:
                    P.dma("sp", O["brec"], brec[:], reads=[brB])
                    for kt in range(KT):
                        P.dma("sp", O["x1T"][:, kt, :], X[:, kt, :], reads=XB[kt])
                        P.dma("sp", O["xc1T"][:, kt, :], Xc[:, kt, :], reads=[XcB[kt]])
                else:
                    c1i, c1o = Buf("cc1i"), Buf("cc1o")
                    P.dma("sp", selm[:], I["selm"], writes=[CB])
                    P.dma("sp", cc1_in, brec[:].rearrange("p a b -> p (a b)"), reads=[brB], writes=[c1i])
                    P.cc((lambda e: e.collective_compute("AllGather", ALU.bypass, replica_groups=[list(range(8))],
                                                         ins=[cc1_in.opt()], outs=[cc1_out.opt()])), reads=[c1i], writes=[c1o])
                    g1 = sb("g1all", [128, 8, NB, 3], F32, st)
                    g1B = Buf("g1all")
                    P.dma("sp", g1[:].rearrange("p r a b -> p r (a b)"), cc1_out.rearrange("(r p) n -> p r n", p=128), reads=[c1o], writes=[g1B])
                    dve(lambda e: e.memset(halo[:], 0.0), [], [haloB])
                    for r in range(8):
                        dve(lambda e, r=r: e.scalar_tensor_tensor(out=halo[:, :, 0:2], in0=g1[:, r, :, 1:3], scalar=selm[:, r:r + 1], in1=halo[:, :, 0:2],
                                                                  op0=ALU.mult, op1=ALU.add), [g1B, haloB, CB], [haloB])
                        dve(lambda e, r=r: e.scalar_tensor_tensor(out=halo[:, :, 2:3], in0=g1[:, r, :, 0:1], scalar=selm[:, 8 + r:9 + r], in1=halo[:, :, 2:3],
                                                                  op0=ALU.mult, op1=ALU.add), [g1B, haloB, CB], [haloB])
                    if dbg:
                        P.dma("sp", O["dbg"][:, 400:430], brec[:].rearrange("p a b -> p (a b)"), reads=[brB])
                        P.dma("sp", O["dbg"][:, 512:752], g1[:].rearrange("p r a b -> p (r a b)"), reads=[g1B])
                        P.dma("sp", O["dbg"][:, 768:820], selm[:], reads=[CB])
                P.barrier()

        if P2 or P3:
            with ExitStack() as st:
                h1T = sb("h1T", [128, KT, T], BF16, st)
                h1B = [Buf("h1_%d" % c) for c in range(NCH)]
                if P2:
                    h1c = sb("h1c", [128, KT, LC], BF16, st)
                    h1cB = Buf("h1c")
                with ExitStack() as stn:
                    nr = norm_rings(stn)
                    for c in range(NCH):
                        norm_chunk(nr, xsrc(c), CH, 2, 0, (lambda kt: modO[:, 0, kt:kt + 1]), (lambda kt, c=c: (h1T[:, kt, c * CH:(c + 1) * CH], h1B[c])))
                    if P2:
                        norm_chunk(nr, xcsrc(), LC, 2, 1, (lambda kt: modO[:, 1, kt:kt + 1]), (lambda kt: (h1c[:, kt, :], h1cB)))
                    P.barrier()
                cw = sb("cw", [128, NB, 4], F32, st)
                cbv = sb("cbv", [128, NB], F32, st)
                lba = sb("lba", [128, 2, NB], F32, st)
                lbx = sb("lbx", [128, 2, NB], F32, st)
                lam = sb("lam", [128, 2, NB], F32, st)
                sc8 = sb("sc8", [128, 2, NB], F32, st)
                sc16 = sb("sc16", [128, 2, NB], F32, st)
                ltmp = sb("ltmp", [128, 2, NB], F32, st)
                for t_, n_ in [(cw, "convw"), (cbv, "convb"), (lba, "lru_ba"), (lbx, "lru_bx"), (lam, "lru_lam")]:
                    P.dma("sp", t_[:], I[n_], writes=[CB])
                if not P1:
                    P.dma("sp", halo[:], I["halo"], writes=[haloB])
                dve(lambda e: e.tensor_scalar(out=ltmp[:], in0=lam[:], scalar1=-1.0, scalar2=None, op0=ALU.mult), [CB], [CB])
                dve(lambda e: e.tensor_tensor(out=ltmp[:], in0=ltmp[:], in1=lam[:], op=ALU.max), [CB], [CB])
                act(lambda e: e.activation(out=ltmp[:], in_=ltmp[:], func=AF.Exp, scale=-1.0), [CB], [CB])
                act(lambda e: e.activation(out=ltmp[:], in_=ltmp[:], func=AF.Ln, bias=1.0, scale=1.0), [CB], [CB])
                dve(lambda e: e.tensor_scalar(out=sc8[:], in0=lam[:], scalar1=-1.0, scalar2=0.0, op0=ALU.mult, op1=ALU.max), [CB], [CB])
                dve(lambda e: e.tensor_tensor(out=sc8[:], in0=sc8[:], in1=ltmp[:], op=ALU.add), [CB], [CB])
                dve(lambda e: e.tensor_scalar(out=sc16[:], in0=sc8[:], scalar1=-16.0, scalar2=None, op0=ALU.mult), [CB], [CB])
                dve(lambda e: e.tensor_scalar(out=sc8[:], in0=sc8[:], scalar1=-8.0, scalar2=None, op0=ALU.mult), [CB], [CB])
                summ = sb("summ", [128, NB, 6], F32, st)
                summB = Buf("summ")
                rsum = sb("rsum", [128, NB, 2, NCH], F32, st)
                rsB = Buf("rsum")
                hin = sb("hin", [128, NB, 2], F32, st)
                hinB = Buf("hin")
                dve(lambda e: e.memset(hin[:], 0.0), [], [hinB])
                dve(lambda e: e.memset(rsum[:], 0.0), [], [rsB])

                def mixer_pass(final):
                    with ExitStack() as st2:
                        wr = Ring(nc, st2, "wrc", [128, KT, 128], BF16, 2)
                        wg = Ring(nc, st2, "wgt", [128, KT, 128], BF16, 2) if final else None
                        lw = Ring(nc, st2, "lw", [128, 4, 128], BF16, 2)
                        wo = Ring(nc, st2, "wo", [128, D], BF16, 2) if final else None
                        recp = sb("recp", [128, T + 3], F32, st2)
                        recpB = Buf("recp")
                        rc = sb("rc", [128, T], F32, st2)
                        rcB = Buf("rc")
                        rcb = sb("rcb", [128, T], BF16, st2)
                        rcbB = Buf("rcb")
                        hs = [sb("hs%d" % d_, [128, T], F32, st2) for d_ in range(2)]
                        hsB = [Buf("hs%d" % d_) for d_ in range(2)]
                        tr_ = Ring(nc, st2, "lt", [128, CH], F32, 16 if final else 20)
                        yb = Ring(nc, st2, "yblk", [128, T], BF16, 2) if final else None
                        if not final:
                            recpc = sb("recpc", [128, LC + 3], F32, st2)
                            rcc = sb("rcc", [128, LC], F32, st2)
                            rccb = sb("rccb", [128, LC], BF16, st2)
                            hsc = sb("hsc", [128, LC], F32, st2)
                            cB_ = Buf("ctxlru")
                            dve(lambda e: e.memset(recpc[:], 0.0), [], [cB_])
                        for blk in range(NB):
                            wt, wb = wr.next()
                            P.dma(WQ, wt[:], wio2[:, :, DR + blk * 128:DR + (blk + 1) * 128], reads=WRD, writes=[wb])
                            lt_, lb_ = lw.next()
                            for d_ in range(2):
                                P.dma(WQ, lt_[:, d_, :], WS["lru_wa"][d_, blk], reads=WRD, writes=[lb_])
                                P.dma(WQ, lt_[:, 2 + d_, :], WS["lru_wx"][d_, blk], reads=WRD, writes=[lb_])
                            if final:
                                gt_, gb_ = wg.next()
                                P.dma(WQ, gt_[:], wio2[:, :, blk * 128:(blk + 1) * 128], reads=WRD, writes=[gb_])
                                wot, wob = wo.next()
                                P.dma(WQ, wot[:], WS["w_out_o"][blk * 128:(blk + 1) * 128, :], reads=WRD, writes=[wob])
                            dve(lambda e, blk=blk: e.tensor_copy(out=recp[:, 0:2], in_=halo[:, blk, 0:2]), [haloB], [recpB])
                            dve(lambda e, blk=blk: e.tensor_copy(out=recp[:, T + 2:T + 3], in_=halo[:, blk, 2:3]), [haloB], [recpB])
                            for c in range(NCH):
                                pr, prb = bank()
                                mm(pr[:], [prb], [(wt[:, kt, :], h1T[:, kt, c * CH:(c + 1) * CH]) for kt in range(KT)], [wb, h1B[c]])
                                act(lambda e, pr=pr, c=c: e.activation(out=recp[:, 2 + c * CH:2 + (c + 1) * CH], in_=pr[:], func=AF.Copy), [prb], [recpB])
                            dve(lambda e, blk=blk: e.tensor_scalar(out=rc[:], in0=recp[:, 0:T], scalar1=cw[:, blk, 0:1], scalar2=cbv[:, blk:blk + 1],
                                                                    op0=ALU.mult, op1=ALU.add), [recpB, CB], [rcB])
                            for j in range(1, 4):
                                dve(lambda e, blk=blk, j=j: e.scalar_tensor_tensor(out=rc[:], in0=recp[:, j:j + T], scalar=cw[:, blk, j:j + 1], in1=rc[:],
                                                                                     op0=ALU.mult, op1=ALU.add), [recpB, rcB, CB], [rcB])
                            pool(lambda e: e.tensor_copy(out=rcb[:], in_=rc[:]), [rcB], [rcbB])
                            seqs = [("lat", rc, rcb, rcB, rcbB, T, hs)]
                            if not final:
                                pr, prb = bank()
                                mm(pr[:, :LC], [prb], [(wt[:, kt, :], h1c[:, kt, :]) for kt in range(KT)], [wb, h1cB])
                                act(lambda e, pr=pr: e.activation(out=recpc[:, 2:2 + LC], in_=pr[:, :LC], func=AF.Copy), [prb], [cB_])
                                dve(lambda e, blk=blk: e.tensor_scalar(out=rcc[:], in0=recpc[:, 0:LC], scalar1=cw[:, blk, 0:1], scalar2=cbv[:, blk:blk + 1],
                                                                        op0=ALU.mult, op1=ALU.add), [cB_, CB], [cB_])
                                for j in range(1, 4):
                                    dve(lambda e, blk=blk, j=j: e.scalar_tensor_tensor(out=rcc[:], in0=recpc[:, j:j + LC], scalar=cw[:, blk, j:j + 1], in1=rcc[:],
                                                                                         op0=ALU.mult, op1=ALU.add), [cB_, CB], [cB_])
                                pool(lambda e: e.tensor_copy(out=rccb[:], in_=rcc[:]), [cB_], [cB_])
                                seqs = [("ctx", rcc, rccb, cB_, cB_, LC, None)] + seqs
                            for kind, r32, rb16, r32B, rb16B, nt, hs_ in seqs:
                                for d_ in range(2):
                                    nch = (nt + CH - 1) // CH
                                    order = list(range(nch)) if d_ == 0 else list(range(nch - 1, -1, -1))
                                    items = []
                                    for oi, c in enumerate(order):
                                        n = min(CH, nt - c * CH)
                                        csl = slice(c * CH, c * CH + n)
                                        pa, pab = bank()
                                        px, pxb = bank()
                                        mm(pa[:, :n], [pab], [(lt_[:, d_, :], rb16[:, csl])], [lb_, rb16B])
                                        mm(px[:, :n], [pxb], [(lt_[:, 2 + d_, :], rb16[:, csl])], [lb_, rb16B])
                                        r_, r_b = tr_.next()
                                        i_, i_b = tr_.next()
                                        a_, a_b = tr_.next()
                                        b_, b_b = tr_.next()
                                        if kind == "lat" and not final:
                                            act(lambda e, r_=r_, pa=pa, n=n, blk=blk, d_=d_, c=c: e.activation(
                                                out=r_[:, :n], in_=pa[:, :n], func=AF.Sigmoid, bias=lba[:, d_, blk:blk + 1], scale=1.0,
                                                accum_out=rsum[:, blk, d_, c:c + 1]), [pab, CB], [r_b, rsB])
                                        else:
                                            act(lambda e, r_=r_, pa=pa, n=n, blk=blk, d_=d_: e.activation(
                                                out=r_[:, :n], in_=pa[:, :n], func=AF.Sigmoid, bias=lba[:, d_, blk:blk + 1], scale=1.0), [pab, CB], [r_b])
                                        act(lambda e, i_=i_, px=px, n=n, blk=blk, d_=d_: e.activation(
                                            out=i_[:, :n], in_=px[:, :n], func=AF.Sigmoid, bias=lbx[:, d_, blk:blk + 1], scale=1.0), [pxb, CB], [i_b])
                                        items.append((oi, c, n, csl, r_, r_b, i_, i_b, a_, a_b, b_, b_b))
                                    for (oi, c, n, csl, r_, r_b, i_, i_b, a_, a_b, b_, b_b) in items:
                                        act(lambda e, a_=a_, r_=r_, n=n, blk=blk, d_=d_: e.activation(
                                            out=a_[:, :n], in_=r_[:, :n], func=AF.Exp, scale=sc8[:, d_, blk:blk + 1]), [r_b, CB], [a_b])
                                        act(lambda e, b_=b_, r_=r_, n=n, blk=blk, d_=d_: e.activation(
                                            out=b_[:, :n], in_=r_[:, :n], func=AF.Exp, scale=sc16[:, d_, blk:blk + 1]), [r_b, CB], [b_b])
                                    for (oi, c, n, csl, r_, r_b, i_, i_b, a_, a_b, b_, b_b) in items:
                                        act(lambda e, b_=b_, n=n: e.activation(out=b_[:, :n], in_=b_[:, :n], func=AF.Sqrt, scale=-1.0, bias=1.0), [b_b], [b_b])
                                    for (oi, c, n, csl, r_, r_b, i_, i_b, a_, a_b, b_, b_b) in items:
                                        dve(lambda e, b_=b_, i_=i_, n=n: e.tensor_tensor(out=b_[:, :n], in0=b_[:, :n], in1=i_[:, :n], op=ALU.mult), [b_b, i_b], [b_b])
                                        pool(lambda e, b_=b_, r32=r32, csl=csl, n=n: e.tensor_tensor(out=b_[:, :n], in0=b_[:, :n], in1=r32[:, csl], op=ALU.mult),
                                             [b_b, r32B], [b_b])
                                        if kind == "ctx":
                                            ho, hoB = hsc, cB_
                                            init = 0.0
                                            initB = []
                                        else:
                                            ho, hoB = hs_[d_], hsB[d_]
                                            if oi == 0:
                                                init = hin[:, blk, d_:d_ + 1]
                                                initB = [hinB]
                                            elif d_ == 0:
                                                init = ho[:, c * CH - 1:c * CH]
                                                initB = []
                                            else:
                                                init = ho[:, (c + 1) * CH:(c + 1) * CH + 1]
                                                initB = []
                                        if d_ == 0:
                                            dve(lambda e, ho=ho, a_=a_, b_=b_, csl=csl, n=n, init=init: e.tensor_tensor_scan(
                                                out=ho[:, csl], data0=a_[:, :n], data1=b_[:, :n], initial=init, op0=ALU.mult, op1=ALU.add),
                                                [a_b, b_b, hoB] + initB, [hoB])
                                        else:
                                            lo = c * CH
                                            dve(lambda e, ho=ho, a_=a_, b_=b_, lo=lo, n=n, init=init: e.tensor_tensor_scan(
                                                out=ho[:, lo:lo + n][:, ::-1], data0=a_[:, 0:n][:, ::-1], data1=b_[:, 0:n][:, ::-1], initial=init,
                                                op0=ALU.mult, op1=ALU.add), [a_b, b_b, hoB] + initB, [hoB])
                                    if not final:
                                        if kind == "ctx":
                                            src_ = hsc[:, LC - 1:LC] if d_ == 0 else hsc[:, 0:1]
                                            dve(lambda e, blk=blk, d_=d_, src_=src_: e.tensor_copy(out=summ[:, blk, 4 + d_:5 + d_], in_=src_), [cB_], [summB])
                                        else:
                                            src_ = hs_[0][:, T - 1:T] if d_ == 0 else hs_[1][:, 0:1]
                                            dve(lambda e, blk=blk, d_=d_, src_=src_: e.tensor_copy(out=summ[:, blk, 2 * d_ + 1:2 * d_ + 2], in_=src_), [hsB[d_]], [summB])
                            if final:
                                yt, ytb = yb.next()
                                for c in range(NCH):
                                    csl = slice(c * CH, (c + 1) * CH)
                                    pg, pgb = bank()
                                    mm(pg[:], [pgb], [(gt_[:, kt, :], h1T[:, kt, csl]) for kt in range(KT)], [gb_, h1B[c]])
                                    g_, g_b = tr_.next()
                                    act(lambda e, g_=g_, pg=pg: e.activation(out=g_[:], in_=pg[:], func=AF.Gelu_apprx_tanh), [pgb], [g_b])
                                    s_, s_b = tr_.next()
                                    dve(lambda e, s_=s_, csl=csl: e.tensor_tensor(out=s_[:], in0=hs[0][:, csl], in1=hs[1][:, csl], op=ALU.add), hsB, [s_b])
                                    pool(lambda e, yt=yt, g_=g_, s_=s_, csl=csl: e.tensor_tensor(out=yt[:, csl], in0=g_[:], in1=s_[:], op=ALU.mult), [g_b, s_b], [ytb])
                                for c in range(NCH):
                                    csl = slice(c * CH, (c + 1) * CH)
                                    for f in range(KT):
                                        py, pyb = bank()
                                        mm(py[:], [pyb], [(wot[:, f * 128:(f + 1) * 128], yt[:, csl])], [wob, ytb])
                                        dve(lambda e, py=py, f=f, csl=csl: e.scalar_tensor_tensor(out=X[:, f, csl], in0=py[:], scalar=modO[:, 0, 16 + f:16 + f + 1],
                                                                                                   in1=X[:, f, csl], op0=ALU.mult, op1=ALU.add), [pyb, XB[f][c], CB], [XB[f][c]])
                        if not final:
                            for d_ in range(2):
                                dve(lambda e, d_=d_: e.tensor_reduce(out=summ[:, :, 2 * d_], in_=rsum[:, :, d_, :], axis=AX.X, op=ALU.add), [rsB], [summB])
                                dve(lambda e, d_=d_: e.tensor_tensor(out=summ[:, :, 2 * d_], in0=summ[:, :, 2 * d_], in1=sc8[:, d_, :], op=ALU.mult), [summB, CB], [summB])
                                act(lambda e, d_=d_: e.activation(out=summ[:, :, 2 * d_], in_=summ[:, :, 2 * d_], func=AF.Exp), [summB], [summB])
                        P.barrier()

                if P2:
                    mixer_pass(False)
                    if last == 2:
                        P.dma("sp", O["summ"], summ[:], reads=[summB])
                        P.barrier()
                if P3:
                    selt = sb("selt", [128, 4], F32, st)
                    cfw = sb("cfw", [128, 4, NB], F32, st)
                    cbw = sb("cbw", [128, 4, NB], F32, st)
                    P.dma("sp", selt[:], I["sel"], writes=[CB])
                    if not FUSED:
                        P.dma("sp", sa[:], I["summ_all"], writes=[CB])
                    else:
                        c2i, c2o = Buf("cc2i"), Buf("cc2o")
                        P.dma("sp", cc2_in, summ[:].rearrange("p a b -> p (a b)"), reads=[summB], writes=[c2i])
                        P.cc((lambda e: e.collective_compute("AllGather", ALU.bypass, replica_groups=[list(range(8))],
                                                             ins=[cc2_in.opt()], outs=[cc2_out.opt()])), reads=[c2i], writes=[c2o])
                        g2a = sb("g2all", [128, 8, NB * 6], F32, st)
                        g2B = Buf("g2all")
                        P.dma("sp", g2a[:], cc2_out.rearrange("(r p) n -> p r n", p=128), reads=[c2o], writes=[g2B])
                        dve(lambda e: e.memset(sa[:], 0.0), [], [CB])
                        for j in range(4):
                            for r in range(8):
                                dve(lambda e, j=j, r=r: e.scalar_tensor_tensor(
                                    out=sa[:, j].rearrange("p a b -> p (a b)"), in0=g2a[:, r, :], scalar=selm[:, 16 + j * 8 + r:17 + j * 8 + r],
                                    in1=sa[:, j].rearrange("p a b -> p (a b)"), op0=ALU.mult, op1=ALU.add), [g2B, CB], [CB])
                    dve(lambda e: e.tensor_copy(out=cfw[:, 0, :], in_=sa[:, 0, :, 4]), [CB], [CB])
                    for j in range(3):
                        dve(lambda e, j=j: e.tensor_tensor(out=cfw[:, j + 1, :], in0=sa[:, j, :, 0], in1=cfw[:, j, :], op=ALU.mult), [CB], [CB])
                        dve(lambda e, j=j: e.tensor_tensor(out=cfw[:, j + 1, :], in0=cfw[:, j + 1, :], in1=sa[:, j, :, 1], op=ALU.add), [CB], [CB])
                    dve(lambda e: e.tensor_copy(out=cbw[:, 3, :], in_=sa[:, 0, :, 5]), [CB], [CB])
                    for j in range(3, 0, -1):
                        dve(lambda e, j=j: e.tensor_tensor(out=cbw[:, j - 1, :], in0=sa[:, j, :, 2], in1=cbw[:, j, :], op=ALU.mult), [CB], [CB])
                        dve(lambda e, j=j: e.tensor_tensor(out=cbw[:, j - 1, :], in0=cbw[:, j - 1, :], in1=sa[:, j, :, 3], op=ALU.add), [CB], [CB])
                    for d_, cc in enumerate([cfw, cbw]):
                        dve(lambda e, d_=d_, cc=cc: e.tensor_scalar(out=hin[:, :, d_], in0=cc[:, 0, :], scalar1=selt[:, 0:1], scalar2=None, op0=ALU.mult), [CB, hinB], [hinB])
                        for j in range(1, 4):
                            dve(lambda e, d_=d_, cc=cc, j=j: e.scalar_tensor_tensor(out=hin[:, :, d_], in0=cc[:, j, :], scalar=selt[:, j:j + 1], in1=hin[:, :, d_],
                                                                                     op0=ALU.mult, op1=ALU.add), [CB, hinB], [hinB])
                    if dbg:
                        P.dma("sp", O["dbg"][:, 0:30], halo[:].rearrange("p a b -> p (a b)"), reads=[haloB])
                        P.dma("sp", O["dbg"][:, 32:32 + 240], sa[:].rearrange("p j a b -> p (j a b)"), reads=[CB])
                        P.dma("sp", O["dbg"][:, 272:292], hin[:].rearrange("p a b -> p (a b)"), reads=[hinB])
                        P.dma("sp", O["dbg"][:, 292:352], summ[:].rearrange("p a b -> p (a b)"), reads=[summB])
                    mixer_pass(True)
                P.barrier()

        if P3:
            with ExitStack() as st:
                h2T = sb("h2Tm", [128, KT, T], BF16, st)
                h2B = [Buf("h2m_%d" % c) for c in range(NCH)]
                gates = sb("gates", [128, T // 128, 8], F32, st)
                gatesB = Buf("gates")
                with ExitStack() as st2:
                    nr = norm_rings(st2)
                    h32 = sb("h32", [128, KT, CH], F32, st2)
                    h32B = Buf("h32")
                    rw = sb("rw", [128, KT, 8], F32, st2)
                    P.dma("sp", rw[:], I["router_w"], writes=[CB])
                    gr = Ring(nc, st2, "gr", [128, 40], F32, 2)
                    for c in range(NCH):
                        norm_chunk(nr, xsrc(c), CH, 3, 0, (lambda kt: modO[:, 0, 24 + kt:24 + kt + 1]),
                                   (lambda kt, c=c: (h2T[:, kt, c * CH:(c + 1) * CH], h2B[c])), dst32=(lambda kt: (h32[:, kt, :], h32B)))
                        for tt in range(4):
                            ti = c * 4 + tt
                            pl, plb = bank()
                            mm(pl[:, 0:8], [plb], [(h32[:, kt, tt * 128:(tt + 1) * 128], rw[:, kt, :]) for kt in range(KT)], [h32B, CB])
                            g_, g_b = gr.next()
                            dve(lambda e, g_=g_, pl=pl: e.tensor_copy(out=g_[:, 0:8], in_=pl[:, 0:8]), [plb], [g_b])
                            dve(lambda e, g_=g_: e.reduce_max(out=g_[:, 8:9], in_=g_[:, 0:8], axis=AX.X), [g_b], [g_b])
                            dve(lambda e, g_=g_: e.tensor_scalar(out=g_[:, 16:24], in0=g_[:, 0:8], scalar1=g_[:, 8:9], scalar2=NEG, op0=ALU.is_equal, op1=ALU.mult), [g_b], [g_b])
                            dve(lambda e, g_=g_: e.tensor_tensor(out=g_[:, 24:32], in0=g_[:, 16:24], in1=g_[:, 0:8], op=ALU.add), [g_b], [g_b])
                            dve(lambda e, g_=g_: e.reduce_max(out=g_[:, 9:10], in_=g_[:, 24:32], axis=AX.X), [g_b], [g_b])
                            dve(lambda e, g_=g_: e.tensor_scalar(out=g_[:, 10:11], in0=g_[:, 8:9], scalar1=-1.0, scalar2=None, op0=ALU.mult), [g_b], [g_b])
                            act(lambda e, g_=g_: e.activation(out=g_[:, 16:24], in_=g_[:, 0:8], func=AF.Exp, bias=g_[:, 10:11], scale=1.0), [g_b], [g_b])
                            dve(lambda e, g_=g_: e.tensor_scalar(out=g_[:, 24:32], in0=g_[:, 0:8], scalar1=g_[:, 9:10], scalar2=None, op0=ALU.is_ge), [g_b], [g_b])
                            dve(lambda e, g_=g_: e.tensor_tensor(out=g_[:, 16:24], in0=g_[:, 16:24], in1=g_[:, 24:32], op=ALU.mult), [g_b], [g_b])
                            dve(lambda e, g_=g_: e.reduce_sum(out=g_[:, 11:12], in_=g_[:, 16:24], axis=AX.X), [g_b], [g_b])
                            dve(lambda e, g_=g_: e.reciprocal(out=g_[:, 11:12], in_=g_[:, 11:12]), [g_b], [g_b])
                            dve(lambda e, g_=g_, ti=ti: e.tensor_scalar(out=gates[:, ti, :], in0=g_[:, 16:24], scalar1=g_[:, 11:12], scalar2=None, op0=ALU.mult), [g_b], [gatesB])
                    P.barrier()
                fr = ffn_rings(st)
                gbr = Ring(nc, st, "gbc", [128, T], F32, 2)
                ger = Ring(nc, st, "Ge", [128, 128], F32, 3)
                for ex in range(0 if FAST else 8):
                    gt, gtb = gbr.next()
                    for c in range(NCH):
                        pgb_, pgbb = lbank()
                        for tt in range(4):
                            ti = c * 4 + tt
                            ge, geb = ger.next()
                            dve(lambda e, ge=ge, ti=ti, ex=ex: e.tensor_scalar(out=ge[:], in0=ones32[:], scalar1=gates[:, ti, ex:ex + 1], scalar2=None, op0=ALU.mult),
                                [gatesB, CB], [geb])
                            mm(pgb_[:, tt * 128:(tt + 1) * 128], [pgbb], [(ge[:], ident32[:])], [geb, CB], skip_self=True)
                        act(lambda e, gt=gt, pgb_=pgb_, c=c: e.activation(out=gt[:, c * CH:(c + 1) * CH], in_=pgb_[:], func=AF.Copy), [pgbb], [gtb])
                    chunks = []
                    for c in range(NCH):
                        chunks.append(((lambda kt, c=c: h2T[:, kt, c * CH:(c + 1) * CH]), h2B[c], CH,
                                       (lambda f, c=c: (X[:, f, c * CH:(c + 1) * CH], XB[f][c])),
                                       (lambda f: modO[:, 0, 40 + f:40 + f + 1]), c * CH))
                    ffn_pass(fr, WS["moe_w1"][ex].rearrange("(kt p) n -> p kt n", p=128), WS["moe_w3"][ex].rearrange("(kt p) n -> p kt n", p=128),
                             WS["moe_w2"][ex].rearrange("(ft p) n -> p ft n", p=128), chunks, gbc=(gt, gtb), wq=WQ, wrd=WRD)
                P.barrier()
            with ExitStack() as st:
                nr = norm_rings(st)
                orr = Ring(nc, st, "ot", [128, CH], F32, 4)
                for c in range(NCH):
                    sqr, rsr, tmr = nr
                    ps, pb = lbank()
                    for kt in range(KT):
                        sq, sqb = sqr.next()
                        act(lambda e, sq=sq, kt=kt, c=c: e.activation(out=sq[:], in_=X[:, kt, c * CH:(c + 1) * CH], func=AF.Square), [XB[kt][c]], [sqb])
                        P.op("pe", (lambda e, ps=ps, sq=sq, kt=kt: e.matmul(ps[:], lhsT=onesb[:], rhs=sq[:], start=(kt == 0), stop=(kt == KT - 1))),
                             [sqb, CB], [pb], skip_self=(kt > 0))
                    rs, rsb = rsr.next()
                    act(lambda e, rs=rs, ps=ps: e.activation(out=rs[:], in_=ps[:], func=AF.Sqrt, scale=1.0 / D, bias=EPS), [pb], [rsb])
                    dve(lambda e, rs=rs: e.reciprocal(out=rs[:], in_=rs[:]), [rsb], [rsb])
                    for kt in range(KT):
                        ot, otb = orr.next()
                        dve(lambda e, ot=ot, kt=kt, c=c, rs=rs: e.scalar_tensor_tensor(out=ot[:], in0=X[:, kt, c * CH:(c + 1) * CH], scalar=vecs[:, 4, kt:kt + 1],
                                                                                       in1=rs[:], op0=ALU.mult, op1=ALU.mult), [XB[kt][c], rsb, CB], [otb])
                        P.dma("sp", O["outT"][:, kt, c * CH:(c + 1) * CH], ot[:], reads=[otb])
                P.barrier()
        P.barrier(final=True)
        P.emit(block)
    return nc


def _fm(v):
    v = np.asarray(v, np.float32)
    return np.ascontiguousarray(v.reshape(-1, 128).T)


def _tokT(a):
    a = np.asarray(a, np.float32)
    return np.ascontiguousarray(a.T.reshape(KT, 128, a.shape[0]).transpose(1, 0, 2))


def _rope_tables(t0):
    pos = np.arange(t0 - 128, t0 - 128 + TE)
    row = (pos // 64).astype(np.float32)
    col = (pos % 64).astype(np.float32)
    freqs = (np.float32(10000.0) ** (-np.arange(16, dtype=np.float32) / np.float32(16))).astype(np.float32)
    out = np.zeros((128, 2, TE), np.float32)
    for p in range(128):
        d = p % 64
        axis, half, f = d // 32, (d % 32) // 16, d % 16
        ang = ((row if axis == 0 else col) * freqs[f]).astype(np.float32)
        out[p, 0] = np.cos(ang)
        out[p, 1] = np.sin(ang) * (-1.0 if half == 0 else 1.0)
    return out


def _masks(s):
    q = np.arange(128)[:, None]
    koff = np.arange(384)[None, :] - 128
    band = np.abs(koff - q) <= 128
    m = np.zeros((128, 3, 384), np.float32)
    for i in range(3):
        v = band.copy()
        if i == 0 and s == 0:
            v &= (koff >= 0)
        if i == 2 and s == 3:
            v &= (koff < 128)
        m[:, i, :] = np.where(v, 0.0, NEG)
    return m


def _swap_idx():
    d = np.arange(64)
    axis, half, f = d // 32, (d % 32) // 16, d % 16
    return axis * 32 + (1 - half) * 16 + f


_CACHE = {}
_LAST = {}


DBG = False


def _prog(phases):
    key = tuple(phases)
    if key not in _CACHE:
        _CACHE[key] = build(list(phases), dbg=DBG)
    return _CACHE[key]


def _common(inp, b):
    f32 = lambda a: np.ascontiguousarray(np.asarray(a, np.float32))
    ccol = np.stack([inp["c"][b], inp["c_ctx"]], 0)
    ccol = np.ascontiguousarray(ccol.reshape(2, KT, 128).transpose(2, 1, 0))
    vecs = np.stack([_fm(inp["norm1_e"][0]), _fm(inp["norm2_e"][0]), _fm(inp["norm1_o"][0]), _fm(inp["norm2_o"][0]), _fm(inp["final_norm"])], 1)
    return {"ccol": f32(ccol), "vecs": f32(vecs), "ident": np.eye(128, dtype=np.float32),
            "ada_w_o": f32(inp["ada_w_o"][0]), "ada_b_o": _fm(inp["ada_b_o"][0]), "w_in_o": f32(inp["w_in_o"][0])}


def _lru_common(inp):
    f32 = lambda a: np.ascontiguousarray(np.asarray(a, np.float32))
    cw = np.asarray(inp["conv_w"][0], np.float32)
    convw = np.ascontiguousarray(cw.reshape(4, NB, 128).transpose(2, 1, 0))
    def d2(a):
        return np.ascontiguousarray(np.asarray(a, np.float32).reshape(2, NB, 128).transpose(2, 0, 1))
    return {"convw": convw, "convb": _fm(inp["conv_b"][0]), "lru_wa": f32(inp["lru_wa"][0]), "lru_wx": f32(inp["lru_wx"][0]),
            "lru_ba": d2(inp["lru_ba"][0]), "lru_bx": d2(inp["lru_bx"][0]), "lru_lam": d2(inp["lru_lambda"][0])}


def _maps1(inp):
    f32 = lambda a: np.ascontiguousarray(np.asarray(a, np.float32))
    x = np.asarray(inp["x"], np.float32)
    S = x.shape[1]
    w_in = np.asarray(inp["w_in_e"][0], np.float32)
    u, v, q, k, val = w_in[:, 0:512], w_in[:, 512:1024], w_in[:, 1024:1536], w_in[:, 1536:1664], w_in[:, 1664:1792]
    sw = _swap_idx()
    qp = np.concatenate([np.concatenate([q[:, j * 64:(j + 1) * 64], q[:, (j + 4) * 64:(j + 5) * 64]], 1) for j in range(4)], 1)
    qps = np.concatenate([np.concatenate([q[:, j * 64:(j + 1) * 64][:, sw], q[:, (j + 4) * 64:(j + 5) * 64][:, sw]], 1) for j in range(4)], 1)
    ks = np.concatenate([k[:, 0:64][:, sw], k[:, 64:128][:, sw]], 1)
    w_in_ext = f32(np.concatenate([u, v, qp, qps, k, ks, val], 1))
    wo = np.asarray(inp["w_out_e"][0], np.float32)
    rows = list(range(512))
    for j in range(4):
        rows += list(range(512 + j * 64, 512 + (j + 1) * 64)) + list(range(512 + (j + 4) * 64, 512 + (j + 5) * 64))
    w_out_p = f32(wo[rows, :])
    wsT = f32(np.asarray(inp["sgu_w"][0], np.float32).transpose(2, 0, 1))
    bsb = f32(np.broadcast_to(np.asarray(inp["sgu_b"][0], np.float32).reshape(1, 512), (128, 512)))
    sinkb = f32(np.broadcast_to(np.asarray(inp["attn_sink"][0], np.float32).reshape(1, 8), (128, 8)))
    shared = {"ada_w_e": f32(inp["ada_w_e"][0]), "ada_b_e": _fm(inp["ada_b_e"][0]), "w_in_e": w_in_ext, "wsT": wsT, "bsb": bsb,
              "sinkb": sinkb, "w_out_e": w_out_p, "ffn_w1": f32(inp["ffn_w1"][0]), "ffn_w3": f32(inp["ffn_w3"][0]), "ffn_w2": f32(inp["ffn_w2"][0])}
    maps = []
    for c in range(8):
        b, s = c // 4, c % 4
        t0 = s * T
        xh = np.zeros((256, D), np.float32)
        if s > 0:
            xh[0:128] = x[b, t0 - 128:t0]
        if t0 + T < S:
            xh[128:256] = x[b, t0 + T:t0 + T + 128]
        m = dict(shared)
        m.update(_common(inp, b))
        m.update({"xT": _tokT(x[b, t0:t0 + T]), "xhT": _tokT(xh), "ctxT": _tokT(inp["ctx"][b]), "ropeCS": _rope_tables(t0), "masks": _masks(s)})
        maps.append(m)
    return maps


def kernel_unfused(**inp):
    inp = {k: np.asarray(v) for k, v in inp.items()}
    cores = list(range(8))
    r1 = run_bass_kernel_spmd(_prog([1]), _maps1(inp), core_ids=cores).results
    lru = _lru_common(inp)
    halos = []
    for c in range(8):
        b, s = c // 4, c % 4
        h = np.zeros((128, NB, 3), np.float32)
        if s > 0:
            h[:, :, 0:2] = r1[c - 1]["brec"][:, :, 1:3]
        if s < 3:
            h[:, :, 2] = r1[c + 1]["brec"][:, :, 0]
        halos.append(h)
    maps2 = []
    for c in range(8):
        m = dict(lru)
        m.update(_common(inp, c // 4))
        m.update({"x1T_in": r1[c]["x1T"], "xc1T_in": r1[c]["xc1T"], "halo": halos[c]})
        maps2.append(m)
    r2 = run_bass_kernel_spmd(_prog([2]), maps2, core_ids=cores).results
    f32 = lambda a: np.ascontiguousarray(np.asarray(a, np.float32))
    rw = np.ascontiguousarray(np.asarray(inp["router_w"][0], np.float32).reshape(KT, 128, 8).transpose(1, 0, 2))
    shared3 = {"w_out_o": f32(inp["w_out_o"][0]), "router_w": rw, "moe_w1": f32(inp["moe_w1"][0]), "moe_w3": f32(inp["moe_w3"][0]),
               "moe_w2": f32(inp["moe_w2"][0])}
    maps3 = []
    for c in range(8):
        b, s = c // 4, c % 4
        m = dict(lru)
        m.update(shared3)
        m.update(_common(inp, b))
        sel = np.zeros((128, 4), np.float32)
        sel[:, s] = 1.0
        sa = np.ascontiguousarray(np.stack([r2[4 * b + j]["summ"] for j in range(4)], 1))
        m.update({"x1T_in": r1[c]["x1T"], "halo": halos[c], "sel": sel, "summ_all": sa})
        maps3.append(m)
    r3 = run_bass_kernel_spmd(_prog([3]), maps3, core_ids=cores).results
    out = np.zeros((2, 4 * T, D), np.float32)
    for c in range(8):
        b, s = c // 4, c % 4
        out[b, s * T:(s + 1) * T, :] = r3[c]["outT"].transpose(2, 1, 0).reshape(T, D)
    return out


def kernel_fused(**inp):
    inp = {k: np.asarray(v) for k, v in inp.items()}
    f32 = lambda a: np.ascontiguousarray(np.asarray(a, np.float32))
    maps = _maps1(inp)
    lru = _lru_common(inp)
    rw = np.ascontiguousarray(np.asarray(inp["router_w"][0], np.float32).reshape(KT, 128, 8).transpose(1, 0, 2))
    shared3 = {"w_out_o": f32(inp["w_out_o"][0]), "router_w": rw, "moe_w1": f32(inp["moe_w1"][0]), "moe_w3": f32(inp["moe_w3"][0]),
               "moe_w2": f32(inp["moe_w2"][0])}
    for c in range(8):
        b, s = c // 4, c % 4
        m = maps[c]
        m.update(lru)
        m.update(shared3)
        sel = np.zeros((128, 4), np.float32)
        sel[:, s] = 1.0
        selm = np.zeros((128, 52), np.float32)
        if s > 0:
            selm[:, c - 1] = 1.0
        if s < 3:
            selm[:, 8 + c + 1] = 1.0
        for j in range(4):
            selm[:, 16 + j * 8 + 4 * b + j] = 1.0
        m.update({"sel": sel, "selm": selm})
    r = run_bass_kernel_spmd(_prog([1, 2, 3]), maps, core_ids=list(range(8))).results
    _LAST["r"] = r
    out = np.zeros((2, 4 * T, D), np.float32)
    for c in range(8):
        b, s = c // 4, c % 4
        out[b, s * T:(s + 1) * T, :] = r[c]["outT"].transpose(2, 1, 0).reshape(T, D)
    return out


FUSED_MODE = False


def kernel(**inp):
    return kernel_fused(**inp) if FUSED_MODE else kernel_unfused(**inp)
```
